# Optimizing a Trainium2 kernel written in Bass

```python
import jax, jax.numpy as jnp
from jax import lax
import numpy as np

D_MODEL = 2048
BATCH = 8
SEQ = 2048
DEPTH = 2

EPS = 1e-6
D_FF = 4 * D_MODEL
GLA_HEADS = 4
GLA_DK = 128
GLA_DV = 256
GLA_GATE_RANK = 16
GLA_GATE_TAU = 16.0
GLA_CHUNK = 64
MOBA_HEADS = 8
MOBA_HD = 128
MOBA_BLOCK = 256
MOBA_TOPK = 3
MOBA_Q_CHUNK = 16
FOX_HEADS = 8
FOX_HD = 128
FOX_Q_BLOCK = 128
HGRN_HEADS = 8
HGRN_DK = 128
HGRN_DV = 128
HGRN_CHUNK = 64
EVEN_SIZES = (GLA_HEADS * GLA_DK, GLA_HEADS * GLA_DK, GLA_HEADS * GLA_DV, GLA_HEADS * GLA_DV, GLA_GATE_RANK,
              MOBA_HEADS * MOBA_HD, MOBA_HEADS * MOBA_HD, MOBA_HEADS * MOBA_HD)
EVEN_IN = 4 * GLA_HEADS * GLA_DK // 2 + 2 * GLA_HEADS * GLA_DV + GLA_GATE_RANK + 3 * MOBA_HEADS * MOBA_HD
EVEN_MIX = GLA_HEADS * GLA_DV + MOBA_HEADS * MOBA_HD
ODD_SIZES = (FOX_HEADS * FOX_HD, FOX_HEADS * FOX_HD, FOX_HEADS * FOX_HD, FOX_HEADS,
             HGRN_HEADS * HGRN_DK, HGRN_HEADS * HGRN_DK, HGRN_HEADS * HGRN_DV, HGRN_HEADS * HGRN_DV)
ODD_IN = 3 * FOX_HEADS * FOX_HD + FOX_HEADS + 2 * HGRN_HEADS * HGRN_DK + 2 * HGRN_HEADS * HGRN_DV
ODD_MIX = FOX_HEADS * FOX_HD + HGRN_HEADS * HGRN_DV
N_EVEN = (DEPTH + 1) // 2
N_ODD = DEPTH // 2

kernel_name = "hybrid_gla_moba_fox_hgrn2_trunk"

F32 = jnp.float32


def rms_norm(x, g):
    xf = x.astype(F32)
    y = xf * lax.rsqrt(jnp.mean(xf * xf, axis=-1, keepdims=True) + EPS)
    return (y * g.astype(F32)).astype(x.dtype)


def _split(u, sizes):
    out, o = [], 0
    for s in sizes:
        out.append(u[..., o:o + s])
        o += s
    return out


def _heads(t, n):
    B, S, _ = t.shape
    return t.reshape(B, S, n, -1).transpose(0, 2, 1, 3)


def _merge(t):
    B, H, S, d = t.shape
    return t.transpose(0, 2, 1, 3).reshape(B, S, H * d)


def alibi_slopes(n):
    return jnp.exp2(-8.0 * jnp.arange(1, n + 1, dtype=F32) / n)


def chunked_gated_linear_attention(q, k, v, log_g, chunk):
    B, H, S, dk = q.shape
    dv = v.shape[-1]
    n = S // chunk

    def to_chunks(t):
        return jnp.moveaxis(t.reshape(B, H, n, chunk, t.shape[-1]), 2, 0)

    qc, kc, vc = to_chunks(q), to_chunks(k), to_chunks(v)
    bc = jnp.cumsum(to_chunks(log_g.astype(F32)), axis=3)
    causal = jnp.tril(jnp.ones((chunk, chunk), dtype=bool))

    def step(state, inp):
        q_, k_, v_, b_ = inp
        b_last = b_[:, :, -1:, :]
        o_inter = jnp.einsum('bhtd,bhde->bhte', q_ * jnp.exp(b_), state)
        diff = b_[:, :, :, None, :] - b_[:, :, None, :, :]
        decay = jnp.exp(jnp.where(causal[:, :, None], diff, -jnp.inf))
        scores = jnp.einsum('bhtd,bhsd,bhtsd->bhts', q_, k_, decay)
        o = o_inter + jnp.einsum('bhts,bhse->bhte', scores, v_)
        state = jnp.exp(b_last[:, :, 0, :, None]) * state + jnp.einsum(
            'bhsd,bhse->bhde', k_ * jnp.exp(b_last - b_), v_)
        return state, o

    init = jnp.zeros((B, H, dk, dv), F32)
    _, o = lax.scan(step, init, (qc, kc, vc, bc))
    return jnp.moveaxis(o, 0, 2).reshape(B, H, S, dv)


def gla_mixer(q, k, v, g, gate_lr, w2, b2, out_gain):
    qh = _heads(q, GLA_HEADS) * (GLA_DK ** -0.5)
    kh = _heads(k, GLA_HEADS)
    vh = _heads(v, GLA_HEADS)
    log_a = jax.nn.log_sigmoid((gate_lr @ w2 + b2).astype(F32)) / GLA_GATE_TAU
    o = chunked_gated_linear_attention(qh, kh, vh, _heads(log_a, GLA_HEADS), GLA_CHUNK)
    o = rms_norm(o, out_gain)
    return (_merge(o) * jax.nn.silu(g.astype(F32))).astype(g.dtype)


def moba_mixer(q, k, v):
    qh = _heads(q, MOBA_HEADS) * (MOBA_HD ** -0.5)
    kh = _heads(k, MOBA_HEADS)
    vh = _heads(v, MOBA_HEADS)
    B, H, S, d = qh.shape
    BLK, CQ = MOBA_BLOCK, MOBA_Q_CHUNK
    nb = -(-S // BLK)
    sp = nb * BLK
    pad = ((0, 0), (0, 0), (0, sp - S), (0, 0))
    qp, kp, vp = jnp.pad(qh, pad), jnp.pad(kh, pad), jnp.pad(vh, pad)
    kb = kp.reshape(B, H, nb, BLK, d)
    vb = vp.reshape(B, H, nb, BLK, d)
    k_mean = jnp.mean(kb.astype(F32), axis=3)
    qblk = jnp.arange(sp) // BLK
    gate = jnp.einsum('bhtd,bhnd->bhtn', qp.astype(F32), k_mean)
    past = jnp.arange(nb)[None, :] < qblk[:, None]
    gate = jnp.where(past, gate, -jnp.inf)
    topk = min(MOBA_TOPK, nb)
    _, sel = lax.top_k(gate, topk)
    slopes = alibi_slopes(H)
    sl5 = slopes.reshape(1, H, 1, 1, 1)
    sl4 = slopes.reshape(1, H, 1, 1)
    b_ix = jnp.arange(B)[:, None, None, None]
    h_ix = jnp.arange(H)[None, :, None, None]
    offs = jnp.arange(BLK)

    def chunk_attend(c):
        t0 = c * CQ
        t = t0 + jnp.arange(CQ)
        j = t0 // BLK
        q_c = lax.dynamic_slice_in_dim(qp, t0, CQ, axis=2)
        idx = lax.dynamic_slice_in_dim(sel, t0, CQ, axis=2)
        k_sel = kb[b_ix, h_ix, idx]
        v_sel = vb[b_ix, h_ix, idx]
        s_sel = idx[..., None] * BLK + offs
        logit_sel = (jnp.einsum('bhqd,bhqjkd->bhqjk', q_c, k_sel).astype(F32)
                     - sl5 * (t[:, None, None] - s_sel).astype(F32))
        logit_sel = jnp.where(idx[..., None] < j, logit_sel, -jnp.inf)
        k_own = lax.dynamic_slice_in_dim(kp, j * BLK, BLK, axis=2)
        v_own = lax.dynamic_slice_in_dim(vp, j * BLK, BLK, axis=2)
        s_own = j * BLK + offs
        logit_own = (jnp.einsum('bhqd,bhkd->bhqk', q_c, k_own).astype(F32)
                     - sl4 * (t[:, None] - s_own[None, :]).astype(F32))
        logit_own = jnp.where(s_own[None, :] <= t[:, None], logit_own, -jnp.inf)
        logits = jnp.concatenate([logit_sel.reshape(B, H, CQ, topk * BLK), logit_own], axis=-1)
        p = jax.nn.softmax(logits, axis=-1)
        p_sel = p[..., :topk * BLK].reshape(B, H, CQ, topk, BLK)
        p_own = p[..., topk * BLK:]
        return (jnp.einsum('bhqjk,bhqjke->bhqe', p_sel, v_sel)
                + jnp.einsum('bhqk,bhke->bhqe', p_own, v_own))

    o = lax.map(chunk_attend, jnp.arange(sp // CQ))
    o = jnp.moveaxis(o, 0, 2).reshape(B, H, sp, d)[:, :, :S]
    return o


def fox_mixer(q, k, v, f_logit, f_bias):
    qh = _heads(q, FOX_HEADS) * (FOX_HD ** -0.5)
    kh = _heads(k, FOX_HEADS)
    vh = _heads(v, FOX_HEADS)
    B, H, S, d = qh.shape
    log_f = jax.nn.log_sigmoid((f_logit + f_bias).astype(F32))
    c = jnp.cumsum(log_f, axis=1).transpose(0, 2, 1)
    outs = []
    for i in range(S // FOX_Q_BLOCK):
        lo, hi = i * FOX_Q_BLOCK, (i + 1) * FOX_Q_BLOCK
        logits = (jnp.einsum('bhqd,bhkd->bhqk', qh[:, :, lo:hi], kh[:, :, :hi]).astype(F32)
                  + c[:, :, lo:hi, None] - c[:, :, None, :hi])
        causal = jnp.arange(lo, hi)[:, None] >= jnp.arange(hi)[None, :]
        p = jax.nn.softmax(jnp.where(causal, logits, -jnp.inf), axis=-1)
        outs.append(jnp.einsum('bhqk,bhkd->bhqd', p, vh[:, :, :hi]))
    return jnp.concatenate(outs, axis=2)


def hgrn2_mixer(q, f_logit, i, g, lower_bound, out_gain):
    f = lower_bound + (1.0 - lower_bound) * jax.nn.sigmoid(f_logit.astype(F32))
    log_f = jnp.log(f)
    k = 1.0 - f
    qa = jax.nn.silu(q)
    o = chunked_gated_linear_attention(_heads(qa, HGRN_HEADS), _heads(k, HGRN_HEADS),
                                       _heads(i, HGRN_HEADS), _heads(log_f, HGRN_HEADS), HGRN_CHUNK)
    o = rms_norm(o, out_gain)
    return (_merge(o) * jax.nn.silu(g.astype(F32))).astype(g.dtype)


def sqrelu_mlp(x, w_up, w_down):
    return jnp.square(jax.nn.relu(x @ w_up)) @ w_down


def setup_inputs(seed: int = 0) -> dict:
    key = jax.random.key(seed)
    ks = jax.random.split(key, 24)

    def nrm(k, shape, scale):
        return jax.random.normal(k, shape, F32) * scale

    def gain(k, shape):
        return 1.0 + 0.02 * jax.random.normal(k, shape, F32)

    return {
        "x": nrm(ks[0], (BATCH, SEQ, D_MODEL), 1.0),
        "ev_norm_mix": gain(ks[1], (N_EVEN, D_MODEL)),
        "ev_w_in": nrm(ks[2], (N_EVEN, D_MODEL, EVEN_IN), D_MODEL ** -0.5),
        "ev_gla_gate_w2": nrm(ks[3], (N_EVEN, GLA_GATE_RANK, GLA_HEADS * GLA_DK), GLA_GATE_RANK ** -0.5),
        "ev_gla_gate_b": nrm(ks[4], (N_EVEN, GLA_HEADS * GLA_DK), 0.1),
        "ev_gla_out_norm": gain(ks[5], (N_EVEN, GLA_DV)),
        "ev_w_out": nrm(ks[6], (N_EVEN, EVEN_MIX, D_MODEL), EVEN_MIX ** -0.5),
        "ev_norm_mlp": gain(ks[7], (N_EVEN, D_MODEL)),
        "ev_w_up": nrm(ks[8], (N_EVEN, D_MODEL, D_FF), D_MODEL ** -0.5),
        "ev_w_down": nrm(ks[9], (N_EVEN, D_FF, D_MODEL), D_FF ** -0.5),
        "od_norm_mix": gain(ks[10], (N_ODD, D_MODEL)),
        "od_w_in": nrm(ks[11], (N_ODD, D_MODEL, ODD_IN), D_MODEL ** -0.5),
        "od_fox_fgate_b": nrm(ks[12], (N_ODD, FOX_HEADS), 0.1),
        "od_hgrn_out_norm": gain(ks[13], (N_ODD, HGRN_DV)),
        "od_w_out": nrm(ks[14], (N_ODD, ODD_MIX, D_MODEL), ODD_MIX ** -0.5),
        "od_norm_mlp": gain(ks[15], (N_ODD, D_MODEL)),
        "od_w_up": nrm(ks[16], (N_ODD, D_MODEL, D_FF), D_MODEL ** -0.5),
        "od_w_down": nrm(ks[17], (N_ODD, D_FF, D_MODEL), D_FF ** -0.5),
        "hgrn_lb_raw": nrm(ks[18], (DEPTH, HGRN_HEADS * HGRN_DK), 0.1),
        "final_norm": gain(ks[19], (D_MODEL,)),
    }


def reference(x, ev_norm_mix, ev_w_in, ev_gla_gate_w2, ev_gla_gate_b, ev_gla_out_norm, ev_w_out,
              ev_norm_mlp, ev_w_up, ev_w_down, od_norm_mix, od_w_in, od_fox_fgate_b, od_hgrn_out_norm,
              od_w_out, od_norm_mlp, od_w_up, od_w_down, hgrn_lb_raw, final_norm):
    lb_soft = jax.nn.softmax(hgrn_lb_raw.astype(F32), axis=0)
    lb_all = jnp.cumsum(lb_soft, axis=0) - lb_soft[0]

    h = x
    for layer in range(DEPTH):
        e = layer // 2
        if layer % 2 == 0:
            u = rms_norm(h, ev_norm_mix[e]) @ ev_w_in[e]
            gq, gk, gv, gg, glr, mq, mk, mv = _split(u, EVEN_SIZES)
            a = gla_mixer(gq, gk, gv, gg, glr, ev_gla_gate_w2[e], ev_gla_gate_b[e], ev_gla_out_norm[e])
            b = _merge(moba_mixer(mq, mk, mv)).astype(a.dtype)
            mix = jnp.concatenate([a, b], axis=-1) @ ev_w_out[e]
            h = h + mix.astype(h.dtype)
            h = h + sqrelu_mlp(rms_norm(h, ev_norm_mlp[e]), ev_w_up[e], ev_w_down[e]).astype(h.dtype)
        else:
            u = rms_norm(h, od_norm_mix[e]) @ od_w_in[e]
            fq, fk, fv, ff, hq, hf, hi, hg = _split(u, ODD_SIZES)
            c = _merge(fox_mixer(fq, fk, fv, ff, od_fox_fgate_b[e]))
            d = hgrn2_mixer(hq, hf, hi, hg, lb_all[layer], od_hgrn_out_norm[e])
            mix = jnp.concatenate([c.astype(d.dtype), d], axis=-1) @ od_w_out[e]
            h = h + mix.astype(h.dtype)
            h = h + sqrelu_mlp(rms_norm(h, od_norm_mlp[e]), od_w_up[e], od_w_down[e]).astype(h.dtype)
    return rms_norm(h, final_norm)
```

```python
import numpy as np
from contextlib import ExitStack
import concourse.bass as bass
import concourse.mybir as mybir
from concourse.bass_utils import run_bass_kernel_spmd

F32 = mybir.dt.float32
BF16 = mybir.dt.bfloat16
AF = mybir.ActivationFunctionType
ALU = mybir.AluOpType
AX = mybir.AxisListType

S = 2048
D = 2048
DFF = 8192
NCH = 16
EPS = 1e-6
NIN = (6160, 7176)
NEG = -30000.0

PE, ACT, DVE, POOL, SP = "pe", "act", "dve", "pool", "sp"
ENGS = [PE, ACT, DVE, POOL, SP]
NDMA_SEM = 8

C_ID, C_UB, C_CM, C_ONE, C_ALC, C_PM, C_P01, C_O01 = 0, 128, 256, 384, 512, 640, 768, 896
C_EM, C_EF, C_AR, C_RM, NCST = 1024, 2048, 3072, 5120, 7168
P_NORM = 0
P_GB = 80
P_GN = 84
P_HN = 86
P_LB = 87
P_FB = 103
NPAR = 104


class Op:
    __slots__ = ("eng", "fn", "deps", "dma", "idx", "signal", "cnt", "dsem", "dval", "prev")

    def __init__(self, eng, fn, dma):
        self.eng, self.fn, self.dma = eng, fn, dma
        self.deps = set()
        self.signal = False
        self.cnt = 0
        self.dsem = None
        self.dval = 0
        self.prev = None


class Prog:
    def __init__(self, nc):
        self.nc = nc
        self.ops = []
        self.lastw = {}
        self.readers = {}
        self.last_eng = {}
        self.dmas_since = []

    def op(self, eng, fn, reads=(), writes=(), dma=False):
        o = Op(eng, fn, dma)
        o.idx = len(self.ops)
        pr = [r for r in reads if r.startswith("pb")]
        if pr:
            reads = [r for r in reads if not r.startswith("pb")]
            writes = list(writes) + pr
        for r in reads:
            w = self.lastw.get(r)
            if w is not None:
                o.deps.add(w)
        for r in writes:
            w = self.lastw.get(r)
            if w is not None:
                o.deps.add(w)
            for rd in self.readers.get(r, ()):
                o.deps.add(rd)
        for r in writes:
            self.lastw[r] = o.idx
            self.readers[r] = []
        for r in reads:
            if r not in writes:
                self.readers.setdefault(r, []).append(o.idx)
        o.deps.discard(o.idx)
        self.ops.append(o)
        if dma:
            self.dmas_since.append(o.idx)
        else:
            self.last_eng[eng] = o.idx
        return o

    def barrier(self):
        deps = set(self.last_eng.values()) | set(self.dmas_since)
        for e in ENGS:
            o = Op(e, None, False)
            o.idx = len(self.ops)
            o.deps = set(deps)
            self.ops.append(o)
        self.lastw.clear()
        self.readers.clear()
        self.dmas_since = []

    def emit(self, stack):
        nc = self.nc
        ops = self.ops
        for o in ops:
            best = {}
            nd = set()
            for d in o.deps:
                p = ops[d]
                if p.dma:
                    nd.add(d)
                    continue
                if p.eng == o.eng and o.eng == PE and not o.dma:
                    continue
                if p.eng not in best or best[p.eng] < d:
                    best[p.eng] = d
            nd |= set(best.values())
            o.deps = nd
            for d in nd:
                ops[d].signal = True
        sems = {e: stack.enter_context(nc.semaphore("s_" + e)) for e in ENGS}
        dsems = {e: [stack.enter_context(nc.semaphore("d_%s_%d" % (e, i))) for i in range(NDMA_SEM)]
                 for e in (ACT, POOL, SP)}
        cnt = {e: 0 for e in ENGS}
        per = {e: [] for e in ENGS}
        hist = {e: [] for e in ENGS}
        for o in ops:
            if o.dma:
                j = len(hist[o.eng])
                o.dsem = dsems[o.eng][j % NDMA_SEM]
                o.dval = 16 * (j // NDMA_SEM + 1)
                if j >= NDMA_SEM:
                    o.prev = hist[o.eng][j - NDMA_SEM]
                hist[o.eng].append(o)
            elif o.signal:
                cnt[o.eng] += 1
                o.cnt = cnt[o.eng]
            per[o.eng].append(o)
        self.stats = {e: len(per[e]) for e in ENGS}
        self.stats["sig"] = dict(cnt)
        block = stack.enter_context(nc.Block())

        def run(eng_name, e):
            waited = {}

            def wait(sem, val):
                k = id(sem)
                if waited.get(k, 0) >= val:
                    return
                waited[k] = val
                e.wait_ge(sem, val)

            for o in per[eng_name]:
                need = {}
                for d in o.deps:
                    p = ops[d]
                    s, v = (p.dsem, p.dval) if p.dma else (sems[p.eng], p.cnt)
                    if id(s) not in need or need[id(s)][1] < v:
                        need[id(s)] = (s, v)
                if o.dma and o.prev is not None:
                    s, v = o.prev.dsem, o.prev.dval
                    if id(s) not in need or need[id(s)][1] < v:
                        need[id(s)] = (s, v)
                for s, v in need.values():
                    wait(s, v)
                if o.fn is None:
                    continue
                ins = o.fn(e)
                if o.dma:
                    ins.then_inc(o.dsem, 16)
                elif o.signal:
                    ins.then_inc(sems[o.eng], 1)
            for o in hist[eng_name][-NDMA_SEM:]:
                wait(o.dsem, o.dval)

        block.tensor(lambda e: run(PE, e))
        block.scalar(lambda e: run(ACT, e))
        block.vector(lambda e: run(DVE, e))
        block.gpsimd(lambda e: run(POOL, e))
        block.sync(lambda e: run(SP, e))


class Arena:
    def __init__(self, ap, nwords):
        self.ap = ap
        self.n = nwords
        self.off = 0

    def alloc(self, shape, dtype=F32, parts=128):
        n = int(np.prod(shape))
        words = n if dtype == F32 else (n + 1) // 2
        words = (words + 7) // 8 * 8
        assert self.off + words <= self.n, ("arena overflow", self.off, words, self.n)
        v = self.ap[0:parts, self.off:self.off + words]
        self.off += words
        if dtype != F32:
            v = v.bitcast(dtype)
        v = v[:, 0:n]
        if len(shape) == 2:
            v = v.rearrange("p (a b) -> p a b", a=shape[0])
        elif len(shape) == 3:
            v = v.rearrange("p (a b c) -> p a b c", a=shape[0], b=shape[1])
        return v


class Builder:
    def __init__(self, dbg=None):
        self.dbg = dbg
        nc = self.nc = bass.Bass("TRN2", target_bir_lowering=False)
        self.P = Prog(nc)

        def di(name, shape, dt=F32):
            return nc.dram_tensor(name, shape, dt, kind="ExternalInput").ap()

        self.xT = di("xT", [D, S])
        self.W = []
        for l, p in ((0, "ev"), (1, "od")):
            self.W.append(dict(w_in=di(p + "_w_in", [D, NIN[l]]), w_out=di(p + "_w_out", [D, D]),
                               w_up=di(p + "_w_up", [D, DFF]), w_down=di(p + "_w_down", [DFF, D])))
        self.w2d = di("w2", [16, 512])
        self.prmd = di("prm", [128, NPAR])
        self.cstd = di("cst", [128, NCST])
        self.outT = nc.dram_tensor("outT", [D, S], F32, kind="ExternalOutput").ap()
        self.hT = nc.dram_tensor("hT", [D, S], F32).ap()
        self.mixd = nc.dram_tensor("mixd", [D, S], BF16, kind=("ExternalOutput" if dbg in ("mix", "s2", "s3", "s4", "s5", "s6") else "Internal")).ap()
        if dbg == "h":
            self.hdbg = nc.dram_tensor("hdbg", [D, S], F32, kind="ExternalOutput").ap()

    def bank(self, b, n=512, c0=0):
        return self.PS[:, 512 * b + c0:512 * b + c0 + n]

    def banks4(self, h):
        return self.PS[:, 2048 * h:2048 * h + 2048]

    def pk(self, h):
        return ["pb%d" % b for b in range(4 * h, 4 * h + 4)]

    def run(self):
        nc, P = self.nc, self.P
        with ExitStack() as st:
            NW = 53000
            arena_t = st.enter_context(nc.sbuf_tensor("arena", [128, NW], F32))
            self.PS = st.enter_context(nc.psum_tensor("ps", [128, 4096], F32))
            A = self.A = Arena(arena_t, NW)
            self.XN = A.alloc([NCH, S], BF16)
            self.WB = [A.alloc([NCH, 256], BF16) for _ in range(2)]
            self.HS = [A.alloc([S], F32) for _ in range(2)]
            self.RSTD = A.alloc([S], F32)
            self.SQB = A.alloc([2, S], BF16)
            self.cbf = A.alloc([512], BF16)
            self.EM = A.alloc([8, 128], BF16)
            self.EF = A.alloc([8, 128], BF16)
            self.ROWM = A.alloc([S], BF16)
            self.ROWF = A.alloc([S], BF16)
            self.RM = A.alloc([S], BF16)
            self.cf = A.alloc([1024], F32)
            self.prm = A.alloc([NPAR], F32)
            self.w2 = A.alloc([512], F32)
            self.lbt = A.alloc([32], F32)
            self.mark = A.off
            self.wslot = 0
            self.ident_bf = self.cbf[:, 0:128]
            self.ublk_bf = self.cbf[:, 128:256]
            self.cm_bf = self.cbf[:, 256:384]
            self.ones_bf = self.cbf[:, 384:512]
            self.ident_f = self.cf[:, C_ID:C_ID + 128]
            self.ublk_f = self.cf[:, C_UB:C_UB + 128]

            self.load_consts()
            self.copy_x()
            if self.dbg != "s1":
                self.layer(0)
            if self.dbg not in ("l0", "mix", "s1", "s2", "s3", "s4"):
                self.layer(1)
            self.final_norm()
            P.emit(st)
        return nc

    def load_consts(self):
        P, c = self.P, self.cstd
        P.op(POOL, lambda e: e.dma_start(out=self.cbf, in_=c[:, 0:512]), writes=["cbf"], dma=True)
        P.op(POOL, lambda e: e.dma_start(out=self.EM.rearrange("p a b -> p (a b)"), in_=c[:, C_EM:C_EM + 1024]),
             writes=["EM"], dma=True)
        P.op(POOL, lambda e: e.dma_start(out=self.EF.rearrange("p a b -> p (a b)"), in_=c[:, C_EF:C_EF + 1024]),
             writes=["EF"], dma=True)
        P.op(POOL, lambda e: e.dma_start(out=self.ROWM, in_=c[:, C_AR:C_AR + S]), writes=["ROWM"], dma=True)
        P.op(POOL, lambda e: e.dma_start(out=self.RM, in_=c[:, C_RM:C_RM + S]), writes=["RM"], dma=True)
        P.op(SP, lambda e: e.dma_start(out=self.cf, in_=c[:, 0:1024]), writes=["cf"], dma=True)
        P.op(SP, lambda e: e.dma_start(out=self.prm, in_=self.prmd), writes=["prm"], dma=True)
        P.op(SP, lambda e: e.dma_start(out=self.w2[0:16, :], in_=self.w2d), writes=["w2"], dma=True)
        P.op(DVE, lambda e: e.memset(self.ROWF, 0.0), writes=["ROWF"])

    def copy_x(self):
        for c in range(NCH):
            self.P.op(SP, lambda e, c=c: e.dma_start(out=self.hT[c * 128:(c + 1) * 128, :],
                                                      in_=self.xT[c * 128:(c + 1) * 128, :]),
                      writes=["hT%d" % c], dma=True)

    def norm(self, gcol, final=False):
        P = self.P
        for c in range(NCH):
            hs, hk = self.HS[c % 2], "HS%d" % (c % 2)
            sq, sk = self.SQB[:, c % 2, :], "SQB%d" % (c % 2)
            P.op(SP, lambda e, c=c, hs=hs: e.dma_start(out=hs, in_=self.hT[c * 128:(c + 1) * 128, :]),
                 reads=["hT%d" % c], writes=[hk], dma=True)
            P.op(ACT, lambda e, hs=hs, sq=sq: e.activation(out=sq, in_=hs, func=AF.Square),
                 reads=[hk], writes=[sk])
            for n in range(4):
                P.op(PE, lambda e, c=c, n=n, sq=sq: e.matmul(self.bank(n), lhsT=self.ones_bf,
                                                             rhs=sq[:, n * 512:(n + 1) * 512],
                                                             start=(c == 0), stop=(c == NCH - 1)),
                     reads=[sk, "cbf"], writes=["pb%d" % n])
        P.op(ACT, lambda e: e.activation(out=self.RSTD, in_=self.banks4(0), func=AF.Ln, scale=1.0 / D, bias=EPS),
             reads=self.pk(0), writes=["RSTD"])
        P.op(ACT, lambda e: e.activation(out=self.RSTD, in_=self.RSTD, func=AF.Exp, scale=-0.5),
             reads=["RSTD"], writes=["RSTD"])
        for c in range(NCH):
            hs, hk = self.HS[c % 2], "HS%d" % (c % 2)
            P.op(SP, lambda e, c=c, hs=hs: e.dma_start(out=hs, in_=self.hT[c * 128:(c + 1) * 128, :]),
                 reads=["hT%d" % c], writes=[hk], dma=True)
            g = self.prm[:, gcol + c:gcol + c + 1]
            if not final:
                P.op(DVE, lambda e, c=c, hs=hs, g=g: e.scalar_tensor_tensor(
                    out=self.XN[:, c, :], in0=hs, scalar=g, in1=self.RSTD, op0=ALU.mult, op1=ALU.mult),
                    reads=[hk, "RSTD", "prm"], writes=["xn%d" % c])
            else:
                P.op(DVE, lambda e, c=c, hs=hs, g=g: e.scalar_tensor_tensor(
                    out=hs, in0=hs, scalar=g, in1=self.RSTD, op0=ALU.mult, op1=ALU.mult),
                    reads=[hk, "RSTD", "prm"], writes=[hk])
                P.op(SP, lambda e, c=c, hs=hs: e.dma_start(out=self.outT[c * 128:(c + 1) * 128, :], in_=hs),
                     reads=[hk], writes=["out%d" % c], dma=True)

    def final_norm(self):
        if self.dbg == "h":
            for c in range(NCH):
                self.P.op(SP, lambda e, c=c: e.dma_start(out=self.hdbg[c * 128:(c + 1) * 128, :],
                                                          in_=self.hT[c * 128:(c + 1) * 128, :]),
                          reads=["hT%d" % c], writes=["hd%d" % c], dma=True)
        self.norm(P_NORM + 64, final=True)

    def load_w(self, wd, r0, nk, c0, ncols):
        s = self.wslot % 2
        self.wslot += 1
        wb, key = self.WB[s], "wb%d" % s
        self.P.op(POOL, lambda e: e.dma_start(
            out=wb[:, 0:nk, 0:ncols],
            in_=wd[r0:r0 + nk * 128, c0:c0 + ncols].rearrange("(c p) n -> p c n", p=128)),
            writes=[key], dma=True)
        return wb, key

    def gemm_fm(self, wd, r0, c0, widths, rhs, rkey, evac, nk=NCH, nbank=4):
        P = self.P
        mi = 0
        i = 0
        while i < len(widths):
            grp = [widths[i]]
            if i + 1 < len(widths) and widths[i] + widths[i + 1] <= 256:
                grp.append(widths[i + 1])
            tot = sum(grp)
            wb, wkey = self.load_w(wd, r0, nk, c0, tot)
            off = 0
            for mw in grp:
                h = self.gpar % 2
                self.gpar += 1
                for k in range(nk):
                    for n in range(nbank):
                        P.op(PE, lambda e, k=k, n=n, h=h, off=off, mw=mw, wb=wb: e.matmul(
                            self.bank(4 * h + n)[0:mw, :], lhsT=wb[:, k, off:off + mw], rhs=rhs(k, n),
                            start=(k == 0), stop=(k == nk - 1)),
                            reads=[wkey, rkey(k)], writes=["pb%d" % (4 * h + n)])
                evac(mi, h)
                mi += 1
                off += mw
            c0 += tot
            i += len(grp)

    def xn_rhs(self, k, n):
        return self.XN[:, k, n * 512:(n + 1) * 512]

    def gemm_tm(self, wd, c0, ncols, vtm, vkey):
        P = self.P
        wb, wkey = self.load_w(wd, 0, NCH, c0, ncols)
        per_bank = 512 // ncols
        for t0 in range(0, NCH, per_bank):
            b = self.tmb % 8
            self.tmb += 1
            for j in range(per_bank):
                t = t0 + j
                for k in range(NCH):
                    P.op(PE, lambda e, k=k, t=t, j=j, b=b: e.matmul(
                        self.bank(b, ncols, j * ncols), lhsT=self.XN[:, k, t * 128:(t + 1) * 128],
                        rhs=wb[:, k, 0:ncols], start=(k == 0), stop=(k == NCH - 1)),
                        reads=[wkey, "xn%d" % k], writes=["pb%d" % b])
            P.op(ACT, lambda e, t0=t0, b=b: e.activation(
                out=vtm[:, t0:t0 + per_bank, :],
                in_=self.bank(b).rearrange("p (a c) -> p a c", a=per_bank), func=AF.Copy),
                reads=["pb%d" % b], writes=[vkey])

    def gla_core(self, qg, kg, kgtm, vtm, eb, dvh, oT, St, Sbf, PT):
        P = self.P
        dv = dvh * 128
        P.op(DVE, lambda e: e.memset(St, 0.0), writes=["St"])
        P.op(DVE, lambda e: e.memset(Sbf[0], 0.0), writes=["Sbf0"])
        sidx = 0
        for i in range(NCH):
            tl = slice(i * 128, (i + 1) * 128)
            g = i // 4
            pt, ptk = PT[i % 2], "PT%d" % (i % 2)
            P.op(PE, lambda e, tl=tl: e.matmul(self.bank(4, 128), lhsT=kg[:, tl], rhs=qg[:, tl], start=True, stop=True),
                 reads=["kg", "qg"], writes=["pb4"])
            P.op(DVE, lambda e, pt=pt: e.tensor_tensor(out=pt, in0=self.bank(4, 128), in1=self.ublk_f, op=ALU.mult),
                 reads=["pb4", "cf"], writes=[ptk])
            for cc in range(2):
                rs = slice(cc * 64, cc * 64 + 64)
                P.op(PE, lambda e, i=i, cc=cc, rs=rs: e.matmul(
                    self.bank(5 + cc, dv), lhsT=kgtm[rs, i, :], rhs=vtm[rs, i, :], start=True, stop=True),
                    reads=["kgtm", "vtm"], writes=["pb%d" % (5 + cc)])
            obanks = [(2 * (g % 2) + hh) for hh in range(dvh)]
            col = (i % 4) * 128
            for hh in range(dvh):
                ob = obanks[hh]
                P.op(PE, lambda e, i=i, hh=hh, ob=ob, col=col, pt=pt: e.matmul(
                    self.bank(ob, 128, col), lhsT=vtm[:, i, hh * 128:(hh + 1) * 128], rhs=pt, start=True, stop=False),
                    reads=["vtm", ptk], writes=["pb%d" % ob])
            for cc in range(2):
                c = 2 * i + cc
                cur, nxt = Sbf[sidx % 2], Sbf[(sidx + 1) % 2]
                ck, nk_ = "Sbf%d" % (sidx % 2), "Sbf%d" % ((sidx + 1) % 2)
                qs = slice(i * 128 + cc * 64, i * 128 + cc * 64 + 64)
                for hh in range(dvh):
                    ob = obanks[hh]
                    P.op(PE, lambda e, hh=hh, ob=ob, col=col, cc=cc, cur=cur, qs=qs: e.matmul(
                        self.bank(ob, 64, col + cc * 64), lhsT=cur[:, hh * 128:(hh + 1) * 128], rhs=qg[:, qs],
                        start=False, stop=(cc == 1)),
                        reads=[ck, "qg"], writes=["pb%d" % ob])
                if c == 2 * NCH - 1:
                    break
                el = eb[:, c * 64 + 63:c * 64 + 64]
                P.op(DVE, lambda e, cc=cc, nxt=nxt, el=el: e.scalar_tensor_tensor(
                    out=nxt, in0=St, scalar=el, in1=self.bank(5 + cc, dv), op0=ALU.mult, op1=ALU.add),
                    reads=["pb%d" % (5 + cc), "St", "eb"], writes=[nk_])
                P.op(DVE, lambda e, cc=cc, el=el: e.scalar_tensor_tensor(
                    out=St, in0=St, scalar=el, in1=self.bank(5 + cc, dv), op0=ALU.mult, op1=ALU.add),
                    reads=["pb%d" % (5 + cc), "St", "eb"], writes=["St"])
                sidx += 1
            if i % 4 == 3:
                for hh in range(dvh):
                    ob = obanks[hh]
                    P.op(ACT, lambda e, hh=hh, ob=ob, g=g: e.activation(
                        out=oT[:, hh, g * 512:(g + 1) * 512], in_=self.bank(ob), func=AF.Copy),
                        reads=["pb%d" % ob], writes=["oT"])

    def transpose_kg(self, kg, kgtm):
        P = self.P
        for hb in range(2):
            b = 6 + hb
            pbv = self.bank(b).bitcast(BF16)
            for j in range(8):
                t = hb * 8 + j
                P.op(PE, lambda e, t=t, j=j, pbv=pbv: e.transpose(pbv[:, j * 128:(j + 1) * 128],
                                                                  kg[:, t * 128:(t + 1) * 128], self.ident_bf),
                     reads=["SQB0", "cbf"], writes=["pb%d" % b])
            P.op(DVE, lambda e, hb=hb, pbv=pbv: e.tensor_copy(
                out=kgtm[:, hb * 8:hb * 8 + 8, :], in_=pbv.rearrange("p (a c) -> p a c", a=8)),
                reads=["pb%d" % b], writes=["kgtm"])

    def gated_out(self, oT, sg, dvh, gain_col, row0):
        P = self.P
        dv = dvh * 128
        for hh in range(dvh):
            P.op(ACT, lambda e, hh=hh: e.activation(out=self.SQB[:, hh, :], in_=oT[:, hh, :], func=AF.Square),
                 reads=["oT"], writes=["SQB%d" % hh])
        for n in range(4):
            for hh in range(dvh):
                P.op(PE, lambda e, n=n, hh=hh: e.matmul(self.bank(4 + n), lhsT=self.ones_bf,
                                                        rhs=self.SQB[:, hh, n * 512:(n + 1) * 512],
                                                        start=(hh == 0), stop=(hh == dvh - 1)),
                     reads=["SQB%d" % hh, "cbf"], writes=["pb%d" % (4 + n)])
        P.op(ACT, lambda e: e.activation(out=self.RSTD, in_=self.banks4(1), func=AF.Ln, scale=1.0 / dv, bias=EPS),
             reads=self.pk(1), writes=["RSTD"])
        P.op(ACT, lambda e: e.activation(out=self.RSTD, in_=self.RSTD, func=AF.Exp, scale=-0.5),
             reads=["RSTD"], writes=["RSTD"])
        for hh in range(dvh):
            g = self.prm[:, gain_col + hh:gain_col + hh + 1]
            P.op(DVE, lambda e, hh=hh, g=g: e.scalar_tensor_tensor(
                out=oT[:, hh, :], in0=oT[:, hh, :], scalar=g, in1=self.RSTD, op0=ALU.mult, op1=ALU.mult),
                reads=["oT", "RSTD", "prm"], writes=["oT"])
            ms = self.HS[1].bitcast(BF16)[:, hh * S:(hh + 1) * S]
            P.op(DVE, lambda e, hh=hh, ms=ms: e.tensor_tensor(out=ms, in0=oT[:, hh, :], in1=sg[:, hh, :], op=ALU.mult),
                 reads=["oT", "sg"], writes=["ms%d" % hh])
            r = row0 + hh * 128
            P.op(SP, lambda e, ms=ms, r=r: e.dma_start(out=self.mixd[r:r + 128, :], in_=ms),
                 reads=["ms%d" % hh], writes=["mix%d" % (r // 128)], dma=True)

    def attention(self, qT, kT, vtm, bias_col, bias_key, row_lhsT, row_rhs, nrow, row_keys, Pb, osb, row0):
        P = self.P
        sidx = 0
        for n in range(4):
            ob, lb = (4, 5) if n % 2 == 0 else (6, 7)
            jmax = 4 * n + 3
            for j in range(jmax + 1):
                c0 = max(0, j - 4 * n) * 128
                N = 512 - c0
                sb = sidx % 4
                pb, pbk = Pb[sidx % 3], "Pb%d" % (sidx % 3)
                sidx += 1
                diag = j >= 4 * n
                tq = slice(512 * n + c0, 512 * (n + 1))
                P.op(PE, lambda e, j=j, sb=sb, c0=c0, N=N, tq=tq: e.matmul(
                    self.bank(sb, N, c0), lhsT=kT[:, j * 128:(j + 1) * 128], rhs=qT[:, tq], start=True, stop=False),
                    reads=["kT", "qT"], writes=["pb%d" % sb])
                P.op(PE, lambda e, j=j, sb=sb, c0=c0, N=N, tq=tq, diag=diag: e.matmul(
                    self.bank(sb, N, c0), lhsT=row_lhsT(j), rhs=row_rhs[0:nrow, tq], start=False, stop=(not diag)),
                    reads=row_keys, writes=["pb%d" % sb])
                if diag:
                    P.op(PE, lambda e, sb=sb, c0=c0: e.matmul(
                        self.bank(sb, 128, c0), lhsT=self.ident_bf, rhs=self.cm_bf, start=False, stop=True),
                        reads=["cbf"], writes=["pb%d" % sb])
                P.op(ACT, lambda e, j=j, sb=sb, c0=c0, N=N, pb=pb: e.activation(
                    out=pb[:, c0:512], in_=self.bank(sb, N, c0), func=AF.Exp, bias=bias_col(j)),
                    reads=["pb%d" % sb, bias_key], writes=[pbk])
                P.op(PE, lambda e, j=j, ob=ob, c0=c0, N=N, pb=pb, jmax=jmax: e.matmul(
                    self.bank(ob, N, c0), lhsT=vtm[:, j, :], rhs=pb[:, c0:512], start=(j == 0), stop=(j == jmax)),
                    reads=["vtm", pbk], writes=["pb%d" % ob])
                P.op(PE, lambda e, j=j, lb=lb, c0=c0, N=N, pb=pb, jmax=jmax: e.matmul(
                    self.bank(lb, N, c0), lhsT=self.ones_bf, rhs=pb[:, c0:512], start=(j == 0), stop=(j == jmax)),
                    reads=["cbf", pbk], writes=["pb%d" % lb])
            rl, rk = osb[n % 2], "osb%d" % (n % 2)
            P.op(DVE, lambda e, lb=lb, rl=rl: e.reciprocal(out=rl, in_=self.bank(lb)), reads=["pb%d" % lb], writes=[rk])
            ms = self.HS[1].bitcast(BF16)[:, n * 512:(n + 1) * 512]
            P.op(DVE, lambda e, ob=ob, rl=rl, ms=ms: e.tensor_tensor(out=ms, in0=self.bank(ob), in1=rl, op=ALU.mult),
                 reads=["pb%d" % ob, rk], writes=["ms%d" % n])
            P.op(SP, lambda e, n=n, ms=ms: e.dma_start(out=self.mixd[row0:row0 + 128, n * 512:(n + 1) * 512], in_=ms),
                 reads=["ms%d" % n], writes=["mix%d_%d" % (row0 // 128, n)], dma=True)

    def layer(self, l):
        P = self.P
        W = self.W[l]
        self.gpar = 0
        self.tmb = 0
        self.norm(P_NORM + 32 * l)
        P.barrier()
        if l == 0:
            self.mix_even(W["w_in"])
        else:
            self.mix_odd(W["w_in"])
        P.barrier()
        self.A.off = self.mark
        if self.dbg in ("s2", "s3", "s5", "s6"):
            return
        P.op(SP, lambda e: e.dma_start(out=self.XN[:, 0:8, :],
                                       in_=self.mixd[0:1024, :].rearrange("(c p) t -> p c t", p=128)),
             writes=["xn%d" % c for c in range(8)], dma=True)
        P.op(SP, lambda e: e.dma_start(out=self.XN[:, 8:16, :],
                                       in_=self.mixd[1024:2048, :].rearrange("(c p) t -> p c t", p=128)),
             writes=["xn%d" % c for c in range(8, 16)], dma=True)
        self.gemm_fm(W["w_out"], 0, 0, [128] * 16, self.xn_rhs, lambda k: "xn%d" % k, self.evac_resid(0))
        P.barrier()
        self.norm(P_NORM + 32 * l + 16)
        aT = self.A.alloc([NCH, S], BF16)
        for fg in range(4):
            def evac_up(mi, h, aT=aT):
                hs, hk = self.HS[mi % 2], "HS%d" % (mi % 2)
                P.op(ACT, lambda e, h=h, hs=hs: e.activation(out=hs, in_=self.banks4(h), func=AF.Relu),
                     reads=self.pk(h), writes=[hk])
                P.op(DVE, lambda e, mi=mi, hs=hs: e.tensor_tensor(out=aT[:, mi, :], in0=hs, in1=hs, op=ALU.mult),
                     reads=[hk], writes=["aT%d" % mi])
            self.gemm_fm(W["w_up"], 0, fg * 2048, [128] * 16, self.xn_rhs, lambda k: "xn%d" % k, evac_up)
            self.gemm_fm(W["w_down"], fg * 2048, 0, [128] * 16,
                         lambda k, n, aT=aT: aT[:, k, n * 512:(n + 1) * 512], lambda k: "aT%d" % k,
                         self.evac_resid(0))
        P.barrier()
        self.A.off = self.mark

    def evac_resid(self, _):
        P = self.P

        def ev(mi, h):
            hs, hk = self.HS[mi % 2], "HS%d" % (mi % 2)
            P.op(ACT, lambda e, h=h, hs=hs: e.activation(out=hs, in_=self.banks4(h), func=AF.Copy),
                 reads=self.pk(h), writes=[hk])
            P.op(POOL, lambda e, mi=mi, hs=hs: e.dma_start(out=self.hT[mi * 128:(mi + 1) * 128, :], in_=hs,
                                                           accum_op=ALU.add),
                 reads=[hk], writes=["hT%d" % mi], dma=True)
        return ev

    def mix_even(self, w_in):
        P, A = self.P, self.A
        xk = lambda k: "xn%d" % k
        glrT = self.HS[0]
        def ev_glr(mi, h):
            P.op(ACT, lambda e, h=h: e.activation(out=glrT[0:16, :], in_=self.banks4(h)[0:16, :], func=AF.Copy),
                 reads=self.pk(h), writes=["glr"])
        self.gemm_fm(w_in, 0, 3072, [16], self.xn_rhs, xk, ev_glr)
        m0 = A.off
        eb, enb, ebl = A.alloc([S]), A.alloc([S]), A.alloc([S])
        qg, kg = A.alloc([S], BF16), A.alloc([S], BF16)
        kgl = self.SQB[:, 0, :]
        c3 = lambda a: a.rearrange("p (c j) -> p c j", j=64)
        kgtm = A.alloc([NCH, 128], BF16)
        vtm = A.alloc([NCH, 256], BF16)
        sg = A.alloc([2, S], BF16)
        oT = A.alloc([2, S])
        St = A.alloc([256])
        Sbf = [A.alloc([256], BF16) for _ in range(2)]
        PT = [A.alloc([128], BF16) for _ in range(2)]
        cs = self.RSTD
        for hd in ([] if self.dbg == "s3" else [0] if self.dbg == "s2" else range(4)):
            for n in range(4):
                P.op(PE, lambda e, hd=hd, n=n: e.matmul(self.bank(n), lhsT=self.w2[0:16, hd * 128:(hd + 1) * 128],
                                                       rhs=glrT[0:16, n * 512:(n + 1) * 512], start=True, stop=True),
                     reads=["w2", "glr"], writes=["pb%d" % n])
            nb = self.lbt[:, 16 + hd:17 + hd]
            P.op(DVE, lambda e, hd=hd, nb=nb: e.tensor_scalar(out=nb, in0=self.prm[:, P_GB + hd:P_GB + hd + 1],
                                                             scalar1=-1.0, scalar2=None, op0=ALU.mult),
                 reads=["prm"], writes=["nb"])
            P.op(ACT, lambda e, nb=nb: e.activation(out=eb, in_=self.banks4(0), func=AF.Exp, scale=-1.0, bias=nb),
                 reads=self.pk(0) + ["nb"], writes=["eb"])
            P.op(ACT, lambda e: e.activation(out=eb, in_=eb, func=AF.Ln, bias=1.0), reads=["eb"], writes=["eb"])
            P.op(DVE, lambda e: e.tensor_tensor_scan(out=cs, data0=self.RM, data1=eb, initial=0.0,
                                                     op0=ALU.mult, op1=ALU.add),
                 reads=["RM", "eb"], writes=["RSTD"])
            P.op(ACT, lambda e: e.activation(out=eb, in_=cs, func=AF.Exp, scale=-1.0 / 16), reads=["RSTD"], writes=["eb"])
            P.op(ACT, lambda e: e.activation(out=enb, in_=cs, func=AF.Exp, scale=1.0 / 16), reads=["RSTD"], writes=["enb"])
            P.op(DVE, lambda e: e.tensor_tensor(out=c3(ebl), in0=c3(enb), in1=c3(eb)[:, :, 63:64].to_broadcast([128, 32, 64]),
                                                op=ALU.mult), reads=["enb", "eb"], writes=["ebl"])

            def ev_q(mi, h):
                P.op(DVE, lambda e, h=h: e.scalar_tensor_tensor(out=qg, in0=self.banks4(h), scalar=128 ** -0.5, in1=eb,
                                                                op0=ALU.mult, op1=ALU.mult),
                     reads=self.pk(h) + ["eb"], writes=["qg"])

            def ev_k(mi, h):
                P.op(DVE, lambda e, h=h: e.tensor_tensor(out=kg, in0=self.banks4(h), in1=enb, op=ALU.mult),
                     reads=self.pk(h) + ["enb"], writes=["kg"])
                P.op(DVE, lambda e, h=h: e.tensor_tensor(out=kgl, in0=self.banks4(h), in1=ebl, op=ALU.mult),
                     reads=self.pk(h) + ["ebl"], writes=["SQB0"])

            def ev_g(mi, h):
                P.op(ACT, lambda e, mi=mi, h=h: e.activation(out=sg[:, mi, :], in_=self.banks4(h), func=AF.Silu),
                     reads=self.pk(h), writes=["sg"])
            self.gemm_fm(w_in, 0, hd * 128, [128], self.xn_rhs, xk, ev_q)
            self.gemm_fm(w_in, 0, 512 + hd * 128, [128], self.xn_rhs, xk, ev_k)
            self.gemm_fm(w_in, 0, 2048 + hd * 256, [128, 128], self.xn_rhs, xk, ev_g)
            self.gemm_tm(w_in, 1024 + hd * 256, 256, vtm, "vtm")
            self.transpose_kg(kgl, kgtm)
            self.gla_core(qg, kg, kgtm, vtm, eb, 2, oT, St, Sbf, PT)
            self.gated_out(oT, sg, 2, P_GN, hd * 256)
        P.barrier()
        A.off = m0
        qT, kT = A.alloc([S], BF16), A.alloc([S], BF16)
        vt = A.alloc([NCH, 128], BF16)
        Pb = [A.alloc([512], BF16) for _ in range(3)]
        osb = [A.alloc([512]) for _ in range(2)]
        Gm = A.alloc([NCH, 8])
        m8 = A.alloc([NCH, 8])
        sel = A.alloc([NCH, 8])
        ksum = A.alloc([8])
        kmT = A.alloc([8], BF16)
        Eh = A.alloc([8, 128], BF16)
        pm = self.cf[:, C_PM:C_PM + 128].rearrange("p (a b) -> p a b", a=NCH)
        p01 = self.cf[:, C_P01:C_P01 + 128].rearrange("p (a b) -> p a b", a=NCH)
        o01 = self.cf[:, C_O01:C_O01 + 128].rearrange("p (a b) -> p a b", a=NCH)
        for hd in ([] if self.dbg == "s2" else [0, 5] if self.dbg == "s3" else range(8)):
            slope = 2.0 ** (-(hd + 1))

            def ev_q(mi, h):
                P.op(DVE, lambda e, h=h: e.tensor_scalar(out=qT, in0=self.banks4(h), scalar1=128 ** -0.5, scalar2=None,
                                                         op0=ALU.mult),
                     reads=self.pk(h), writes=["qT"])

            def ev_k(mi, h):
                for n in range(8):
                    P.op(DVE, lambda e, h=h, n=n: e.tensor_scalar(
                        out=kT[:, n * 256:(n + 1) * 256], in0=self.banks4(h)[:, n * 256:(n + 1) * 256], scalar1=1.0,
                        scalar2=None, op0=ALU.mult, op1=ALU.add, accum_out=ksum[:, n:n + 1]),
                        reads=self.pk(h), writes=["kT", "ksum"])
                P.op(DVE, lambda e: e.tensor_scalar(out=kmT, in0=ksum, scalar1=1.0 / 256, scalar2=None, op0=ALU.mult),
                     reads=["ksum"], writes=["kmT"])
            self.gemm_fm(w_in, 0, 3088 + hd * 128, [128], self.xn_rhs, xk, ev_q)
            self.gemm_fm(w_in, 0, 4112 + hd * 128, [128], self.xn_rhs, xk, ev_k)
            self.gemm_tm(w_in, 5136 + hd * 128, 128, vt, "vtm")
            import os
            stop = os.environ.get("MOBA_STOP", "")
            if stop in ("a", "a0"):
                continue
            for i in range(NCH):
                P.op(PE, lambda e, i=i: e.matmul(self.bank(0, 8, i * 8), lhsT=qT[:, i * 128:(i + 1) * 128], rhs=kmT,
                                                start=True, stop=True),
                     reads=["qT", "kmT"], writes=["pb0"])
            P.op(DVE, lambda e: e.tensor_tensor(out=Gm, in0=self.bank(0, 128).rearrange("p (a b) -> p a b", a=NCH),
                                                in1=pm, op=ALU.add),
                 reads=["pb0", "cf"], writes=["Gm"])
            for i in range(NCH):
                P.op(DVE, lambda e, i=i: e.max(out=m8[:, i, :], in_=Gm[:, i, :]), reads=["Gm"], writes=["m8"])
            P.op(DVE, lambda e: e.tensor_tensor(out=sel, in0=Gm, in1=m8[:, :, 2:3].to_broadcast([128, NCH, 8]),
                                                op=ALU.is_ge),
                 reads=["Gm", "m8"], writes=["sel"])
            P.op(DVE, lambda e: e.tensor_tensor(out=sel, in0=sel, in1=p01, op=ALU.mult),
                 reads=["sel", "cf"], writes=["sel"])
            P.op(DVE, lambda e: e.tensor_tensor(out=sel, in0=sel, in1=o01, op=ALU.add),
                 reads=["sel", "cf"], writes=["sel"])
            P.op(DVE, lambda e: e.tensor_scalar(out=sel, in0=sel, scalar1=-1.0, scalar2=-NEG, op0=ALU.add, op1=ALU.mult),
                 reads=["sel"], writes=["sel"])
            if stop == "b":
                continue
            for i in range(NCH):
                P.op(PE, lambda e, i=i: e.transpose(self.bank(4 + i // 4, 128, (i % 4) * 128)[0:8, :],
                                                   sel[:, i, :], self.ident_f),
                     reads=["sel", "cf"], writes=["pb%d" % (4 + i // 4)])
            P.op(ACT, lambda e: e.activation(out=self.ROWM[0:8, :], in_=self.banks4(1)[0:8, :], func=AF.Copy),
                 reads=self.pk(1), writes=["ROWM"])
            P.op(DVE, lambda e: e.tensor_copy(out=Eh[0:64], in_=self.EM[0:64]), reads=["EM"], writes=["Eh"])
            P.op(DVE, lambda e, slope=slope: e.tensor_scalar(out=Eh[32:34], in0=self.EM[32:34], scalar1=slope,
                                                            scalar2=None, op0=ALU.mult),
                 reads=["EM", "Eh"], writes=["Eh"])
            if stop == "c":
                continue
            self.attention(qT, kT, vt,
                           lambda j, hd=hd: self.cf[:, C_ALC + hd * 16 + j:C_ALC + hd * 16 + j + 1], "cf",
                           lambda j: Eh[0:64, j // 2, :], self.ROWM, 64, ["Eh", "ROWM"], Pb, osb, 1024 + hd * 128)
        P.barrier()
        A.off = m0

    def mix_odd(self, w_in):
        P, A = self.P, self.A
        xk = lambda k: "xn%d" % k
        m0 = A.off
        lbt = self.lbt
        P.op(ACT, lambda e: e.activation(out=lbt[:, 0:16], in_=self.prm[:, P_LB:P_LB + 16], func=AF.Exp),
             reads=["prm"], writes=["lbt"])
        P.op(DVE, lambda e: e.tensor_tensor(out=lbt[:, 0:8], in0=lbt[:, 0:8], in1=lbt[:, 8:16], op=ALU.add),
             reads=["lbt"], writes=["lbt"])
        P.op(DVE, lambda e: e.reciprocal(out=lbt[:, 0:8], in_=lbt[:, 0:8]), reads=["lbt"], writes=["lbt"])
        P.op(DVE, lambda e: e.tensor_tensor(out=lbt[:, 8:16], in0=lbt[:, 8:16], in1=lbt[:, 0:8], op=ALU.mult),
             reads=["lbt"], writes=["lbt"])
        P.op(DVE, lambda e: e.tensor_scalar(out=lbt[:, 0:8], in0=lbt[:, 8:16], scalar1=-1.0, scalar2=1.0,
                                            op0=ALU.mult, op1=ALU.add),
             reads=["lbt"], writes=["lbt"])
        lf = self.HS[0]
        cs = self.RSTD
        cstm = A.alloc([NCH, 8])
        tmp = A.alloc([S])
        nfb = lbt[:, 24:25]
        P.op(DVE, lambda e: e.tensor_scalar(out=nfb[0:8], in0=self.prm[0:8, P_FB:P_FB + 1], scalar1=-1.0, scalar2=None,
                                            op0=ALU.mult), reads=["prm"], writes=["nfb"])

        def ev_f(mi, h):
            P.op(ACT, lambda e, h=h: e.activation(out=lf[0:8, :], in_=self.banks4(h)[0:8, :], func=AF.Exp, scale=-1.0,
                                                  bias=nfb[0:8]), reads=self.pk(h) + ["nfb"], writes=["lf"])
        self.gemm_fm(w_in, 0, 3072, [8], self.xn_rhs, xk, ev_f)
        P.op(ACT, lambda e: e.activation(out=lf[0:8, :], in_=lf[0:8, :], func=AF.Ln, bias=1.0), reads=["lf"], writes=["lf"])
        P.op(DVE, lambda e: e.tensor_tensor_scan(out=cs[0:8, :], data0=self.cbf[0:8, 384:385].to_broadcast([8, S]),
                                                 data1=lf[0:8, :], initial=0.0, op0=ALU.mult, op1=ALU.add),
             reads=["cbf", "lf"], writes=["RSTD"])
        P.op(DVE, lambda e: e.tensor_copy(out=self.ROWF[0:8, :], in_=cs[0:8, :]), reads=["RSTD"], writes=["ROWF"])
        P.op(DVE, lambda e: e.tensor_tensor(out=tmp[0:8, :], in0=cs[0:8, :], in1=self.ROWF[0:8, :], op=ALU.subtract),
             reads=["RSTD", "ROWF"], writes=["tmp"])
        P.op(SP, lambda e: e.dma_start(out=tmp[32:40, :], in_=tmp[0:8, :]), reads=["tmp"], writes=["tmp32"], dma=True)
        P.op(DVE, lambda e: e.tensor_copy(out=self.ROWF[32:40, :], in_=tmp[32:40, :]), reads=["tmp32"], writes=["ROWF"])
        P.op(DVE, lambda e: e.tensor_tensor(out=tmp[32:40, :], in0=tmp[32:40, :], in1=self.ROWF[32:40, :],
                                            op=ALU.subtract), reads=["tmp32", "ROWF"], writes=["tmp32"])
        P.op(SP, lambda e: e.dma_start(out=tmp[64:72, :], in_=tmp[32:40, :]), reads=["tmp32"], writes=["tmp64"], dma=True)
        P.op(DVE, lambda e: e.tensor_copy(out=self.ROWF[64:72, :], in_=tmp[64:72, :]), reads=["tmp64"], writes=["ROWF"])
        for i in range(NCH):
            P.op(PE, lambda e, i=i: e.transpose(self.bank(0, 8, i * 8), cs[0:8, i * 128:(i + 1) * 128],
                                               self.ident_f[0:8, 0:8]),
                 reads=["RSTD", "cf"], writes=["pb0"])
        P.op(DVE, lambda e: e.tensor_copy(out=cstm, in_=self.bank(0, 128).rearrange("p (a b) -> p a b", a=NCH)),
             reads=["pb0"], writes=["bcol"])
        qT, kT = A.alloc([S], BF16), A.alloc([S], BF16)
        vt = A.alloc([NCH, 128], BF16)
        Pb = [A.alloc([512], BF16) for _ in range(3)]
        osb = [A.alloc([512]) for _ in range(2)]
        for hd in range(8):
            def ev_q(mi, h):
                P.op(DVE, lambda e, h=h: e.tensor_scalar(out=qT, in0=self.banks4(h), scalar1=128 ** -0.5, scalar2=None,
                                                         op0=ALU.mult),
                     reads=self.pk(h), writes=["qT"])

            def ev_k(mi, h):
                P.op(ACT, lambda e, h=h: e.activation(out=kT, in_=self.banks4(h), func=AF.Copy),
                     reads=self.pk(h), writes=["kT"])
            self.gemm_fm(w_in, 0, hd * 128, [128], self.xn_rhs, xk, ev_q)
            self.gemm_fm(w_in, 0, 1024 + hd * 128, [128], self.xn_rhs, xk, ev_k)
            self.gemm_tm(w_in, 2048 + hd * 128, 128, vt, "vtm")
            self.attention(qT, kT, vt, lambda j, hd=hd: cstm[:, j, hd:hd + 1], "bcol",
                           lambda j, hd=hd: self.EF[:, hd, :], self.ROWF, 128, ["EF", "ROWF"], Pb, osb, hd * 128)
        P.barrier()
        A.off = m0
        F1, eb, enb, ebl = A.alloc([S]), A.alloc([S]), A.alloc([S]), A.alloc([S])
        qg, kg = A.alloc([S], BF16), A.alloc([S], BF16)
        kgl = self.SQB[:, 0, :]
        c3 = lambda a: a.rearrange("p (c j) -> p c j", j=64)
        kgtm = A.alloc([NCH, 128], BF16)
        vtm = A.alloc([NCH, 128], BF16)
        sg = A.alloc([1, S], BF16)
        oT = A.alloc([1, S])
        St = A.alloc([128])
        Sbf = [A.alloc([128], BF16) for _ in range(2)]
        PT = [A.alloc([128], BF16) for _ in range(2)]
        for hd in range(8):
            lb = lbt[:, 8 + hd:9 + hd]
            oml = lbt[:, hd:hd + 1]

            def ev_f(mi, h, oml=oml, lb=lb):
                P.op(ACT, lambda e, h=h: e.activation(out=F1, in_=self.banks4(h), func=AF.Sigmoid),
                     reads=self.pk(h), writes=["F1"])
                P.op(DVE, lambda e, oml=oml, lb=lb: e.tensor_scalar(out=F1, in0=F1, scalar1=oml, scalar2=lb,
                                                                   op0=ALU.mult, op1=ALU.add),
                     reads=["F1", "lbt"], writes=["F1"])
                P.op(ACT, lambda e: e.activation(out=eb, in_=F1, func=AF.Ln), reads=["F1"], writes=["eb"])
                P.op(DVE, lambda e: e.tensor_tensor_scan(out=cs, data0=self.RM, data1=eb, initial=0.0,
                                                         op0=ALU.mult, op1=ALU.add),
                     reads=["RM", "eb"], writes=["RSTD"])
                P.op(ACT, lambda e: e.activation(out=enb, in_=cs, func=AF.Exp, scale=-1.0), reads=["RSTD"], writes=["enb"])
                P.op(ACT, lambda e: e.activation(out=eb, in_=cs, func=AF.Exp), reads=["RSTD"], writes=["eb"])
                P.op(DVE, lambda e: e.tensor_scalar(out=F1, in0=F1, scalar1=-1.0, scalar2=1.0, op0=ALU.mult, op1=ALU.add),
                     reads=["F1"], writes=["F1"])
                P.op(DVE, lambda e: e.tensor_tensor(out=kg, in0=F1, in1=enb, op=ALU.mult),
                     reads=["F1", "enb"], writes=["kg"])
                P.op(DVE, lambda e: e.tensor_tensor(out=c3(ebl), in0=c3(enb),
                                                    in1=c3(eb)[:, :, 63:64].to_broadcast([128, 32, 64]), op=ALU.mult),
                     reads=["enb", "eb"], writes=["ebl"])
                P.op(DVE, lambda e: e.tensor_tensor(out=kgl, in0=F1, in1=ebl, op=ALU.mult),
                     reads=["F1", "ebl"], writes=["SQB0"])

            def ev_q(mi, h):
                P.op(ACT, lambda e, h=h: e.activation(out=F1, in_=self.banks4(h), func=AF.Silu),
                     reads=self.pk(h), writes=["F1"])
                P.op(DVE, lambda e: e.tensor_tensor(out=qg, in0=F1, in1=eb, op=ALU.mult),
                     reads=["F1", "eb"], writes=["qg"])

            def ev_g(mi, h):
                P.op(ACT, lambda e, h=h: e.activation(out=sg[:, 0, :], in_=self.banks4(h), func=AF.Silu),
                     reads=self.pk(h), writes=["sg"])
            self.gemm_fm(w_in, 0, 4104 + hd * 128, [128], self.xn_rhs, xk, ev_f)
            self.gemm_fm(w_in, 0, 3080 + hd * 128, [128], self.xn_rhs, xk, ev_q)
            self.gemm_fm(w_in, 0, 6152 + hd * 128, [128], self.xn_rhs, xk, ev_g)
            self.gemm_tm(w_in, 5128 + hd * 128, 128, vtm, "vtm")
            self.transpose_kg(kgl, kgtm)
            self.gla_core(qg, kg, kgtm, vtm, eb, 1, oT, St, Sbf, PT)
            self.gated_out(oT, sg, 1, P_HN, 1024 + hd * 128)
        P.barrier()
        A.off = m0


def _consts():
    c = np.zeros((128, NCST), np.float32)
    p = np.arange(128)
    c[:, C_ID:C_ID + 128] = np.eye(128, dtype=np.float32)
    s, t = p[:, None], p[None, :]
    c[:, C_UB:C_UB + 128] = ((s <= t) & (s // 64 == t // 64)).astype(np.float32)
    c[:, C_CM:C_CM + 128] = np.where(s <= t, 0.0, NEG).astype(np.float32)
    c[:, C_ONE:C_ONE + 128] = 1.0
    for h in range(8):
        for j in range(16):
            c[:, C_ALC + h * 16 + j] = (2.0 ** (-(h + 1))) * (128 * j + p)
    for i in range(16):
        for n in range(8):
            c[:, C_PM + i * 8 + n] = 0.0 if n < i // 2 else -1e30
            c[:, C_P01 + i * 8 + n] = 1.0 if n < i // 2 else 0.0
            c[:, C_O01 + i * 8 + n] = 1.0 if n == i // 2 else 0.0
    for n in range(8):
        c[n, C_EM + n * 128:C_EM + (n + 1) * 128] = 1.0
        c[32:34, C_EM + n * 128:C_EM + (n + 1) * 128] = 1.0
        for r in (n, 32 + n, 64 + n):
            c[r, C_EF + n * 128:C_EF + (n + 1) * 128] = -1.0
    tt = np.arange(S)
    c[32, C_AR:C_AR + S] = -128.0 * (tt // 128)
    c[33, C_AR:C_AR + S] = -(tt % 128).astype(np.float32)
    c[:, C_RM:C_RM + S] = (tt % 64 != 0).astype(np.float32)[None, :]
    return c


def _params(inp):
    pr = np.zeros((128, NPAR), np.float32)
    fm = lambda v: np.asarray(v, np.float32).reshape(-1, 128).T
    pr[:, 0:16] = fm(inp["ev_norm_mix"][0])
    pr[:, 16:32] = fm(inp["ev_norm_mlp"][0])
    pr[:, 32:48] = fm(inp["od_norm_mix"][0])
    pr[:, 48:64] = fm(inp["od_norm_mlp"][0])
    pr[:, 64:80] = fm(inp["final_norm"])
    pr[:, P_GB:P_GB + 4] = fm(inp["ev_gla_gate_b"][0])
    pr[:, P_GN:P_GN + 2] = fm(inp["ev_gla_out_norm"][0])
    pr[:, P_HN:P_HN + 1] = fm(inp["od_hgrn_out_norm"][0])
    pr[:, P_LB:P_LB + 8] = fm(inp["hgrn_lb_raw"][0])
    pr[:, P_LB + 8:P_LB + 16] = fm(inp["hgrn_lb_raw"][1])
    pr[0:8, P_FB] = np.asarray(inp["od_fox_fgate_b"][0], np.float32)
    return pr


_NC_CACHE = {}


def make_in_maps(inp, ncores=8):
    cst = _consts()
    prm = _params(inp)
    shared = {"w2": np.ascontiguousarray(np.asarray(inp["ev_gla_gate_w2"][0], np.float32)), "prm": prm, "cst": cst}
    for p in ("ev", "od"):
        for w in ("w_in", "w_out", "w_up", "w_down"):
            shared["%s_%s" % (p, w)] = np.ascontiguousarray(np.asarray(inp["%s_%s" % (p, w)][0], np.float32))
    x = np.asarray(inp["x"], np.float32)
    maps = []
    for b in range(ncores):
        m = dict(shared)
        m["xT"] = np.ascontiguousarray(x[b].T)
        maps.append(m)
    return maps


def kernel(**inputs):
    if "nc" not in _NC_CACHE:
        _NC_CACHE["nc"] = Builder().run()
    nc = _NC_CACHE["nc"]
    maps = make_in_maps(inputs)
    res = run_bass_kernel_spmd(nc, maps, core_ids=list(range(8)))
    out = np.stack([np.asarray(r["outT"]).T for r in res.results], axis=0)
    return np.ascontiguousarray(out.astype(np.float32))
```

```python
import numpy as np
from contextlib import ExitStack
import concourse.bass as bass
import concourse.mybir as mybir
from concourse.bass_utils import run_bass_kernel_spmd

F32 = mybir.dt.float32
BF16 = mybir.dt.bfloat16
AF = mybir.ActivationFunctionType
ALU = mybir.AluOpType
AX = mybir.AxisListType

S = 2048
D = 2048
DFF = 8192
NCH = 16
EPS = 1e-6
NIN = (6160, 7176)
NEG = -30000.0

PE, ACT, DVE, POOL, SP = "pe", "act", "dve", "pool", "sp"
ENGS = [PE, ACT, DVE, POOL, SP]
NDMA_SEM = 8

C_ID, C_UB, C_CM, C_ONE, C_ALC, C_PM, C_P01, C_O01 = 0, 128, 256, 384, 512, 640, 768, 896
C_EM, C_EF, C_AR, C_RM, NCST = 1024, 2048, 3072, 5120, 7168
P_NORM = 0
P_GB = 80
P_GN = 84
P_HN = 86
P_LB = 87
P_FB = 103
NPAR = 104


class Op:
    __slots__ = ("eng", "fn", "deps", "dma", "idx", "signal", "cnt", "dsem", "dval", "prev")

    def __init__(self, eng, fn, dma):
        self.eng, self.fn, self.dma = eng, fn, dma
        self.deps = set()
        self.signal = False
        self.cnt = 0
        self.dsem = None
        self.dval = 0
        self.prev = None


class Prog:
    def __init__(self, nc):
        self.nc = nc
        self.ops = []
        self.lastw = {}
        self.readers = {}
        self.last_eng = {}
        self.dmas_since = []

    def op(self, eng, fn, reads=(), writes=(), dma=False):
        o = Op(eng, fn, dma)
        o.idx = len(self.ops)
        pr = [r for r in reads if r.startswith("pb")]
        if pr:
            reads = [r for r in reads if not r.startswith("pb")]
            writes = list(writes) + pr
        for r in reads:
            w = self.lastw.get(r)
            if w is not None:
                o.deps.add(w)
        for r in writes:
            w = self.lastw.get(r)
            if w is not None:
                o.deps.add(w)
            for rd in self.readers.get(r, ()):
                o.deps.add(rd)
        for r in writes:
            self.lastw[r] = o.idx
            self.readers[r] = []
        for r in reads:
            if r not in writes:
                self.readers.setdefault(r, []).append(o.idx)
        o.deps.discard(o.idx)
        self.ops.append(o)
        if dma:
            self.dmas_since.append(o.idx)
        else:
            self.last_eng[eng] = o.idx
        return o

    def barrier(self):
        deps = set(self.last_eng.values()) | set(self.dmas_since)
        for e in ENGS:
            o = Op(e, None, False)
            o.idx = len(self.ops)
            o.deps = set(deps)
            self.ops.append(o)
        self.lastw.clear()
        self.readers.clear()
        self.dmas_since = []

    def emit(self, stack):
        nc = self.nc
        ops = self.ops
        for o in ops:
            best = {}
            nd = set()
            for d in o.deps:
                p = ops[d]
                if p.dma:
                    nd.add(d)
                    continue
                if p.eng == o.eng and o.eng == PE and not o.dma:
                    continue
                if p.eng not in best or best[p.eng] < d:
                    best[p.eng] = d
            nd |= set(best.values())
            o.deps = nd
            for d in nd:
                ops[d].signal = True
        sems = {e: stack.enter_context(nc.semaphore("s_" + e)) for e in ENGS}
        dsems = {e: [stack.enter_context(nc.semaphore("d_%s_%d" % (e, i))) for i in range(NDMA_SEM)]
                 for e in (ACT, POOL, SP)}
        cnt = {e: 0 for e in ENGS}
        per = {e: [] for e in ENGS}
        hist = {e: [] for e in ENGS}
        for o in ops:
            if o.dma:
                j = len(hist[o.eng])
                o.dsem = dsems[o.eng][j % NDMA_SEM]
                o.dval = 16 * (j // NDMA_SEM + 1)
                if j >= NDMA_SEM:
                    o.prev = hist[o.eng][j - NDMA_SEM]
                hist[o.eng].append(o)
            elif o.signal:
                cnt[o.eng] += 1
                o.cnt = cnt[o.eng]
            per[o.eng].append(o)
        self.stats = {e: len(per[e]) for e in ENGS}
        self.stats["sig"] = dict(cnt)
        block = stack.enter_context(nc.Block())

        def run(eng_name, e):
            waited = {}

            def wait(sem, val):
                k = id(sem)
                if waited.get(k, 0) >= val:
                    return
                waited[k] = val
                e.wait_ge(sem, val)

            for o in per[eng_name]:
                need = {}
                for d in o.deps:
                    p = ops[d]
                    s, v = (p.dsem, p.dval) if p.dma else (sems[p.eng], p.cnt)
                    if id(s) not in need or need[id(s)][1] < v:
                        need[id(s)] = (s, v)
                if o.dma and o.prev is not None:
                    s, v = o.prev.dsem, o.prev.dval
                    if id(s) not in need or need[id(s)][1] < v:
                        need[id(s)] = (s, v)
                for s, v in need.values():
                    wait(s, v)
                if o.fn is None:
                    continue
                ins = o.fn(e)
                if o.dma:
                    ins.then_inc(o.dsem, 16)
                elif o.signal:
                    ins.then_inc(sems[o.eng], 1)
            for o in hist[eng_name][-NDMA_SEM:]:
                wait(o.dsem, o.dval)

        block.tensor(lambda e: run(PE, e))
        block.scalar(lambda e: run(ACT, e))
        block.vector(lambda e: run(DVE, e))
        block.gpsimd(lambda e: run(POOL, e))
        block.sync(lambda e: run(SP, e))


class Arena:
    def __init__(self, ap, nwords):
        self.ap = ap
        self.n = nwords
        self.off = 0

    def alloc(self, shape, dtype=F32, parts=128):
        n = int(np.prod(shape))
        words = n if dtype == F32 else (n + 1) // 2
        words = (words + 7) // 8 * 8
        assert self.off + words <= self.n, ("arena overflow", self.off, words, self.n)
        v = self.ap[0:parts, self.off:self.off + words]
        self.off += words
        if dtype != F32:
            v = v.bitcast(dtype)
        v = v[:, 0:n]
        if len(shape) == 2:
            v = v.rearrange("p (a b) -> p a b", a=shape[0])
        elif len(shape) == 3:
            v = v.rearrange("p (a b c) -> p a b c", a=shape[0], b=shape[1])
        return v


class Builder:
    def __init__(self, dbg=None):
        self.dbg = dbg
        nc = self.nc = bass.Bass("TRN2", target_bir_lowering=False)
        self.P = Prog(nc)

        def di(name, shape, dt=F32):
            return nc.dram_tensor(name, shape, dt, kind="ExternalInput").ap()

        self.xT = di("xT", [D, S])
        self.W = []
        for l, p in ((0, "ev"), (1, "od")):
            self.W.append(dict(w_in=di(p + "_w_in", [D, NIN[l]]), w_out=di(p + "_w_out", [D, D]),
                               w_up=di(p + "_w_up", [D, DFF]), w_down=di(p + "_w_down", [DFF, D])))
        self.w2d = di("w2", [16, 512])
        self.prmd = di("prm", [128, NPAR])
        self.cstd = di("cst", [128, NCST])
        self.outT = nc.dram_tensor("outT", [D, S], F32, kind="ExternalOutput").ap()
        self.hT = nc.dram_tensor("hT", [D, S], F32).ap()
        self.mixd = nc.dram_tensor("mixd", [D, S], BF16, kind=("ExternalOutput" if dbg in ("mix", "s2", "s3", "s4", "s5", "s6") else "Internal")).ap()
        if dbg == "h":
            self.hdbg = nc.dram_tensor("hdbg", [D, S], F32, kind="ExternalOutput").ap()

    def bank(self, b, n=512, c0=0):
        return self.PS[:, 512 * b + c0:512 * b + c0 + n]

    def banks4(self, h):
        return self.PS[:, 2048 * h:2048 * h + 2048]

    def pk(self, h):
        return ["pb%d" % b for b in range(4 * h, 4 * h + 4)]

    def run(self):
        nc, P = self.nc, self.P
        with ExitStack() as st:
            NW = 53000
            arena_t = st.enter_context(nc.sbuf_tensor("arena", [128, NW], F32))
            self.PS = st.enter_context(nc.psum_tensor("ps", [128, 4096], F32))
            A = self.A = Arena(arena_t, NW)
            self.XN = A.alloc([NCH, S], BF16)
            self.WB = [A.alloc([NCH, 256], BF16) for _ in range(2)]
            self.HS = [A.alloc([S], F32) for _ in range(2)]
            self.RSTD = A.alloc([S], F32)
            self.SQB = A.alloc([2, S], BF16)
            self.cbf = A.alloc([512], BF16)
            self.EM = A.alloc([8, 128], BF16)
            self.EF = A.alloc([8, 128], BF16)
            self.ROWM = A.alloc([S], BF16)
            self.ROWF = A.alloc([S], BF16)
            self.RM = A.alloc([S], BF16)
            self.cf = A.alloc([1024], F32)
            self.prm = A.alloc([NPAR], F32)
            self.w2 = A.alloc([512], F32)
            self.lbt = A.alloc([32], F32)
            self.mark = A.off
            self.wslot = 0
            self.ident_bf = self.cbf[:, 0:128]
            self.ublk_bf = self.cbf[:, 128:256]
            self.cm_bf = self.cbf[:, 256:384]
            self.ones_bf = self.cbf[:, 384:512]
            self.ident_f = self.cf[:, C_ID:C_ID + 128]
            self.ublk_f = self.cf[:, C_UB:C_UB + 128]

            self.load_consts()
            self.copy_x()
            if self.dbg != "s1":
                self.layer(0)
            if self.dbg not in ("l0", "mix", "s1", "s2", "s3", "s4"):
                self.layer(1)
            self.final_norm()
            P.emit(st)
        return nc

    def load_consts(self):
        P, c = self.P, self.cstd
        P.op(POOL, lambda e: e.dma_start(out=self.cbf, in_=c[:, 0:512]), writes=["cbf"], dma=True)
        P.op(POOL, lambda e: e.dma_start(out=self.EM.rearrange("p a b -> p (a b)"), in_=c[:, C_EM:C_EM + 1024]),
             writes=["EM"], dma=True)
        P.op(POOL, lambda e: e.dma_start(out=self.EF.rearrange("p a b -> p (a b)"), in_=c[:, C_EF:C_EF + 1024]),
             writes=["EF"], dma=True)
        P.op(POOL, lambda e: e.dma_start(out=self.ROWM, in_=c[:, C_AR:C_AR + S]), writes=["ROWM"], dma=True)
        P.op(POOL, lambda e: e.dma_start(out=self.RM, in_=c[:, C_RM:C_RM + S]), writes=["RM"], dma=True)
        P.op(SP, lambda e: e.dma_start(out=self.cf, in_=c[:, 0:1024]), writes=["cf"], dma=True)
        P.op(SP, lambda e: e.dma_start(out=self.prm, in_=self.prmd), writes=["prm"], dma=True)
        P.op(SP, lambda e: e.dma_start(out=self.w2[0:16, :], in_=self.w2d), writes=["w2"], dma=True)
        P.op(DVE, lambda e: e.memset(self.ROWF, 0.0), writes=["ROWF"])

    def copy_x(self):
        for c in range(NCH):
            self.P.op(SP, lambda e, c=c: e.dma_start(out=self.hT[c * 128:(c + 1) * 128, :],
                                                      in_=self.xT[c * 128:(c + 1) * 128, :]),
                      writes=["hT%d" % c], dma=True)

    def norm(self, gcol, final=False):
        P = self.P
        for c in range(NCH):
            hs, hk = self.HS[c % 2], "HS%d" % (c % 2)
            sq, sk = self.SQB[:, c % 2, :], "SQB%d" % (c % 2)
            P.op(SP, lambda e, c=c, hs=hs: e.dma_start(out=hs, in_=self.hT[c * 128:(c + 1) * 128, :]),
                 reads=["hT%d" % c], writes=[hk], dma=True)
            P.op(ACT, lambda e, hs=hs, sq=sq: e.activation(out=sq, in_=hs, func=AF.Square),
                 reads=[hk], writes=[sk])
            for n in range(4):
                P.op(PE, lambda e, c=c, n=n, sq=sq: e.matmul(self.bank(n), lhsT=self.ones_bf,
                                                             rhs=sq[:, n * 512:(n + 1) * 512],
                                                             start=(c == 0), stop=(c == NCH - 1)),
                     reads=[sk, "cbf"], writes=["pb%d" % n])
        P.op(ACT, lambda e: e.activation(out=self.RSTD, in_=self.banks4(0), func=AF.Ln, scale=1.0 / D, bias=EPS),
             reads=self.pk(0), writes=["RSTD"])
        P.op(ACT, lambda e: e.activation(out=self.RSTD, in_=self.RSTD, func=AF.Exp, scale=-0.5),
             reads=["RSTD"], writes=["RSTD"])
        for c in range(NCH):
            hs, hk = self.HS[c % 2], "HS%d" % (c % 2)
            P.op(SP, lambda e, c=c, hs=hs: e.dma_start(out=hs, in_=self.hT[c * 128:(c + 1) * 128, :]),
                 reads=["hT%d" % c], writes=[hk], dma=True)
            g = self.prm[:, gcol + c:gcol + c + 1]
            if not final:
                P.op(DVE, lambda e, c=c, hs=hs, g=g: e.scalar_tensor_tensor(
                    out=self.XN[:, c, :], in0=hs, scalar=g, in1=self.RSTD, op0=ALU.mult, op1=ALU.mult),
                    reads=[hk, "RSTD", "prm"], writes=["xn%d" % c])
            else:
                P.op(DVE, lambda e, c=c, hs=hs, g=g: e.scalar_tensor_tensor(
                    out=hs, in0=hs, scalar=g, in1=self.RSTD, op0=ALU.mult, op1=ALU.mult),
                    reads=[hk, "RSTD", "prm"], writes=[hk])
                P.op(SP, lambda e, c=c, hs=hs: e.dma_start(out=self.outT[c * 128:(c + 1) * 128, :], in_=hs),
                     reads=[hk], writes=["out%d" % c], dma=True)

    def final_norm(self):
        if self.dbg == "h":
            for c in range(NCH):
                self.P.op(SP, lambda e, c=c: e.dma_start(out=self.hdbg[c * 128:(c + 1) * 128, :],
                                                          in_=self.hT[c * 128:(c + 1) * 128, :]),
                          reads=["hT%d" % c], writes=["hd%d" % c], dma=True)
        self.norm(P_NORM + 64, final=True)

    def load_w(self, wd, r0, nk, c0, ncols):
        s = self.wslot % 2
        self.wslot += 1
        wb, key = self.WB[s], "wb%d" % s
        self.P.op(POOL, lambda e: e.dma_start(
            out=wb[:, 0:nk, 0:ncols],
            in_=wd[r0:r0 + nk * 128, c0:c0 + ncols].rearrange("(c p) n -> p c n", p=128)),
            writes=[key], dma=True)
        return wb, key

    def gemm_fm(self, wd, r0, c0, widths, rhs, rkey, evac, nk=NCH, nbank=4):
        P = self.P
        mi = 0
        i = 0
        while i < len(widths):
            grp = [widths[i]]
            if i + 1 < len(widths) and widths[i] + widths[i + 1] <= 256:
                grp.append(widths[i + 1])
            tot = sum(grp)
            wb, wkey = self.load_w(wd, r0, nk, c0, tot)
            off = 0
            for mw in grp:
                h = self.gpar % 2
                self.gpar += 1
                for k in range(nk):
                    for n in range(nbank):
                        P.op(PE, lambda e, k=k, n=n, h=h, off=off, mw=mw, wb=wb: e.matmul(
                            self.bank(4 * h + n)[0:mw, :], lhsT=wb[:, k, off:off + mw], rhs=rhs(k, n),
                            start=(k == 0), stop=(k == nk - 1)),
                            reads=[wkey, rkey(k)], writes=["pb%d" % (4 * h + n)])
                evac(mi, h)
                mi += 1
                off += mw
            c0 += tot
            i += len(grp)

    def xn_rhs(self, k, n):
        return self.XN[:, k, n * 512:(n + 1) * 512]

    def gemm_tm(self, wd, c0, ncols, vtm, vkey):
        P = self.P
        vT = self.SQB[:, 1, :]

        def ev(mi, h):
            P.op(ACT, lambda e, h=h: e.activation(out=vT, in_=self.banks4(h), func=AF.Copy),
                 reads=self.pk(h), writes=["SQB1"])
            for hb in range(2):
                b = 4 * h + 2 * (self.tmb % 2) + hb
                pbv = self.bank(b).bitcast(BF16)
                for j in range(8):
                    t = hb * 8 + j
                    P.op(PE, lambda e, t=t, j=j, pbv=pbv: e.transpose(pbv[:, j * 128:(j + 1) * 128],
                                                                      vT[:, t * 128:(t + 1) * 128], self.ident_bf),
                         reads=["SQB1", "cbf"], writes=["pb%d" % b])
                P.op(DVE, lambda e, hb=hb, pbv=pbv, mi=mi: e.tensor_copy(
                    out=vtm[:, hb * 8:hb * 8 + 8, mi * 128:(mi + 1) * 128],
                    in_=pbv.rearrange("p (a c) -> p a c", a=8)),
                    reads=["pb%d" % b], writes=[vkey])
            self.tmb += 1
        self.gemm_fm(wd, 0, c0, [128] * (ncols // 128), self.xn_rhs, lambda k: "xn%d" % k, ev)

    def gla_core(self, qg, kg, kgtm, vtm, eb, dvh, oT, St, Sbf, PT):
        P = self.P
        dv = dvh * 128
        P.op(DVE, lambda e: e.memset(St, 0.0), writes=["St"])
        P.op(DVE, lambda e: e.memset(Sbf[0], 0.0), writes=["Sbf0"])
        sidx = 0
        for i in range(NCH):
            tl = slice(i * 128, (i + 1) * 128)
            g = i // 4
            pt, ptk = PT[i % 2], "PT%d" % (i % 2)
            P.op(PE, lambda e, tl=tl: e.matmul(self.bank(4, 128), lhsT=kg[:, tl], rhs=qg[:, tl], start=True, stop=True),
                 reads=["kg", "qg"], writes=["pb4"])
            P.op(DVE, lambda e, pt=pt: e.tensor_tensor(out=pt, in0=self.bank(4, 128), in1=self.ublk_f, op=ALU.mult),
                 reads=["pb4", "cf"], writes=[ptk])
            for cc in range(2):
                rs = slice(cc * 64, cc * 64 + 64)
                P.op(PE, lambda e, i=i, cc=cc, rs=rs: e.matmul(
                    self.bank(5 + cc, dv), lhsT=kgtm[rs, i, :], rhs=vtm[rs, i, :], start=True, stop=True),
                    reads=["kgtm", "vtm"], writes=["pb%d" % (5 + cc)])
            obanks = [(2 * (g % 2) + hh) for hh in range(dvh)]
            col = (i % 4) * 128
            for hh in range(dvh):
                ob = obanks[hh]
                P.op(PE, lambda e, i=i, hh=hh, ob=ob, col=col, pt=pt: e.matmul(
                    self.bank(ob, 128, col), lhsT=vtm[:, i, hh * 128:(hh + 1) * 128], rhs=pt, start=True, stop=False),
                    reads=["vtm", ptk], writes=["pb%d" % ob])
            for cc in range(2):
                c = 2 * i + cc
                cur, nxt = Sbf[sidx % 2], Sbf[(sidx + 1) % 2]
                ck, nk_ = "Sbf%d" % (sidx % 2), "Sbf%d" % ((sidx + 1) % 2)
                qs = slice(i * 128 + cc * 64, i * 128 + cc * 64 + 64)
                for hh in range(dvh):
                    ob = obanks[hh]
                    P.op(PE, lambda e, hh=hh, ob=ob, col=col, cc=cc, cur=cur, qs=qs: e.matmul(
                        self.bank(ob, 64, col + cc * 64), lhsT=cur[:, hh * 128:(hh + 1) * 128], rhs=qg[:, qs],
                        start=False, stop=(cc == 1)),
                        reads=[ck, "qg"], writes=["pb%d" % ob])
                if c == 2 * NCH - 1:
                    break
                el = eb[:, c * 64 + 63:c * 64 + 64]
                P.op(DVE, lambda e, cc=cc, nxt=nxt, el=el: e.scalar_tensor_tensor(
                    out=nxt, in0=St, scalar=el, in1=self.bank(5 + cc, dv), op0=ALU.mult, op1=ALU.add),
                    reads=["pb%d" % (5 + cc), "St", "eb"], writes=[nk_])
                P.op(DVE, lambda e, cc=cc, el=el: e.scalar_tensor_tensor(
                    out=St, in0=St, scalar=el, in1=self.bank(5 + cc, dv), op0=ALU.mult, op1=ALU.add),
                    reads=["pb%d" % (5 + cc), "St", "eb"], writes=["St"])
                sidx += 1
            if i % 4 == 3:
                for hh in range(dvh):
                    ob = obanks[hh]
                    P.op(ACT, lambda e, hh=hh, ob=ob, g=g: e.activation(
                        out=oT[:, hh, g * 512:(g + 1) * 512], in_=self.bank(ob), func=AF.Copy),
                        reads=["pb%d" % ob], writes=["oT"])

    def transpose_kg(self, kg, kgtm):
        P = self.P
        for hb in range(2):
            b = 6 + hb
            pbv = self.bank(b).bitcast(BF16)
            for j in range(8):
                t = hb * 8 + j
                P.op(PE, lambda e, t=t, j=j, pbv=pbv: e.transpose(pbv[:, j * 128:(j + 1) * 128],
                                                                  kg[:, t * 128:(t + 1) * 128], self.ident_bf),
                     reads=["SQB0", "cbf"], writes=["pb%d" % b])
            P.op(DVE, lambda e, hb=hb, pbv=pbv: e.tensor_copy(
                out=kgtm[:, hb * 8:hb * 8 + 8, :], in_=pbv.rearrange("p (a c) -> p a c", a=8)),
                reads=["pb%d" % b], writes=["kgtm"])

    def gated_out(self, oT, sg, dvh, gain_col, row0):
        P = self.P
        dv = dvh * 128
        for hh in range(dvh):
            P.op(ACT, lambda e, hh=hh: e.activation(out=self.SQB[:, hh, :], in_=oT[:, hh, :], func=AF.Square),
                 reads=["oT"], writes=["SQB%d" % hh])
        for n in range(4):
            for hh in range(dvh):
                P.op(PE, lambda e, n=n, hh=hh: e.matmul(self.bank(4 + n), lhsT=self.ones_bf,
                                                        rhs=self.SQB[:, hh, n * 512:(n + 1) * 512],
                                                        start=(hh == 0), stop=(hh == dvh - 1)),
                     reads=["SQB%d" % hh, "cbf"], writes=["pb%d" % (4 + n)])
        P.op(ACT, lambda e: e.activation(out=self.RSTD, in_=self.banks4(1), func=AF.Ln, scale=1.0 / dv, bias=EPS),
             reads=self.pk(1), writes=["RSTD"])
        P.op(ACT, lambda e: e.activation(out=self.RSTD, in_=self.RSTD, func=AF.Exp, scale=-0.5),
             reads=["RSTD"], writes=["RSTD"])
        for hh in range(dvh):
            g = self.prm[:, gain_col + hh:gain_col + hh + 1]
            P.op(DVE, lambda e, hh=hh, g=g: e.scalar_tensor_tensor(
                out=oT[:, hh, :], in0=oT[:, hh, :], scalar=g, in1=self.RSTD, op0=ALU.mult, op1=ALU.mult),
                reads=["oT", "RSTD", "prm"], writes=["oT"])
            ms = self.HS[1].bitcast(BF16)[:, hh * S:(hh + 1) * S]
            P.op(DVE, lambda e, hh=hh, ms=ms: e.tensor_tensor(out=ms, in0=oT[:, hh, :], in1=sg[:, hh, :], op=ALU.mult),
                 reads=["oT", "sg"], writes=["ms%d" % hh])
            r = row0 + hh * 128
            P.op(SP, lambda e, ms=ms, r=r: e.dma_start(out=self.mixd[r:r + 128, :], in_=ms),
                 reads=["ms%d" % hh], writes=["mix%d" % (r // 128)], dma=True)

    def attention(self, qT, kT, vtm, bias_col, bias_key, row_lhsT, row_rhs, nrow, row_keys, Pb, osb, row0):
        P = self.P
        sidx = 0
        for n in range(4):
            ob, lb = (4, 5) if n % 2 == 0 else (6, 7)
            jmax = 4 * n + 3
            for j in range(jmax + 1):
                c0 = max(0, j - 4 * n) * 128
                N = 512 - c0
                sb = sidx % 4
                pb, pbk = Pb[sidx % 3], "Pb%d" % (sidx % 3)
                sidx += 1
                diag = j >= 4 * n
                tq = slice(512 * n + c0, 512 * (n + 1))
                P.op(PE, lambda e, j=j, sb=sb, c0=c0, N=N, tq=tq: e.matmul(
                    self.bank(sb, N, c0), lhsT=kT[:, j * 128:(j + 1) * 128], rhs=qT[:, tq], start=True, stop=False),
                    reads=["kT", "qT"], writes=["pb%d" % sb])
                P.op(PE, lambda e, j=j, sb=sb, c0=c0, N=N, tq=tq, diag=diag: e.matmul(
                    self.bank(sb, N, c0), lhsT=row_lhsT(j), rhs=row_rhs[0:nrow, tq], start=False, stop=(not diag)),
                    reads=row_keys, writes=["pb%d" % sb])
                if diag:
                    P.op(PE, lambda e, sb=sb, c0=c0: e.matmul(
                        self.bank(sb, 128, c0), lhsT=self.ident_bf, rhs=self.cm_bf, start=False, stop=True),
                        reads=["cbf"], writes=["pb%d" % sb])
                P.op(ACT, lambda e, j=j, sb=sb, c0=c0, N=N, pb=pb: e.activation(
                    out=pb[:, c0:512], in_=self.bank(sb, N, c0), func=AF.Exp, bias=bias_col(j)),
                    reads=["pb%d" % sb, bias_key], writes=[pbk])
                P.op(PE, lambda e, j=j, ob=ob, c0=c0, N=N, pb=pb, jmax=jmax: e.matmul(
                    self.bank(ob, N, c0), lhsT=vtm[:, j, :], rhs=pb[:, c0:512], start=(j == 0), stop=(j == jmax)),
                    reads=["vtm", pbk], writes=["pb%d" % ob])
                P.op(PE, lambda e, j=j, lb=lb, c0=c0, N=N, pb=pb, jmax=jmax: e.matmul(
                    self.bank(lb, N, c0), lhsT=self.ones_bf, rhs=pb[:, c0:512], start=(j == 0), stop=(j == jmax)),
                    reads=["cbf", pbk], writes=["pb%d" % lb])
            rl, rk = osb[n % 2], "osb%d" % (n % 2)
            P.op(DVE, lambda e, lb=lb, rl=rl: e.reciprocal(out=rl, in_=self.bank(lb)), reads=["pb%d" % lb], writes=[rk])
            ms = self.HS[1].bitcast(BF16)[:, n * 512:(n + 1) * 512]
            P.op(DVE, lambda e, ob=ob, rl=rl, ms=ms: e.tensor_tensor(out=ms, in0=self.bank(ob), in1=rl, op=ALU.mult),
                 reads=["pb%d" % ob, rk], writes=["ms%d" % n])
            P.op(SP, lambda e, n=n, ms=ms: e.dma_start(out=self.mixd[row0:row0 + 128, n * 512:(n + 1) * 512], in_=ms),
                 reads=["ms%d" % n], writes=["mix%d_%d" % (row0 // 128, n)], dma=True)

    def layer(self, l):
        P = self.P
        W = self.W[l]
        self.gpar = 0
        self.tmb = 0
        self.norm(P_NORM + 32 * l)
        P.barrier()
        if l == 0:
            self.mix_even(W["w_in"])
        else:
            self.mix_odd(W["w_in"])
        P.barrier()
        self.A.off = self.mark
        if self.dbg in ("s2", "s3", "s5", "s6"):
            return
        P.op(SP, lambda e: e.dma_start(out=self.XN[:, 0:8, :],
                                       in_=self.mixd[0:1024, :].rearrange("(c p) t -> p c t", p=128)),
             writes=["xn%d" % c for c in range(8)], dma=True)
        P.op(SP, lambda e: e.dma_start(out=self.XN[:, 8:16, :],
                                       in_=self.mixd[1024:2048, :].rearrange("(c p) t -> p c t", p=128)),
             writes=["xn%d" % c for c in range(8, 16)], dma=True)
        self.gemm_fm(W["w_out"], 0, 0, [128] * 16, self.xn_rhs, lambda k: "xn%d" % k, self.evac_resid(0))
        P.barrier()
        self.norm(P_NORM + 32 * l + 16)
        aT = self.A.alloc([NCH, S], BF16)
        for fg in range(4):
            def evac_up(mi, h, aT=aT):
                hs, hk = self.HS[mi % 2], "HS%d" % (mi % 2)
                P.op(ACT, lambda e, h=h, hs=hs: e.activation(out=hs, in_=self.banks4(h), func=AF.Relu),
                     reads=self.pk(h), writes=[hk])
                P.op(DVE, lambda e, mi=mi, hs=hs: e.tensor_tensor(out=aT[:, mi, :], in0=hs, in1=hs, op=ALU.mult),
                     reads=[hk], writes=["aT%d" % mi])
            self.gemm_fm(W["w_up"], 0, fg * 2048, [128] * 16, self.xn_rhs, lambda k: "xn%d" % k, evac_up)
            self.gemm_fm(W["w_down"], fg * 2048, 0, [128] * 16,
                         lambda k, n, aT=aT: aT[:, k, n * 512:(n + 1) * 512], lambda k: "aT%d" % k,
                         self.evac_resid(0))
        P.barrier()
        self.A.off = self.mark

    def evac_resid(self, _):
        P = self.P

        def pre(mi):
            hs, hk = self.HS[mi % 2], "HS%d" % (mi % 2)
            P.op(SP, lambda e, mi=mi, hs=hs: e.dma_start(out=hs, in_=self.hT[mi * 128:(mi + 1) * 128, :]),
                 reads=["hT%d" % mi], writes=[hk], dma=True)

        def ev(mi, h):
            hs, hk = self.HS[mi % 2], "HS%d" % (mi % 2)
            if mi < 2:
                pre(mi)
            P.op(DVE, lambda e, h=h, hs=hs: e.tensor_tensor(out=hs, in0=self.banks4(h), in1=hs, op=ALU.add),
                 reads=self.pk(h) + [hk], writes=[hk])
            P.op(SP, lambda e, mi=mi, hs=hs: e.dma_start(out=self.hT[mi * 128:(mi + 1) * 128, :], in_=hs),
                 reads=[hk], writes=["hT%d" % mi], dma=True)
            if mi + 2 < NCH:
                pre(mi + 2)
        return ev

    def mix_even(self, w_in):
        P, A = self.P, self.A
        xk = lambda k: "xn%d" % k
        glrT = self.HS[0]
        def ev_glr(mi, h):
            P.op(ACT, lambda e, h=h: e.activation(out=glrT[0:16, :], in_=self.banks4(h)[0:16, :], func=AF.Copy),
                 reads=self.pk(h), writes=["glr"])
        self.gemm_fm(w_in, 0, 3072, [16], self.xn_rhs, xk, ev_glr)
        m0 = A.off
        eb, enb, ebl = A.alloc([S]), A.alloc([S]), A.alloc([S])
        qg, kg = A.alloc([S], BF16), A.alloc([S], BF16)
        kgl = self.SQB[:, 0, :]
        c3 = lambda a: a.rearrange("p (c j) -> p c j", j=64)
        kgtm = A.alloc([NCH, 128], BF16)
        vtm = A.alloc([NCH, 256], BF16)
        sg = A.alloc([2, S], BF16)
        oT = A.alloc([2, S])
        St = A.alloc([256])
        Sbf = [A.alloc([256], BF16) for _ in range(2)]
        PT = [A.alloc([128], BF16) for _ in range(2)]
        cs = self.RSTD
        for hd in ([] if self.dbg == "s3" else [0] if self.dbg == "s2" else range(4)):
            for n in range(4):
                P.op(PE, lambda e, hd=hd, n=n: e.matmul(self.bank(n), lhsT=self.w2[0:16, hd * 128:(hd + 1) * 128],
                                                       rhs=glrT[0:16, n * 512:(n + 1) * 512], start=True, stop=True),
                     reads=["w2", "glr"], writes=["pb%d" % n])
            nb = self.lbt[:, 16 + hd:17 + hd]
            P.op(DVE, lambda e, hd=hd, nb=nb: e.tensor_scalar(out=nb, in0=self.prm[:, P_GB + hd:P_GB + hd + 1],
                                                             scalar1=-1.0, scalar2=None, op0=ALU.mult),
                 reads=["prm"], writes=["nb"])
            P.op(ACT, lambda e, nb=nb: e.activation(out=eb, in_=self.banks4(0), func=AF.Exp, scale=-1.0, bias=nb),
                 reads=self.pk(0) + ["nb"], writes=["eb"])
            P.op(ACT, lambda e: e.activation(out=eb, in_=eb, func=AF.Ln, bias=1.0), reads=["eb"], writes=["eb"])
            P.op(DVE, lambda e: e.tensor_tensor_scan(out=cs, data0=self.RM, data1=eb, initial=0.0,
                                                     op0=ALU.mult, op1=ALU.add),
                 reads=["RM", "eb"], writes=["RSTD"])
            P.op(ACT, lambda e: e.activation(out=eb, in_=cs, func=AF.Exp, scale=-1.0 / 16), reads=["RSTD"], writes=["eb"])
            P.op(ACT, lambda e: e.activation(out=enb, in_=cs, func=AF.Exp, scale=1.0 / 16), reads=["RSTD"], writes=["enb"])
            P.op(DVE, lambda e: e.tensor_tensor(out=c3(ebl), in0=c3(enb), in1=c3(eb)[:, :, 63:64].to_broadcast([128, 32, 64]),
                                                op=ALU.mult), reads=["enb", "eb"], writes=["ebl"])

            def ev_q(mi, h):
                P.op(DVE, lambda e, h=h: e.scalar_tensor_tensor(out=qg, in0=self.banks4(h), scalar=128 ** -0.5, in1=eb,
                                                                op0=ALU.mult, op1=ALU.mult),
                     reads=self.pk(h) + ["eb"], writes=["qg"])

            def ev_k(mi, h):
                P.op(DVE, lambda e, h=h: e.tensor_tensor(out=kg, in0=self.banks4(h), in1=enb, op=ALU.mult),
                     reads=self.pk(h) + ["enb"], writes=["kg"])
                P.op(DVE, lambda e, h=h: e.tensor_tensor(out=kgl, in0=self.banks4(h), in1=ebl, op=ALU.mult),
                     reads=self.pk(h) + ["ebl"], writes=["SQB0"])

            def ev_g(mi, h):
                P.op(ACT, lambda e, mi=mi, h=h: e.activation(out=sg[:, mi, :], in_=self.banks4(h), func=AF.Silu),
                     reads=self.pk(h), writes=["sg"])
            self.gemm_fm(w_in, 0, hd * 128, [128], self.xn_rhs, xk, ev_q)
            self.gemm_fm(w_in, 0, 512 + hd * 128, [128], self.xn_rhs, xk, ev_k)
            self.gemm_fm(w_in, 0, 2048 + hd * 256, [128, 128], self.xn_rhs, xk, ev_g)
            self.gemm_tm(w_in, 1024 + hd * 256, 256, vtm, "vtm")
            self.transpose_kg(kgl, kgtm)
            self.gla_core(qg, kg, kgtm, vtm, eb, 2, oT, St, Sbf, PT)
            self.gated_out(oT, sg, 2, P_GN, hd * 256)
        P.barrier()
        A.off = m0
        qT, kT = A.alloc([S], BF16), A.alloc([S], BF16)
        vt = A.alloc([NCH, 128], BF16)
        Pb = [A.alloc([512], BF16) for _ in range(3)]
        osb = [A.alloc([512]) for _ in range(2)]
        Gm = A.alloc([NCH, 8])
        m8 = A.alloc([NCH, 8])
        sel = A.alloc([NCH, 8])
        ksum = A.alloc([8])
        kmT = A.alloc([8], BF16)
        Eh = A.alloc([8, 128], BF16)
        pm = self.cf[:, C_PM:C_PM + 128].rearrange("p (a b) -> p a b", a=NCH)
        p01 = self.cf[:, C_P01:C_P01 + 128].rearrange("p (a b) -> p a b", a=NCH)
        o01 = self.cf[:, C_O01:C_O01 + 128].rearrange("p (a b) -> p a b", a=NCH)
        for hd in ([] if self.dbg == "s2" else [0, 5] if self.dbg == "s3" else range(8)):
            slope = 2.0 ** (-(hd + 1))

            def ev_q(mi, h):
                P.op(DVE, lambda e, h=h: e.tensor_scalar(out=qT, in0=self.banks4(h), scalar1=128 ** -0.5, scalar2=None,
                                                         op0=ALU.mult),
                     reads=self.pk(h), writes=["qT"])

            def ev_k(mi, h):
                for n in range(8):
                    P.op(DVE, lambda e, h=h, n=n: e.tensor_scalar(
                        out=kT[:, n * 256:(n + 1) * 256], in0=self.banks4(h)[:, n * 256:(n + 1) * 256], scalar1=1.0,
                        scalar2=None, op0=ALU.mult, op1=ALU.add, accum_out=ksum[:, n:n + 1]),
                        reads=self.pk(h), writes=["kT", "ksum"])
                P.op(DVE, lambda e: e.tensor_scalar(out=kmT, in0=ksum, scalar1=1.0 / 256, scalar2=None, op0=ALU.mult),
                     reads=["ksum"], writes=["kmT"])
            self.gemm_fm(w_in, 0, 3088 + hd * 128, [128], self.xn_rhs, xk, ev_q)
            self.gemm_fm(w_in, 0, 4112 + hd * 128, [128], self.xn_rhs, xk, ev_k)
            self.gemm_tm(w_in, 5136 + hd * 128, 128, vt, "vtm")
            import os
            stop = os.environ.get("MOBA_STOP", "")
            if stop in ("a", "a0"):
                continue
            for i in range(NCH):
                P.op(PE, lambda e, i=i: e.matmul(self.bank(0, 8, i * 8), lhsT=qT[:, i * 128:(i + 1) * 128], rhs=kmT,
                                                start=True, stop=True),
                     reads=["qT", "kmT"], writes=["pb0"])
            P.op(DVE, lambda e: e.tensor_tensor(out=Gm, in0=self.bank(0, 128).rearrange("p (a b) -> p a b", a=NCH),
                                                in1=pm, op=ALU.add),
                 reads=["pb0", "cf"], writes=["Gm"])
            for i in range(NCH):
                P.op(DVE, lambda e, i=i: e.max(out=m8[:, i, :], in_=Gm[:, i, :]), reads=["Gm"], writes=["m8"])
            P.op(DVE, lambda e: e.tensor_tensor(out=sel, in0=Gm, in1=m8[:, :, 2:3].to_broadcast([128, NCH, 8]),
                                                op=ALU.is_ge),
                 reads=["Gm", "m8"], writes=["sel"])
            P.op(DVE, lambda e: e.tensor_tensor(out=sel, in0=sel, in1=p01, op=ALU.mult),
                 reads=["sel", "cf"], writes=["sel"])
            P.op(DVE, lambda e: e.tensor_tensor(out=sel, in0=sel, in1=o01, op=ALU.add),
                 reads=["sel", "cf"], writes=["sel"])
            P.op(DVE, lambda e: e.tensor_scalar(out=sel, in0=sel, scalar1=-1.0, scalar2=-NEG, op0=ALU.add, op1=ALU.mult),
                 reads=["sel"], writes=["sel"])
            if stop == "b":
                continue
            for i in range(NCH):
                P.op(PE, lambda e, i=i: e.transpose(self.bank(4 + i // 4, 128, (i % 4) * 128)[0:8, :],
                                                   sel[:, i, :], self.ident_f),
                     reads=["sel", "cf"], writes=["pb%d" % (4 + i // 4)])
            P.op(ACT, lambda e: e.activation(out=self.ROWM[0:8, :], in_=self.banks4(1)[0:8, :], func=AF.Copy),
                 reads=self.pk(1), writes=["ROWM"])
            P.op(DVE, lambda e: e.tensor_copy(out=Eh[0:64], in_=self.EM[0:64]), reads=["EM"], writes=["Eh"])
            P.op(DVE, lambda e, slope=slope: e.tensor_scalar(out=Eh[32:34], in0=self.EM[32:34], scalar1=slope,
                                                            scalar2=None, op0=ALU.mult),
                 reads=["EM", "Eh"], writes=["Eh"])
            if stop == "c":
                continue
            self.attention(qT, kT, vt,
                           lambda j, hd=hd: self.cf[:, C_ALC + hd * 16 + j:C_ALC + hd * 16 + j + 1], "cf",
                           lambda j: Eh[0:64, j // 2, :], self.ROWM, 64, ["Eh", "ROWM"], Pb, osb, 1024 + hd * 128)
        P.barrier()
        A.off = m0

    def mix_odd(self, w_in):
        P, A = self.P, self.A
        xk = lambda k: "xn%d" % k
        m0 = A.off
        lbt = self.lbt
        P.op(ACT, lambda e: e.activation(out=lbt[:, 0:16], in_=self.prm[:, P_LB:P_LB + 16], func=AF.Exp),
             reads=["prm"], writes=["lbt"])
        P.op(DVE, lambda e: e.tensor_tensor(out=lbt[:, 0:8], in0=lbt[:, 0:8], in1=lbt[:, 8:16], op=ALU.add),
             reads=["lbt"], writes=["lbt"])
        P.op(DVE, lambda e: e.reciprocal(out=lbt[:, 0:8], in_=lbt[:, 0:8]), reads=["lbt"], writes=["lbt"])
        P.op(DVE, lambda e: e.tensor_tensor(out=lbt[:, 8:16], in0=lbt[:, 8:16], in1=lbt[:, 0:8], op=ALU.mult),
             reads=["lbt"], writes=["lbt"])
        P.op(DVE, lambda e: e.tensor_scalar(out=lbt[:, 0:8], in0=lbt[:, 8:16], scalar1=-1.0, scalar2=1.0,
                                            op0=ALU.mult, op1=ALU.add),
             reads=["lbt"], writes=["lbt"])
        lf = self.HS[0]
        cs = self.RSTD
        cstm = A.alloc([NCH, 8])
        tmp = A.alloc([S])
        nfb = lbt[:, 24:25]
        P.op(DVE, lambda e: e.tensor_scalar(out=nfb[0:8], in0=self.prm[0:8, P_FB:P_FB + 1], scalar1=-1.0, scalar2=None,
                                            op0=ALU.mult), reads=["prm"], writes=["nfb"])

        def ev_f(mi, h):
            P.op(ACT, lambda e, h=h: e.activation(out=lf[0:8, :], in_=self.banks4(h)[0:8, :], func=AF.Exp, scale=-1.0,
                                                  bias=nfb[0:8]), reads=self.pk(h) + ["nfb"], writes=["lf"])
        self.gemm_fm(w_in, 0, 3072, [8], self.xn_rhs, xk, ev_f)
        P.op(ACT, lambda e: e.activation(out=lf[0:8, :], in_=lf[0:8, :], func=AF.Ln, bias=1.0), reads=["lf"], writes=["lf"])
        P.op(DVE, lambda e: e.tensor_tensor_scan(out=cs[0:8, :], data0=self.cbf[0:8, 384:385].to_broadcast([8, S]),
                                                 data1=lf[0:8, :], initial=0.0, op0=ALU.mult, op1=ALU.add),
             reads=["cbf", "lf"], writes=["RSTD"])
        P.op(DVE, lambda e: e.tensor_copy(out=self.ROWF[0:8, :], in_=cs[0:8, :]), reads=["RSTD"], writes=["ROWF"])
        P.op(DVE, lambda e: e.tensor_tensor(out=tmp[0:8, :], in0=cs[0:8, :], in1=self.ROWF[0:8, :], op=ALU.subtract),
             reads=["RSTD", "ROWF"], writes=["tmp"])
        P.op(SP, lambda e: e.dma_start(out=tmp[32:40, :], in_=tmp[0:8, :]), reads=["tmp"], writes=["tmp32"], dma=True)
        P.op(DVE, lambda e: e.tensor_copy(out=self.ROWF[32:40, :], in_=tmp[32:40, :]), reads=["tmp32"], writes=["ROWF"])
        P.op(DVE, lambda e: e.tensor_tensor(out=tmp[32:40, :], in0=tmp[32:40, :], in1=self.ROWF[32:40, :],
                                            op=ALU.subtract), reads=["tmp32", "ROWF"], writes=["tmp32"])
        P.op(SP, lambda e: e.dma_start(out=tmp[64:72, :], in_=tmp[32:40, :]), reads=["tmp32"], writes=["tmp64"], dma=True)
        P.op(DVE, lambda e: e.tensor_copy(out=self.ROWF[64:72, :], in_=tmp[64:72, :]), reads=["tmp64"], writes=["ROWF"])
        for i in range(NCH):
            P.op(PE, lambda e, i=i: e.transpose(self.bank(0, 8, i * 8), cs[0:8, i * 128:(i + 1) * 128],
                                               self.ident_f[0:8, 0:8]),
                 reads=["RSTD", "cf"], writes=["pb0"])
        P.op(DVE, lambda e: e.tensor_copy(out=cstm, in_=self.bank(0, 128).rearrange("p (a b) -> p a b", a=NCH)),
             reads=["pb0"], writes=["bcol"])
        qT, kT = A.alloc([S], BF16), A.alloc([S], BF16)
        vt = A.alloc([NCH, 128], BF16)
        Pb = [A.alloc([512], BF16) for _ in range(3)]
        osb = [A.alloc([512]) for _ in range(2)]
        for hd in range(8):
            def ev_q(mi, h):
                P.op(DVE, lambda e, h=h: e.tensor_scalar(out=qT, in0=self.banks4(h), scalar1=128 ** -0.5, scalar2=None,
                                                         op0=ALU.mult),
                     reads=self.pk(h), writes=["qT"])

            def ev_k(mi, h):
                P.op(ACT, lambda e, h=h: e.activation(out=kT, in_=self.banks4(h), func=AF.Copy),
                     reads=self.pk(h), writes=["kT"])
            self.gemm_fm(w_in, 0, hd * 128, [128], self.xn_rhs, xk, ev_q)
            self.gemm_fm(w_in, 0, 1024 + hd * 128, [128], self.xn_rhs, xk, ev_k)
            self.gemm_tm(w_in, 2048 + hd * 128, 128, vt, "vtm")
            self.attention(qT, kT, vt, lambda j, hd=hd: cstm[:, j, hd:hd + 1], "bcol",
                           lambda j, hd=hd: self.EF[:, hd, :], self.ROWF, 128, ["EF", "ROWF"], Pb, osb, hd * 128)
        P.barrier()
        A.off = m0
        F1, eb, enb, ebl = A.alloc([S]), A.alloc([S]), A.alloc([S]), A.alloc([S])
        qg, kg = A.alloc([S], BF16), A.alloc([S], BF16)
        kgl = self.SQB[:, 0, :]
        c3 = lambda a: a.rearrange("p (c j) -> p c j", j=64)
        kgtm = A.alloc([NCH, 128], BF16)
        vtm = A.alloc([NCH, 128], BF16)
        sg = A.alloc([1, S], BF16)
        oT = A.alloc([1, S])
        St = A.alloc([128])
        Sbf = [A.alloc([128], BF16) for _ in range(2)]
        PT = [A.alloc([128], BF16) for _ in range(2)]
        for hd in range(8):
            lb = lbt[:, 8 + hd:9 + hd]
            oml = lbt[:, hd:hd + 1]

            def ev_f(mi, h, oml=oml, lb=lb):
                P.op(ACT, lambda e, h=h: e.activation(out=F1, in_=self.banks4(h), func=AF.Sigmoid),
                     reads=self.pk(h), writes=["F1"])
                P.op(DVE, lambda e, oml=oml, lb=lb: e.tensor_scalar(out=F1, in0=F1, scalar1=oml, scalar2=lb,
                                                                   op0=ALU.mult, op1=ALU.add),
                     reads=["F1", "lbt"], writes=["F1"])
                P.op(ACT, lambda e: e.activation(out=eb, in_=F1, func=AF.Ln), reads=["F1"], writes=["eb"])
                P.op(DVE, lambda e: e.tensor_tensor_scan(out=cs, data0=self.RM, data1=eb, initial=0.0,
                                                         op0=ALU.mult, op1=ALU.add),
                     reads=["RM", "eb"], writes=["RSTD"])
                P.op(ACT, lambda e: e.activation(out=enb, in_=cs, func=AF.Exp, scale=-1.0), reads=["RSTD"], writes=["enb"])
                P.op(ACT, lambda e: e.activation(out=eb, in_=cs, func=AF.Exp), reads=["RSTD"], writes=["eb"])
                P.op(DVE, lambda e: e.tensor_scalar(out=F1, in0=F1, scalar1=-1.0, scalar2=1.0, op0=ALU.mult, op1=ALU.add),
                     reads=["F1"], writes=["F1"])
                P.op(DVE, lambda e: e.tensor_tensor(out=kg, in0=F1, in1=enb, op=ALU.mult),
                     reads=["F1", "enb"], writes=["kg"])
                P.op(DVE, lambda e: e.tensor_tensor(out=c3(ebl), in0=c3(enb),
                                                    in1=c3(eb)[:, :, 63:64].to_broadcast([128, 32, 64]), op=ALU.mult),
                     reads=["enb", "eb"], writes=["ebl"])
                P.op(DVE, lambda e: e.tensor_tensor(out=kgl, in0=F1, in1=ebl, op=ALU.mult),
                     reads=["F1", "ebl"], writes=["SQB0"])

            def ev_q(mi, h):
                P.op(ACT, lambda e, h=h: e.activation(out=F1, in_=self.banks4(h), func=AF.Silu),
                     reads=self.pk(h), writes=["F1"])
                P.op(DVE, lambda e: e.tensor_tensor(out=qg, in0=F1, in1=eb, op=ALU.mult),
                     reads=["F1", "eb"], writes=["qg"])

            def ev_g(mi, h):
                P.op(ACT, lambda e, h=h: e.activation(out=sg[:, 0, :], in_=self.banks4(h), func=AF.Silu),
                     reads=self.pk(h), writes=["sg"])
            self.gemm_fm(w_in, 0, 4104 + hd * 128, [128], self.xn_rhs, xk, ev_f)
            self.gemm_fm(w_in, 0, 3080 + hd * 128, [128], self.xn_rhs, xk, ev_q)
            self.gemm_fm(w_in, 0, 6152 + hd * 128, [128], self.xn_rhs, xk, ev_g)
            self.gemm_tm(w_in, 5128 + hd * 128, 128, vtm, "vtm")
            self.transpose_kg(kgl, kgtm)
            self.gla_core(qg, kg, kgtm, vtm, eb, 1, oT, St, Sbf, PT)
            self.gated_out(oT, sg, 1, P_HN, 1024 + hd * 128)
        P.barrier()
        A.off = m0


def _consts():
    c = np.zeros((128, NCST), np.float32)
    p = np.arange(128)
    c[:, C_ID:C_ID + 128] = np.eye(128, dtype=np.float32)
    s, t = p[:, None], p[None, :]
    c[:, C_UB:C_UB + 128] = ((s <= t) & (s // 64 == t // 64)).astype(np.float32)
    c[:, C_CM:C_CM + 128] = np.where(s <= t, 0.0, NEG).astype(np.float32)
    c[:, C_ONE:C_ONE + 128] = 1.0
    for h in range(8):
        for j in range(16):
            c[:, C_ALC + h * 16 + j] = (2.0 ** (-(h + 1))) * (128 * j + p)
    for i in range(16):
        for n in range(8):
            c[:, C_PM + i * 8 + n] = 0.0 if n < i // 2 else -1e30
            c[:, C_P01 + i * 8 + n] = 1.0 if n < i // 2 else 0.0
            c[:, C_O01 + i * 8 + n] = 1.0 if n == i // 2 else 0.0
    for n in range(8):
        c[n, C_EM + n * 128:C_EM + (n + 1) * 128] = 1.0
        c[32:34, C_EM + n * 128:C_EM + (n + 1) * 128] = 1.0
        for r in (n, 32 + n, 64 + n):
            c[r, C_EF + n * 128:C_EF + (n + 1) * 128] = -1.0
    tt = np.arange(S)
    c[32, C_AR:C_AR + S] = -128.0 * (tt // 128)
    c[33, C_AR:C_AR + S] = -(tt % 128).astype(np.float32)
    c[:, C_RM:C_RM + S] = (tt % 64 != 0).astype(np.float32)[None, :]
    return c


def _params(inp):
    pr = np.zeros((128, NPAR), np.float32)
    fm = lambda v: np.asarray(v, np.float32).reshape(-1, 128).T
    pr[:, 0:16] = fm(inp["ev_norm_mix"][0])
    pr[:, 16:32] = fm(inp["ev_norm_mlp"][0])
    pr[:, 32:48] = fm(inp["od_norm_mix"][0])
    pr[:, 48:64] = fm(inp["od_norm_mlp"][0])
    pr[:, 64:80] = fm(inp["final_norm"])
    pr[:, P_GB:P_GB + 4] = fm(inp["ev_gla_gate_b"][0])
    pr[:, P_GN:P_GN + 2] = fm(inp["ev_gla_out_norm"][0])
    pr[:, P_HN:P_HN + 1] = fm(inp["od_hgrn_out_norm"][0])
    pr[:, P_LB:P_LB + 8] = fm(inp["hgrn_lb_raw"][0])
    pr[:, P_LB + 8:P_LB + 16] = fm(inp["hgrn_lb_raw"][1])
    pr[0:8, P_FB] = np.asarray(inp["od_fox_fgate_b"][0], np.float32)
    return pr


_NC_CACHE = {}


def make_in_maps(inp, ncores=8):
    cst = _consts()
    prm = _params(inp)
    shared = {"w2": np.ascontiguousarray(np.asarray(inp["ev_gla_gate_w2"][0], np.float32)), "prm": prm, "cst": cst}
    for p in ("ev", "od"):
        for w in ("w_in", "w_out", "w_up", "w_down"):
            shared["%s_%s" % (p, w)] = np.ascontiguousarray(np.asarray(inp["%s_%s" % (p, w)][0], np.float32))
    x = np.asarray(inp["x"], np.float32)
    maps = []
    for b in range(ncores):
        m = dict(shared)
        m["xT"] = np.ascontiguousarray(x[b].T)
        maps.append(m)
    return maps


def kernel(**inputs):
    if "nc" not in _NC_CACHE:
        _NC_CACHE["nc"] = Builder().run()
    nc = _NC_CACHE["nc"]
    maps = make_in_maps(inputs)
    res = run_bass_kernel_spmd(nc, maps, core_ids=list(range(8)))
    out = np.stack([np.asarray(r["outT"]).T for r in res.results], axis=0)
    return np.ascontiguousarray(out.astype(np.float32))
```

```python
import numpy as np
from contextlib import ExitStack
import concourse.bass as bass
import concourse.mybir as mybir
from concourse.bass_utils import run_bass_kernel_spmd

F32 = mybir.dt.float32
BF16 = mybir.dt.bfloat16
AF = mybir.ActivationFunctionType
ALU = mybir.AluOpType
AX = mybir.AxisListType

S = 2048
D = 2048
DFF = 8192
NCH = 16
EPS = 1e-6
NIN = (6160, 7176)
NEG = -30000.0

PE, ACT, DVE, POOL, SP = "pe", "act", "dve", "pool", "sp"
ENGS = [PE, ACT, DVE, POOL, SP]
NDMA_SEM = 8

C_ID, C_UB, C_CM, C_ONE, C_ALC, C_PM, C_P01, C_O01 = 0, 128, 256, 384, 512, 640, 768, 896
C_EM, C_EF, C_AR, C_RM, NCST = 1024, 2048, 3072, 5120, 7168
P_NORM = 0
P_GB = 80
P_GN = 84
P_HN = 86
P_LB = 87
P_FB = 103
NPAR = 104


class Op:
    __slots__ = ("eng", "fn", "deps", "dma", "idx", "signal", "cnt", "dsem", "dval", "prev")

    def __init__(self, eng, fn, dma):
        self.eng, self.fn, self.dma = eng, fn, dma
        self.deps = set()
        self.signal = False
        self.cnt = 0
        self.dsem = None
        self.dval = 0
        self.prev = None


class Prog:
    def __init__(self, nc):
        self.nc = nc
        self.ops = []
        self.lastw = {}
        self.readers = {}
        self.last_eng = {}
        self.dmas_since = []

    def op(self, eng, fn, reads=(), writes=(), dma=False):
        o = Op(eng, fn, dma)
        o.idx = len(self.ops)
        pr = [r for r in reads if r.startswith("pb")]
        if pr:
            reads = [r for r in reads if not r.startswith("pb")]
            writes = list(writes) + pr
        for r in reads:
            w = self.lastw.get(r)
            if w is not None:
                o.deps.add(w)
        for r in writes:
            w = self.lastw.get(r)
            if w is not None:
                o.deps.add(w)
            for rd in self.readers.get(r, ()):
                o.deps.add(rd)
        for r in writes:
            self.lastw[r] = o.idx
            self.readers[r] = []
        for r in reads:
            if r not in writes:
                self.readers.setdefault(r, []).append(o.idx)
        o.deps.discard(o.idx)
        self.ops.append(o)
        if dma:
            self.dmas_since.append(o.idx)
        else:
            self.last_eng[eng] = o.idx
        return o

    def barrier(self):
        deps = set(self.last_eng.values()) | set(self.dmas_since)
        for e in ENGS:
            o = Op(e, None, False)
            o.idx = len(self.ops)
            o.deps = set(deps)
            self.ops.append(o)
        self.lastw.clear()
        self.readers.clear()
        self.dmas_since = []

    def emit(self, stack):
        nc = self.nc
        ops = self.ops
        for o in ops:
            best = {}
            nd = set()
            for d in o.deps:
                p = ops[d]
                if p.dma:
                    nd.add(d)
                    continue
                if p.eng == o.eng and o.eng == PE and not o.dma:
                    continue
                if p.eng not in best or best[p.eng] < d:
                    best[p.eng] = d
            nd |= set(best.values())
            o.deps = nd
            for d in nd:
                ops[d].signal = True
        sems = {e: stack.enter_context(nc.semaphore("s_" + e)) for e in ENGS}
        dsems = {e: [stack.enter_context(nc.semaphore("d_%s_%d" % (e, i))) for i in range(NDMA_SEM)]
                 for e in (ACT, POOL, SP)}
        cnt = {e: 0 for e in ENGS}
        per = {e: [] for e in ENGS}
        hist = {e: [] for e in ENGS}
        for o in ops:
            if o.dma:
                j = len(hist[o.eng])
                o.dsem = dsems[o.eng][j % NDMA_SEM]
                o.dval = 16 * (j // NDMA_SEM + 1)
                if j >= NDMA_SEM:
                    o.prev = hist[o.eng][j - NDMA_SEM]
                hist[o.eng].append(o)
            elif o.signal:
                cnt[o.eng] += 1
                o.cnt = cnt[o.eng]
            per[o.eng].append(o)
        self.stats = {e: len(per[e]) for e in ENGS}
        self.stats["sig"] = dict(cnt)
        block = stack.enter_context(nc.Block())

        def run(eng_name, e):
            waited = {}

            def wait(sem, val):
                k = id(sem)
                if waited.get(k, 0) >= val:
                    return
                waited[k] = val
                e.wait_ge(sem, val)

            for o in per[eng_name]:
                need = {}
                for d in o.deps:
                    p = ops[d]
                    s, v = (p.dsem, p.dval) if p.dma else (sems[p.eng], p.cnt)
                    if id(s) not in need or need[id(s)][1] < v:
                        need[id(s)] = (s, v)
                if o.dma and o.prev is not None:
                    s, v = o.prev.dsem, o.prev.dval
                    if id(s) not in need or need[id(s)][1] < v:
                        need[id(s)] = (s, v)
                for s, v in need.values():
                    wait(s, v)
                if o.fn is None:
                    continue
                ins = o.fn(e)
                if o.dma:
                    ins.then_inc(o.dsem, 16)
                elif o.signal:
                    ins.then_inc(sems[o.eng], 1)
            for o in hist[eng_name][-NDMA_SEM:]:
                wait(o.dsem, o.dval)

        block.tensor(lambda e: run(PE, e))
        block.scalar(lambda e: run(ACT, e))
        block.vector(lambda e: run(DVE, e))
        block.gpsimd(lambda e: run(POOL, e))
        block.sync(lambda e: run(SP, e))


class Arena:
    def __init__(self, ap, nwords):
        self.ap = ap
        self.n = nwords
        self.off = 0

    def alloc(self, shape, dtype=F32, parts=128):
        n = int(np.prod(shape))
        words = n if dtype == F32 else (n + 1) // 2
        words = (words + 7) // 8 * 8
        assert self.off + words <= self.n, ("arena overflow", self.off, words, self.n)
        v = self.ap[0:parts, self.off:self.off + words]
        self.off += words
        if dtype != F32:
            v = v.bitcast(dtype)
        v = v[:, 0:n]
        if len(shape) == 2:
            v = v.rearrange("p (a b) -> p a b", a=shape[0])
        elif len(shape) == 3:
            v = v.rearrange("p (a b c) -> p a b c", a=shape[0], b=shape[1])
        return v


class Builder:
    def __init__(self, dbg=None):
        self.dbg = dbg
        nc = self.nc = bass.Bass("TRN2", target_bir_lowering=False)
        self.P = Prog(nc)

        def di(name, shape, dt=F32):
            return nc.dram_tensor(name, shape, dt, kind="ExternalInput").ap()

        self.xT = di("xT", [D, S])
        self.W = []
        for l, p in ((0, "ev"), (1, "od")):
            self.W.append(dict(w_in=di(p + "_w_in", [D, NIN[l]]), w_out=di(p + "_w_out", [D, D]),
                               w_up=di(p + "_w_up", [D, DFF]), w_down=di(p + "_w_down", [DFF, D])))
        self.w2d = di("w2", [16, 512])
        self.prmd = di("prm", [128, NPAR])
        self.cstd = di("cst", [128, NCST])
        self.outT = nc.dram_tensor("outT", [D, S], F32, kind="ExternalOutput").ap()
        self.hT = nc.dram_tensor("hT", [D, S], F32).ap()
        self.mixd = nc.dram_tensor("mixd", [D, S], BF16, kind=("ExternalOutput" if dbg in ("mix", "s2", "s3", "s4", "s5", "s6") else "Internal")).ap()
        if dbg == "h":
            self.hdbg = nc.dram_tensor("hdbg", [D, S], F32, kind="ExternalOutput").ap()

    def bank(self, b, n=512, c0=0):
        return self.PS[:, 512 * b + c0:512 * b + c0 + n]

    def banks4(self, h):
        return self.PS[:, 2048 * h:2048 * h + 2048]

    def pk(self, h):
        return ["pb%d" % b for b in range(4 * h, 4 * h + 4)]

    def run(self):
        nc, P = self.nc, self.P
        with ExitStack() as st:
            NW = 53000
            arena_t = st.enter_context(nc.sbuf_tensor("arena", [128, NW], F32))
            self.PS = st.enter_context(nc.psum_tensor("ps", [128, 4096], F32))
            A = self.A = Arena(arena_t, NW)
            self.XN = A.alloc([NCH, S], BF16)
            self.WB = [A.alloc([NCH, 256], BF16) for _ in range(2)]
            self.HS = [A.alloc([S], F32) for _ in range(2)]
            self.RSTD = A.alloc([S], F32)
            self.SQB = A.alloc([2, S], BF16)
            self.cbf = A.alloc([512], BF16)
            self.EM = A.alloc([8, 128], BF16)
            self.EF = A.alloc([8, 128], BF16)
            self.ROWM = A.alloc([S], BF16)
            self.ROWF = A.alloc([S], BF16)
            self.RM = A.alloc([S], BF16)
            self.cf = A.alloc([1024], F32)
            self.prm = A.alloc([NPAR], F32)
            self.w2 = A.alloc([512], F32)
            self.lbt = A.alloc([32], F32)
            self.mark = A.off
            self.wslot = 0
            self.ident_bf = self.cbf[:, 0:128]
            self.ublk_bf = self.cbf[:, 128:256]
            self.cm_bf = self.cbf[:, 256:384]
            self.ones_bf = self.cbf[:, 384:512]
            self.ident_f = self.cf[:, C_ID:C_ID + 128]
            self.ublk_f = self.cf[:, C_UB:C_UB + 128]

            self.load_consts()
            self.copy_x()
            if self.dbg != "s1":
                self.layer(0)
            if self.dbg not in ("l0", "mix", "s1", "s2", "s3", "s4"):
                self.layer(1)
            self.final_norm()
            P.emit(st)
        return nc

    def load_consts(self):
        P, c = self.P, self.cstd
        P.op(POOL, lambda e: e.dma_start(out=self.cbf, in_=c[:, 0:512]), writes=["cbf"], dma=True)
        P.op(POOL, lambda e: e.dma_start(out=self.EM.rearrange("p a b -> p (a b)"), in_=c[:, C_EM:C_EM + 1024]),
             writes=["EM"], dma=True)
        P.op(POOL, lambda e: e.dma_start(out=self.EF.rearrange("p a b -> p (a b)"), in_=c[:, C_EF:C_EF + 1024]),
             writes=["EF"], dma=True)
        P.op(POOL, lambda e: e.dma_start(out=self.ROWM, in_=c[:, C_AR:C_AR + S]), writes=["ROWM"], dma=True)
        P.op(POOL, lambda e: e.dma_start(out=self.RM, in_=c[:, C_RM:C_RM + S]), writes=["RM"], dma=True)
        P.op(SP, lambda e: e.dma_start(out=self.cf, in_=c[:, 0:1024]), writes=["cf"], dma=True)
        P.op(SP, lambda e: e.dma_start(out=self.prm, in_=self.prmd), writes=["prm"], dma=True)
        P.op(SP, lambda e: e.dma_start(out=self.w2[0:16, :], in_=self.w2d), writes=["w2"], dma=True)
        P.op(DVE, lambda e: e.memset(self.ROWF, 0.0), writes=["ROWF"])

    def copy_x(self):
        for c in range(NCH):
            self.P.op(SP, lambda e, c=c: e.dma_start(out=self.hT[c * 128:(c + 1) * 128, :],
                                                      in_=self.xT[c * 128:(c + 1) * 128, :]),
                      writes=["hT%d" % c], dma=True)

    def norm(self, gcol, final=False):
        P = self.P
        for c in range(NCH):
            hs, hk = self.HS[c % 2], "HS%d" % (c % 2)
            sq, sk = self.SQB[:, c % 2, :], "SQB%d" % (c % 2)
            P.op(SP, lambda e, c=c, hs=hs: e.dma_start(out=hs, in_=self.hT[c * 128:(c + 1) * 128, :]),
                 reads=["hT%d" % c], writes=[hk], dma=True)
            P.op(ACT, lambda e, hs=hs, sq=sq: e.activation(out=sq, in_=hs, func=AF.Square),
                 reads=[hk], writes=[sk])
            for n in range(4):
                P.op(PE, lambda e, c=c, n=n, sq=sq: e.matmul(self.bank(n), lhsT=self.ones_bf,
                                                             rhs=sq[:, n * 512:(n + 1) * 512],
                                                             start=(c == 0), stop=(c == NCH - 1)),
                     reads=[sk, "cbf"], writes=["pb%d" % n])
        P.op(ACT, lambda e: e.activation(out=self.RSTD, in_=self.banks4(0), func=AF.Ln, scale=1.0 / D, bias=EPS),
             reads=self.pk(0), writes=["RSTD"])
        P.op(ACT, lambda e: e.activation(out=self.RSTD, in_=self.RSTD, func=AF.Exp, scale=-0.5),
             reads=["RSTD"], writes=["RSTD"])
        for c in range(NCH):
            hs, hk = self.HS[c % 2], "HS%d" % (c % 2)
            P.op(SP, lambda e, c=c, hs=hs: e.dma_start(out=hs, in_=self.hT[c * 128:(c + 1) * 128, :]),
                 reads=["hT%d" % c], writes=[hk], dma=True)
            g = self.prm[:, gcol + c:gcol + c + 1]
            if not final:
                P.op(DVE, lambda e, c=c, hs=hs, g=g: e.scalar_tensor_tensor(
                    out=self.XN[:, c, :], in0=hs, scalar=g, in1=self.RSTD, op0=ALU.mult, op1=ALU.mult),
                    reads=[hk, "RSTD", "prm"], writes=["xn%d" % c])
            else:
                P.op(DVE, lambda e, c=c, hs=hs, g=g: e.scalar_tensor_tensor(
                    out=hs, in0=hs, scalar=g, in1=self.RSTD, op0=ALU.mult, op1=ALU.mult),
                    reads=[hk, "RSTD", "prm"], writes=[hk])
                P.op(SP, lambda e, c=c, hs=hs: e.dma_start(out=self.outT[c * 128:(c + 1) * 128, :], in_=hs),
                     reads=[hk], writes=["out%d" % c], dma=True)

    def final_norm(self):
        if self.dbg == "h":
            for c in range(NCH):
                self.P.op(SP, lambda e, c=c: e.dma_start(out=self.hdbg[c * 128:(c + 1) * 128, :],
                                                          in_=self.hT[c * 128:(c + 1) * 128, :]),
                          reads=["hT%d" % c], writes=["hd%d" % c], dma=True)
        self.norm(P_NORM + 64, final=True)

    def load_w(self, wd, r0, nk, c0, ncols):
        s = self.wslot % 2
        self.wslot += 1
        wb, key = self.WB[s], "wb%d" % s
        self.P.op(POOL, lambda e: e.dma_start(
            out=wb[:, 0:nk, 0:ncols],
            in_=wd[r0:r0 + nk * 128, c0:c0 + ncols].rearrange("(c p) n -> p c n", p=128)),
            writes=[key], dma=True)
        return wb, key

    def gemm_fm(self, wd, r0, c0, widths, rhs, rkey, evac, nk=NCH, nbank=4):
        P = self.P
        mi = 0
        i = 0
        while i < len(widths):
            grp = [widths[i]]
            if i + 1 < len(widths) and widths[i] + widths[i + 1] <= 256:
                grp.append(widths[i + 1])
            tot = sum(grp)
            wb, wkey = self.load_w(wd, r0, nk, c0, tot)
            off = 0
            for mw in grp:
                h = self.gpar % 2
                self.gpar += 1
                for k in range(nk):
                    for n in range(nbank):
                        P.op(PE, lambda e, k=k, n=n, h=h, off=off, mw=mw, wb=wb: e.matmul(
                            self.bank(4 * h + n)[0:mw, :], lhsT=wb[:, k, off:off + mw], rhs=rhs(k, n),
                            start=(k == 0), stop=(k == nk - 1)),
                            reads=[wkey, rkey(k)], writes=["pb%d" % (4 * h + n)])
                evac(mi, h)
                mi += 1
                off += mw
            c0 += tot
            i += len(grp)

    def xn_rhs(self, k, n):
        return self.XN[:, k, n * 512:(n + 1) * 512]

    def gemm_tm(self, wd, c0, ncols, vtm, vkey):
        P = self.P
        vT = self.SQB[:, 1, :]

        def ev(mi, h):
            P.op(ACT, lambda e, h=h: e.activation(out=vT, in_=self.banks4(h), func=AF.Copy),
                 reads=self.pk(h), writes=["SQB1"])
            for hb in range(2):
                b = 4 * h + 2 * (self.tmb % 2) + hb
                pbv = self.bank(b).bitcast(BF16)
                for j in range(8):
                    t = hb * 8 + j
                    P.op(PE, lambda e, t=t, j=j, pbv=pbv: e.transpose(pbv[:, j * 128:(j + 1) * 128],
                                                                      vT[:, t * 128:(t + 1) * 128], self.ident_bf),
                         reads=["SQB1", "cbf"], writes=["pb%d" % b])
                P.op(DVE, lambda e, hb=hb, pbv=pbv, mi=mi: e.tensor_copy(
                    out=vtm[:, hb * 8:hb * 8 + 8, mi * 128:(mi + 1) * 128],
                    in_=pbv.rearrange("p (a c) -> p a c", a=8)),
                    reads=["pb%d" % b], writes=[vkey])
            self.tmb += 1
        self.gemm_fm(wd, 0, c0, [128] * (ncols // 128), self.xn_rhs, lambda k: "xn%d" % k, ev)

    def gla_core(self, qg, kg, kgtm, vtm, eb, dvh, oT, St, Sbf, PT):
        P = self.P
        dv = dvh * 128
        P.op(DVE, lambda e: e.memset(St, 0.0), writes=["St"])
        P.op(DVE, lambda e: e.memset(Sbf[0], 0.0), writes=["Sbf0"])
        sidx = 0

        def emit_sc(i):
            tl = slice(i * 128, (i + 1) * 128)
            sb = 4 if i % 2 == 0 else 7
            pt, ptk = PT[i % 2], "PT%d" % (i % 2)
            P.op(PE, lambda e: e.matmul(self.bank(sb, 128), lhsT=kg[:, tl], rhs=qg[:, tl], start=True, stop=True),
                 reads=["kg", "qg"], writes=["pb%d" % sb])
            P.op(DVE, lambda e: e.tensor_tensor(out=pt, in0=self.bank(sb, 128), in1=self.ublk_f, op=ALU.mult),
                 reads=["pb%d" % sb, "cf"], writes=[ptk])

        def ubank(i, cc):
            if dvh == 1 and i % 2 == 1:
                return 1 + 2 * cc
            return 5 + cc

        def emit_u(i):
            for cc in range(2):
                rs = slice(cc * 64, cc * 64 + 64)
                ub = ubank(i, cc)
                P.op(PE, lambda e, cc=cc, rs=rs, ub=ub: e.matmul(
                    self.bank(ub, dv), lhsT=kgtm[rs, i, :], rhs=vtm[rs, i, :], start=True, stop=True),
                    reads=["kgtm", "vtm"], writes=["pb%d" % ub])

        emit_u(0)
        emit_sc(0)
        for i in range(NCH):
            g = i // 4
            pt, ptk = PT[i % 2], "PT%d" % (i % 2)
            obanks = [(2 * (g % 2) + hh) for hh in range(dvh)]
            col = (i % 4) * 128
            for hh in range(dvh):
                ob = obanks[hh]
                P.op(PE, lambda e, i=i, hh=hh, ob=ob, col=col, pt=pt: e.matmul(
                    self.bank(ob, 128, col), lhsT=vtm[:, i, hh * 128:(hh + 1) * 128], rhs=pt, start=True, stop=False),
                    reads=["vtm", ptk], writes=["pb%d" % ob])
            if i + 1 < NCH:
                emit_sc(i + 1)
                if dvh == 1:
                    emit_u(i + 1)
            for cc in range(2):
                ub = ubank(i, cc)
                c = 2 * i + cc
                cur, nxt = Sbf[sidx % 2], Sbf[(sidx + 1) % 2]
                ck, nk_ = "Sbf%d" % (sidx % 2), "Sbf%d" % ((sidx + 1) % 2)
                qs = slice(i * 128 + cc * 64, i * 128 + cc * 64 + 64)
                for hh in range(dvh):
                    ob = obanks[hh]
                    P.op(PE, lambda e, hh=hh, ob=ob, col=col, cc=cc, cur=cur, qs=qs: e.matmul(
                        self.bank(ob, 64, col + cc * 64), lhsT=cur[:, hh * 128:(hh + 1) * 128], rhs=qg[:, qs],
                        start=False, stop=(cc == 1)),
                        reads=[ck, "qg"], writes=["pb%d" % ob])
                if c == 2 * NCH - 1:
                    break
                el = eb[:, c * 64 + 63:c * 64 + 64]
                P.op(DVE, lambda e, cc=cc, nxt=nxt, el=el, ub=ub: e.scalar_tensor_tensor(
                    out=nxt, in0=St, scalar=el, in1=self.bank(ub, dv), op0=ALU.mult, op1=ALU.add),
                    reads=["pb%d" % ub, "St", "eb"], writes=[nk_])
                P.op(DVE, lambda e, cc=cc, el=el, ub=ub: e.scalar_tensor_tensor(
                    out=St, in0=St, scalar=el, in1=self.bank(ub, dv), op0=ALU.mult, op1=ALU.add),
                    reads=["pb%d" % ub, "St", "eb"], writes=["St"])
                sidx += 1
            if dvh != 1 and i + 1 < NCH:
                emit_u(i + 1)
            if i % 4 == 3:
                for hh in range(dvh):
                    ob = obanks[hh]
                    P.op(ACT, lambda e, hh=hh, ob=ob, g=g: e.activation(
                        out=oT[:, hh, g * 512:(g + 1) * 512], in_=self.bank(ob), func=AF.Copy),
                        reads=["pb%d" % ob], writes=["oT"])

    def transpose_kg(self, kg, kgtm):
        P = self.P
        for hb in range(2):
            b = 6 + hb
            pbv = self.bank(b).bitcast(BF16)
            for j in range(8):
                t = hb * 8 + j
                P.op(PE, lambda e, t=t, j=j, pbv=pbv: e.transpose(pbv[:, j * 128:(j + 1) * 128],
                                                                  kg[:, t * 128:(t + 1) * 128], self.ident_bf),
                     reads=["SQB0", "cbf"], writes=["pb%d" % b])
            P.op(DVE, lambda e, hb=hb, pbv=pbv: e.tensor_copy(
                out=kgtm[:, hb * 8:hb * 8 + 8, :], in_=pbv.rearrange("p (a c) -> p a c", a=8)),
                reads=["pb%d" % b], writes=["kgtm"])

    def gated_out(self, oT, sg, dvh, gain_col, row0):
        P = self.P
        dv = dvh * 128
        for hh in range(dvh):
            P.op(ACT, lambda e, hh=hh: e.activation(out=self.SQB[:, hh, :], in_=oT[:, hh, :], func=AF.Square),
                 reads=["oT"], writes=["SQB%d" % hh])
        for n in range(4):
            for hh in range(dvh):
                P.op(PE, lambda e, n=n, hh=hh: e.matmul(self.bank(4 + n), lhsT=self.ones_bf,
                                                        rhs=self.SQB[:, hh, n * 512:(n + 1) * 512],
                                                        start=(hh == 0), stop=(hh == dvh - 1)),
                     reads=["SQB%d" % hh, "cbf"], writes=["pb%d" % (4 + n)])
        P.op(ACT, lambda e: e.activation(out=self.RSTD, in_=self.banks4(1), func=AF.Ln, scale=1.0 / dv, bias=EPS),
             reads=self.pk(1), writes=["RSTD"])
        P.op(ACT, lambda e: e.activation(out=self.RSTD, in_=self.RSTD, func=AF.Exp, scale=-0.5),
             reads=["RSTD"], writes=["RSTD"])
        for hh in range(dvh):
            g = self.prm[:, gain_col + hh:gain_col + hh + 1]
            P.op(DVE, lambda e, hh=hh, g=g: e.scalar_tensor_tensor(
                out=oT[:, hh, :], in0=oT[:, hh, :], scalar=g, in1=self.RSTD, op0=ALU.mult, op1=ALU.mult),
                reads=["oT", "RSTD", "prm"], writes=["oT"])
            ms = self.HS[1].bitcast(BF16)[:, hh * S:(hh + 1) * S]
            P.op(DVE, lambda e, hh=hh, ms=ms: e.tensor_tensor(out=ms, in0=oT[:, hh, :], in1=sg[:, hh, :], op=ALU.mult),
                 reads=["oT", "sg"], writes=["ms%d" % hh])
            r = row0 + hh * 128
            P.op(SP, lambda e, ms=ms, r=r: e.dma_start(out=self.mixd[r:r + 128, :], in_=ms),
                 reads=["ms%d" % hh], writes=["mix%d" % (r // 128)], dma=True)

    def attention(self, qT, kT, vtm, bias_col, bias_key, row_lhsT, row_rhs, nrow, row_keys, Pb, osb, row0):
        P = self.P
        steps = [(n, j) for n in range(4) for j in range(4 * n + 4)]

        def geo(s):
            n, j = steps[s]
            c0 = max(0, j - 4 * n) * 128
            return n, j, c0, 512 - c0, s % 4, Pb[s % 3], "Pb%d" % (s % 3)

        def emit_qk(s):
            n, j, c0, N, sb, pb, pbk = geo(s)
            diag = j >= 4 * n
            tq = slice(512 * n + c0, 512 * (n + 1))
            P.op(PE, lambda e: e.matmul(self.bank(sb, N, c0), lhsT=kT[:, j * 128:(j + 1) * 128], rhs=qT[:, tq],
                                        start=True, stop=False),
                 reads=["kT", "qT"], writes=["pb%d" % sb])
            P.op(PE, lambda e: e.matmul(self.bank(sb, N, c0), lhsT=row_lhsT(j), rhs=row_rhs[0:nrow, tq],
                                        start=False, stop=(not diag)),
                 reads=row_keys, writes=["pb%d" % sb])
            if diag:
                P.op(PE, lambda e: e.matmul(self.bank(sb, 128, c0), lhsT=self.ident_bf, rhs=self.cm_bf,
                                            start=False, stop=True),
                     reads=["cbf"], writes=["pb%d" % sb])
            P.op(ACT, lambda e: e.activation(out=pb[:, c0:512], in_=self.bank(sb, N, c0), func=AF.Exp, bias=bias_col(j)),
                 reads=["pb%d" % sb, bias_key], writes=[pbk])

        def emit_pv(s):
            n, j, c0, N, sb, pb, pbk = geo(s)
            ob, lb = (4, 5) if n % 2 == 0 else (6, 7)
            jmax = 4 * n + 3
            P.op(PE, lambda e: e.matmul(self.bank(ob, N, c0), lhsT=vtm[:, j, :], rhs=pb[:, c0:512],
                                        start=(j == 0), stop=(j == jmax)),
                 reads=["vtm", pbk], writes=["pb%d" % ob])
            P.op(PE, lambda e: e.matmul(self.bank(lb, N, c0), lhsT=self.ones_bf, rhs=pb[:, c0:512],
                                        start=(j == 0), stop=(j == jmax)),
                 reads=["cbf", pbk], writes=["pb%d" % lb])
            if j == jmax:
                rl, rk = osb[n % 2], "osb%d" % (n % 2)
                P.op(DVE, lambda e: e.reciprocal(out=rl, in_=self.bank(lb)), reads=["pb%d" % lb], writes=[rk])
                ms = self.HS[1].bitcast(BF16)[:, n * 512:(n + 1) * 512]
                P.op(DVE, lambda e: e.tensor_tensor(out=ms, in0=self.bank(ob), in1=rl, op=ALU.mult),
                     reads=["pb%d" % ob, rk], writes=["ms%d" % n])
                P.op(SP, lambda e: e.dma_start(out=self.mixd[row0:row0 + 128, n * 512:(n + 1) * 512], in_=ms),
                     reads=["ms%d" % n], writes=["mix%d_%d" % (row0 // 128, n)], dma=True)

        emit_qk(0)
        emit_qk(1)
        for s in range(len(steps)):
            emit_pv(s)
            if s + 2 < len(steps):
                emit_qk(s + 2)

    def layer(self, l):
        P = self.P
        W = self.W[l]
        self.gpar = 0
        self.tmb = 0
        self.norm(P_NORM + 32 * l)
        P.barrier()
        if l == 0:
            self.mix_even(W["w_in"])
        else:
            self.mix_odd(W["w_in"])
        P.barrier()
        self.A.off = self.mark
        if self.dbg in ("s2", "s3", "s5", "s6"):
            return
        P.op(SP, lambda e: e.dma_start(out=self.XN[:, 0:8, :],
                                       in_=self.mixd[0:1024, :].rearrange("(c p) t -> p c t", p=128)),
             writes=["xn%d" % c for c in range(8)], dma=True)
        P.op(SP, lambda e: e.dma_start(out=self.XN[:, 8:16, :],
                                       in_=self.mixd[1024:2048, :].rearrange("(c p) t -> p c t", p=128)),
             writes=["xn%d" % c for c in range(8, 16)], dma=True)
        self.gemm_fm(W["w_out"], 0, 0, [128] * 16, self.xn_rhs, lambda k: "xn%d" % k, self.evac_resid(0))
        P.barrier()
        self.norm(P_NORM + 32 * l + 16)
        aT = self.A.alloc([NCH, S], BF16)
        for fg in range(4):
            def evac_up(mi, h, aT=aT):
                hs, hk = self.HS[mi % 2], "HS%d" % (mi % 2)
                P.op(ACT, lambda e, h=h, hs=hs: e.activation(out=hs, in_=self.banks4(h), func=AF.Relu),
                     reads=self.pk(h), writes=[hk])
                P.op(DVE, lambda e, mi=mi, hs=hs: e.tensor_tensor(out=aT[:, mi, :], in0=hs, in1=hs, op=ALU.mult),
                     reads=[hk], writes=["aT%d" % mi])
            self.gemm_fm(W["w_up"], 0, fg * 2048, [128] * 16, self.xn_rhs, lambda k: "xn%d" % k, evac_up)
            self.gemm_fm(W["w_down"], fg * 2048, 0, [128] * 16,
                         lambda k, n, aT=aT: aT[:, k, n * 512:(n + 1) * 512], lambda k: "aT%d" % k,
                         self.evac_resid(0))
        P.barrier()
        self.A.off = self.mark

    def evac_resid(self, _):
        P = self.P

        def pre(mi):
            hs, hk = self.HS[mi % 2], "HS%d" % (mi % 2)
            P.op(SP, lambda e, mi=mi, hs=hs: e.dma_start(out=hs, in_=self.hT[mi * 128:(mi + 1) * 128, :]),
                 reads=["hT%d" % mi], writes=[hk], dma=True)

        def ev(mi, h):
            hs, hk = self.HS[mi % 2], "HS%d" % (mi % 2)
            if mi < 2:
                pre(mi)
            P.op(DVE, lambda e, h=h, hs=hs: e.tensor_tensor(out=hs, in0=self.banks4(h), in1=hs, op=ALU.add),
                 reads=self.pk(h) + [hk], writes=[hk])
            P.op(SP, lambda e, mi=mi, hs=hs: e.dma_start(out=self.hT[mi * 128:(mi + 1) * 128, :], in_=hs),
                 reads=[hk], writes=["hT%d" % mi], dma=True)
            if mi + 2 < NCH:
                pre(mi + 2)
        return ev

    def mix_even(self, w_in):
        P, A = self.P, self.A
        xk = lambda k: "xn%d" % k
        glrT = self.HS[0]
        def ev_glr(mi, h):
            P.op(ACT, lambda e, h=h: e.activation(out=glrT[0:16, :], in_=self.banks4(h)[0:16, :], func=AF.Copy),
                 reads=self.pk(h), writes=["glr"])
        self.gemm_fm(w_in, 0, 3072, [16], self.xn_rhs, xk, ev_glr)
        m0 = A.off
        eb, enb, ebl = A.alloc([S]), A.alloc([S]), A.alloc([S])
        qg, kg = A.alloc([S], BF16), A.alloc([S], BF16)
        kgl = self.SQB[:, 0, :]
        c3 = lambda a: a.rearrange("p (c j) -> p c j", j=64)
        kgtm = A.alloc([NCH, 128], BF16)
        vtm = A.alloc([NCH, 256], BF16)
        sg = A.alloc([2, S], BF16)
        oT = A.alloc([2, S])
        St = A.alloc([256])
        Sbf = [A.alloc([256], BF16) for _ in range(2)]
        PT = [A.alloc([128], BF16) for _ in range(2)]
        cs = self.RSTD
        for hd in ([] if self.dbg == "s3" else [0] if self.dbg == "s2" else range(4)):
            for n in range(4):
                P.op(PE, lambda e, hd=hd, n=n: e.matmul(self.bank(n), lhsT=self.w2[0:16, hd * 128:(hd + 1) * 128],
                                                       rhs=glrT[0:16, n * 512:(n + 1) * 512], start=True, stop=True),
                     reads=["w2", "glr"], writes=["pb%d" % n])
            nb = self.lbt[:, 16 + hd:17 + hd]
            P.op(DVE, lambda e, hd=hd, nb=nb: e.tensor_scalar(out=nb, in0=self.prm[:, P_GB + hd:P_GB + hd + 1],
                                                             scalar1=-1.0, scalar2=None, op0=ALU.mult),
                 reads=["prm"], writes=["nb"])
            P.op(ACT, lambda e, nb=nb: e.activation(out=eb, in_=self.banks4(0), func=AF.Exp, scale=-1.0, bias=nb),
                 reads=self.pk(0) + ["nb"], writes=["eb"])
            P.op(ACT, lambda e: e.activation(out=eb, in_=eb, func=AF.Ln, bias=1.0), reads=["eb"], writes=["eb"])
            P.op(DVE, lambda e: e.tensor_tensor_scan(out=cs, data0=self.RM, data1=eb, initial=0.0,
                                                     op0=ALU.mult, op1=ALU.add),
                 reads=["RM", "eb"], writes=["RSTD"])
            P.op(ACT, lambda e: e.activation(out=eb, in_=cs, func=AF.Exp, scale=-1.0 / 16), reads=["RSTD"], writes=["eb"])
            P.op(ACT, lambda e: e.activation(out=enb, in_=cs, func=AF.Exp, scale=1.0 / 16), reads=["RSTD"], writes=["enb"])
            P.op(DVE, lambda e: e.tensor_tensor(out=c3(ebl), in0=c3(enb), in1=c3(eb)[:, :, 63:64].to_broadcast([128, 32, 64]),
                                                op=ALU.mult), reads=["enb", "eb"], writes=["ebl"])

            def ev_q(mi, h):
                P.op(DVE, lambda e, h=h: e.scalar_tensor_tensor(out=qg, in0=self.banks4(h), scalar=128 ** -0.5, in1=eb,
                                                                op0=ALU.mult, op1=ALU.mult),
                     reads=self.pk(h) + ["eb"], writes=["qg"])

            def ev_k(mi, h):
                P.op(DVE, lambda e, h=h: e.tensor_tensor(out=kg, in0=self.banks4(h), in1=enb, op=ALU.mult),
                     reads=self.pk(h) + ["enb"], writes=["kg"])
                P.op(DVE, lambda e, h=h: e.tensor_tensor(out=kgl, in0=self.banks4(h), in1=ebl, op=ALU.mult),
                     reads=self.pk(h) + ["ebl"], writes=["SQB0"])

            def ev_g(mi, h):
                P.op(ACT, lambda e, mi=mi, h=h: e.activation(out=sg[:, mi, :], in_=self.banks4(h), func=AF.Silu),
                     reads=self.pk(h), writes=["sg"])
            self.gemm_fm(w_in, 0, hd * 128, [128], self.xn_rhs, xk, ev_q)
            self.gemm_fm(w_in, 0, 512 + hd * 128, [128], self.xn_rhs, xk, ev_k)
            self.gemm_fm(w_in, 0, 2048 + hd * 256, [128, 128], self.xn_rhs, xk, ev_g)
            self.gemm_tm(w_in, 1024 + hd * 256, 256, vtm, "vtm")
            self.transpose_kg(kgl, kgtm)
            self.gla_core(qg, kg, kgtm, vtm, eb, 2, oT, St, Sbf, PT)
            self.gated_out(oT, sg, 2, P_GN, hd * 256)
        P.barrier()
        A.off = m0
        qT, kT = A.alloc([S], BF16), A.alloc([S], BF16)
        vt = A.alloc([NCH, 128], BF16)
        Pb = [A.alloc([512], BF16) for _ in range(3)]
        osb = [A.alloc([512]) for _ in range(2)]
        Gm = A.alloc([NCH, 8])
        m8 = A.alloc([NCH, 8])
        sel = A.alloc([NCH, 8])
        ksum = A.alloc([8])
        kmT = A.alloc([8], BF16)
        Eh = A.alloc([8, 128], BF16)
        pm = self.cf[:, C_PM:C_PM + 128].rearrange("p (a b) -> p a b", a=NCH)
        p01 = self.cf[:, C_P01:C_P01 + 128].rearrange("p (a b) -> p a b", a=NCH)
        o01 = self.cf[:, C_O01:C_O01 + 128].rearrange("p (a b) -> p a b", a=NCH)
        for hd in ([] if self.dbg == "s2" else [0, 5] if self.dbg == "s3" else range(8)):
            slope = 2.0 ** (-(hd + 1))

            def ev_q(mi, h):
                P.op(DVE, lambda e, h=h: e.tensor_scalar(out=qT, in0=self.banks4(h), scalar1=128 ** -0.5, scalar2=None,
                                                         op0=ALU.mult),
                     reads=self.pk(h), writes=["qT"])

            def ev_k(mi, h):
                for n in range(8):
                    P.op(DVE, lambda e, h=h, n=n: e.tensor_scalar(
                        out=kT[:, n * 256:(n + 1) * 256], in0=self.banks4(h)[:, n * 256:(n + 1) * 256], scalar1=1.0,
                        scalar2=None, op0=ALU.mult, op1=ALU.add, accum_out=ksum[:, n:n + 1]),
                        reads=self.pk(h), writes=["kT", "ksum"])
                P.op(DVE, lambda e: e.tensor_scalar(out=kmT, in0=ksum, scalar1=1.0 / 256, scalar2=None, op0=ALU.mult),
                     reads=["ksum"], writes=["kmT"])
            self.gemm_fm(w_in, 0, 3088 + hd * 128, [128], self.xn_rhs, xk, ev_q)
            self.gemm_fm(w_in, 0, 4112 + hd * 128, [128], self.xn_rhs, xk, ev_k)
            self.gemm_tm(w_in, 5136 + hd * 128, 128, vt, "vtm")
            import os
            stop = os.environ.get("MOBA_STOP", "")
            if stop in ("a", "a0"):
                continue
            for i in range(NCH):
                P.op(PE, lambda e, i=i: e.matmul(self.bank(0, 8, i * 8), lhsT=qT[:, i * 128:(i + 1) * 128], rhs=kmT,
                                                start=True, stop=True),
                     reads=["qT", "kmT"], writes=["pb0"])
            P.op(DVE, lambda e: e.tensor_tensor(out=Gm, in0=self.bank(0, 128).rearrange("p (a b) -> p a b", a=NCH),
                                                in1=pm, op=ALU.add),
                 reads=["pb0", "cf"], writes=["Gm"])
            for i in range(NCH):
                P.op(DVE, lambda e, i=i: e.max(out=m8[:, i, :], in_=Gm[:, i, :]), reads=["Gm"], writes=["m8"])
            P.op(DVE, lambda e: e.tensor_tensor(out=sel, in0=Gm, in1=m8[:, :, 2:3].to_broadcast([128, NCH, 8]),
                                                op=ALU.is_ge),
                 reads=["Gm", "m8"], writes=["sel"])
            P.op(DVE, lambda e: e.tensor_tensor(out=sel, in0=sel, in1=p01, op=ALU.mult),
                 reads=["sel", "cf"], writes=["sel"])
            P.op(DVE, lambda e: e.tensor_tensor(out=sel, in0=sel, in1=o01, op=ALU.add),
                 reads=["sel", "cf"], writes=["sel"])
            P.op(DVE, lambda e: e.tensor_scalar(out=sel, in0=sel, scalar1=-1.0, scalar2=-NEG, op0=ALU.add, op1=ALU.mult),
                 reads=["sel"], writes=["sel"])
            if stop == "b":
                continue
            for i in range(NCH):
                P.op(PE, lambda e, i=i: e.transpose(self.bank(4 + i // 4, 128, (i % 4) * 128)[0:8, :],
                                                   sel[:, i, :], self.ident_f),
                     reads=["sel", "cf"], writes=["pb%d" % (4 + i // 4)])
            P.op(ACT, lambda e: e.activation(out=self.ROWM[0:8, :], in_=self.banks4(1)[0:8, :], func=AF.Copy),
                 reads=self.pk(1), writes=["ROWM"])
            P.op(DVE, lambda e: e.tensor_copy(out=Eh[0:64], in_=self.EM[0:64]), reads=["EM"], writes=["Eh"])
            P.op(DVE, lambda e, slope=slope: e.tensor_scalar(out=Eh[32:34], in0=self.EM[32:34], scalar1=slope,
                                                            scalar2=None, op0=ALU.mult),
                 reads=["EM", "Eh"], writes=["Eh"])
            if stop == "c":
                continue
            self.attention(qT, kT, vt,
                           lambda j, hd=hd: self.cf[:, C_ALC + hd * 16 + j:C_ALC + hd * 16 + j + 1], "cf",
                           lambda j: Eh[0:64, j // 2, :], self.ROWM, 64, ["Eh", "ROWM"], Pb, osb, 1024 + hd * 128)
        P.barrier()
        A.off = m0

    def mix_odd(self, w_in):
        P, A = self.P, self.A
        xk = lambda k: "xn%d" % k
        m0 = A.off
        lbt = self.lbt
        P.op(ACT, lambda e: e.activation(out=lbt[:, 0:16], in_=self.prm[:, P_LB:P_LB + 16], func=AF.Exp),
             reads=["prm"], writes=["lbt"])
        P.op(DVE, lambda e: e.tensor_tensor(out=lbt[:, 0:8], in0=lbt[:, 0:8], in1=lbt[:, 8:16], op=ALU.add),
             reads=["lbt"], writes=["lbt"])
        P.op(DVE, lambda e: e.reciprocal(out=lbt[:, 0:8], in_=lbt[:, 0:8]), reads=["lbt"], writes=["lbt"])
        P.op(DVE, lambda e: e.tensor_tensor(out=lbt[:, 8:16], in0=lbt[:, 8:16], in1=lbt[:, 0:8], op=ALU.mult),
             reads=["lbt"], writes=["lbt"])
        P.op(DVE, lambda e: e.tensor_scalar(out=lbt[:, 0:8], in0=lbt[:, 8:16], scalar1=-1.0, scalar2=1.0,
                                            op0=ALU.mult, op1=ALU.add),
             reads=["lbt"], writes=["lbt"])
        lf = self.HS[0]
        cs = self.RSTD
        cstm = A.alloc([NCH, 8])
        tmp = A.alloc([S])
        nfb = lbt[:, 24:25]
        P.op(DVE, lambda e: e.tensor_scalar(out=nfb[0:8], in0=self.prm[0:8, P_FB:P_FB + 1], scalar1=-1.0, scalar2=None,
                                            op0=ALU.mult), reads=["prm"], writes=["nfb"])

        def ev_f(mi, h):
            P.op(ACT, lambda e, h=h: e.activation(out=lf[0:8, :], in_=self.banks4(h)[0:8, :], func=AF.Exp, scale=-1.0,
                                                  bias=nfb[0:8]), reads=self.pk(h) + ["nfb"], writes=["lf"])
        self.gemm_fm(w_in, 0, 3072, [8], self.xn_rhs, xk, ev_f)
        P.op(ACT, lambda e: e.activation(out=lf[0:8, :], in_=lf[0:8, :], func=AF.Ln, bias=1.0), reads=["lf"], writes=["lf"])
        P.op(DVE, lambda e: e.tensor_tensor_scan(out=cs[0:8, :], data0=self.cbf[0:8, 384:385].to_broadcast([8, S]),
                                                 data1=lf[0:8, :], initial=0.0, op0=ALU.mult, op1=ALU.add),
             reads=["cbf", "lf"], writes=["RSTD"])
        P.op(DVE, lambda e: e.tensor_copy(out=self.ROWF[0:8, :], in_=cs[0:8, :]), reads=["RSTD"], writes=["ROWF"])
        P.op(DVE, lambda e: e.tensor_tensor(out=tmp[0:8, :], in0=cs[0:8, :], in1=self.ROWF[0:8, :], op=ALU.subtract),
             reads=["RSTD", "ROWF"], writes=["tmp"])
        P.op(SP, lambda e: e.dma_start(out=tmp[32:40, :], in_=tmp[0:8, :]), reads=["tmp"], writes=["tmp32"], dma=True)
        P.op(DVE, lambda e: e.tensor_copy(out=self.ROWF[32:40, :], in_=tmp[32:40, :]), reads=["tmp32"], writes=["ROWF"])
        P.op(DVE, lambda e: e.tensor_tensor(out=tmp[32:40, :], in0=tmp[32:40, :], in1=self.ROWF[32:40, :],
                                            op=ALU.subtract), reads=["tmp32", "ROWF"], writes=["tmp32"])
        P.op(SP, lambda e: e.dma_start(out=tmp[64:72, :], in_=tmp[32:40, :]), reads=["tmp32"], writes=["tmp64"], dma=True)
        P.op(DVE, lambda e: e.tensor_copy(out=self.ROWF[64:72, :], in_=tmp[64:72, :]), reads=["tmp64"], writes=["ROWF"])
        for i in range(NCH):
            P.op(PE, lambda e, i=i: e.transpose(self.bank(0, 8, i * 8), cs[0:8, i * 128:(i + 1) * 128],
                                               self.ident_f[0:8, 0:8]),
                 reads=["RSTD", "cf"], writes=["pb0"])
        P.op(DVE, lambda e: e.tensor_copy(out=cstm, in_=self.bank(0, 128).rearrange("p (a b) -> p a b", a=NCH)),
             reads=["pb0"], writes=["bcol"])
        qT, kT = A.alloc([S], BF16), A.alloc([S], BF16)
        vt = A.alloc([NCH, 128], BF16)
        Pb = [A.alloc([512], BF16) for _ in range(3)]
        osb = [A.alloc([512]) for _ in range(2)]
        for hd in range(8):
            def ev_q(mi, h):
                P.op(DVE, lambda e, h=h: e.tensor_scalar(out=qT, in0=self.banks4(h), scalar1=128 ** -0.5, scalar2=None,
                                                         op0=ALU.mult),
                     reads=self.pk(h), writes=["qT"])

            def ev_k(mi, h):
                P.op(ACT, lambda e, h=h: e.activation(out=kT, in_=self.banks4(h), func=AF.Copy),
                     reads=self.pk(h), writes=["kT"])
            self.gemm_fm(w_in, 0, hd * 128, [128], self.xn_rhs, xk, ev_q)
            self.gemm_fm(w_in, 0, 1024 + hd * 128, [128], self.xn_rhs, xk, ev_k)
            self.gemm_tm(w_in, 2048 + hd * 128, 128, vt, "vtm")
            self.attention(qT, kT, vt, lambda j, hd=hd: cstm[:, j, hd:hd + 1], "bcol",
                           lambda j, hd=hd: self.EF[:, hd, :], self.ROWF, 128, ["EF", "ROWF"], Pb, osb, hd * 128)
        P.barrier()
        A.off = m0
        F1, eb, enb, ebl = A.alloc([S]), A.alloc([S]), A.alloc([S]), A.alloc([S])
        qg, kg = A.alloc([S], BF16), A.alloc([S], BF16)
        kgl = self.SQB[:, 0, :]
        c3 = lambda a: a.rearrange("p (c j) -> p c j", j=64)
        kgtm = A.alloc([NCH, 128], BF16)
        vtm = A.alloc([NCH, 128], BF16)
        sg = A.alloc([1, S], BF16)
        oT = A.alloc([1, S])
        St = A.alloc([128])
        Sbf = [A.alloc([128], BF16) for _ in range(2)]
        PT = [A.alloc([128], BF16) for _ in range(2)]
        for hd in range(8):
            lb = lbt[:, 8 + hd:9 + hd]
            oml = lbt[:, hd:hd + 1]

            def ev_f(mi, h, oml=oml, lb=lb):
                P.op(ACT, lambda e, h=h: e.activation(out=F1, in_=self.banks4(h), func=AF.Sigmoid),
                     reads=self.pk(h), writes=["F1"])
                P.op(DVE, lambda e, oml=oml, lb=lb: e.tensor_scalar(out=F1, in0=F1, scalar1=oml, scalar2=lb,
                                                                   op0=ALU.mult, op1=ALU.add),
                     reads=["F1", "lbt"], writes=["F1"])
                P.op(ACT, lambda e: e.activation(out=eb, in_=F1, func=AF.Ln), reads=["F1"], writes=["eb"])
                P.op(DVE, lambda e: e.tensor_tensor_scan(out=cs, data0=self.RM, data1=eb, initial=0.0,
                                                         op0=ALU.mult, op1=ALU.add),
                     reads=["RM", "eb"], writes=["RSTD"])
                P.op(ACT, lambda e: e.activation(out=enb, in_=cs, func=AF.Exp, scale=-1.0), reads=["RSTD"], writes=["enb"])
                P.op(ACT, lambda e: e.activation(out=eb, in_=cs, func=AF.Exp), reads=["RSTD"], writes=["eb"])
                P.op(DVE, lambda e: e.tensor_scalar(out=F1, in0=F1, scalar1=-1.0, scalar2=1.0, op0=ALU.mult, op1=ALU.add),
                     reads=["F1"], writes=["F1"])
                P.op(DVE, lambda e: e.tensor_tensor(out=kg, in0=F1, in1=enb, op=ALU.mult),
                     reads=["F1", "enb"], writes=["kg"])
                P.op(DVE, lambda e: e.tensor_tensor(out=c3(ebl), in0=c3(enb),
                                                    in1=c3(eb)[:, :, 63:64].to_broadcast([128, 32, 64]), op=ALU.mult),
                     reads=["enb", "eb"], writes=["ebl"])
                P.op(DVE, lambda e: e.tensor_tensor(out=kgl, in0=F1, in1=ebl, op=ALU.mult),
                     reads=["F1", "ebl"], writes=["SQB0"])

            def ev_q(mi, h):
                P.op(ACT, lambda e, h=h: e.activation(out=F1, in_=self.banks4(h), func=AF.Silu),
                     reads=self.pk(h), writes=["F1"])
                P.op(DVE, lambda e: e.tensor_tensor(out=qg, in0=F1, in1=eb, op=ALU.mult),
                     reads=["F1", "eb"], writes=["qg"])

            def ev_g(mi, h):
                P.op(ACT, lambda e, h=h: e.activation(out=sg[:, 0, :], in_=self.banks4(h), func=AF.Silu),
                     reads=self.pk(h), writes=["sg"])
            self.gemm_fm(w_in, 0, 4104 + hd * 128, [128], self.xn_rhs, xk, ev_f)
            self.gemm_fm(w_in, 0, 3080 + hd * 128, [128], self.xn_rhs, xk, ev_q)
            self.gemm_fm(w_in, 0, 6152 + hd * 128, [128], self.xn_rhs, xk, ev_g)
            self.gemm_tm(w_in, 5128 + hd * 128, 128, vtm, "vtm")
            self.transpose_kg(kgl, kgtm)
            self.gla_core(qg, kg, kgtm, vtm, eb, 1, oT, St, Sbf, PT)
            self.gated_out(oT, sg, 1, P_HN, 1024 + hd * 128)
        P.barrier()
        A.off = m0


def _consts():
    c = np.zeros((128, NCST), np.float32)
    p = np.arange(128)
    c[:, C_ID:C_ID + 128] = np.eye(128, dtype=np.float32)
    s, t = p[:, None], p[None, :]
    c[:, C_UB:C_UB + 128] = ((s <= t) & (s // 64 == t // 64)).astype(np.float32)
    c[:, C_CM:C_CM + 128] = np.where(s <= t, 0.0, NEG).astype(np.float32)
    c[:, C_ONE:C_ONE + 128] = 1.0
    for h in range(8):
        for j in range(16):
            c[:, C_ALC + h * 16 + j] = (2.0 ** (-(h + 1))) * (128 * j + p)
    for i in range(16):
        for n in range(8):
            c[:, C_PM + i * 8 + n] = 0.0 if n < i // 2 else -1e30
            c[:, C_P01 + i * 8 + n] = 1.0 if n < i // 2 else 0.0
            c[:, C_O01 + i * 8 + n] = 1.0 if n == i // 2 else 0.0
    for n in range(8):
        c[n, C_EM + n * 128:C_EM + (n + 1) * 128] = 1.0
        c[32:34, C_EM + n * 128:C_EM + (n + 1) * 128] = 1.0
        for r in (n, 32 + n, 64 + n):
            c[r, C_EF + n * 128:C_EF + (n + 1) * 128] = -1.0
    tt = np.arange(S)
    c[32, C_AR:C_AR + S] = -128.0 * (tt // 128)
    c[33, C_AR:C_AR + S] = -(tt % 128).astype(np.float32)
    c[:, C_RM:C_RM + S] = (tt % 64 != 0).astype(np.float32)[None, :]
    return c


def _params(inp):
    pr = np.zeros((128, NPAR), np.float32)
    fm = lambda v: np.asarray(v, np.float32).reshape(-1, 128).T
    pr[:, 0:16] = fm(inp["ev_norm_mix"][0])
    pr[:, 16:32] = fm(inp["ev_norm_mlp"][0])
    pr[:, 32:48] = fm(inp["od_norm_mix"][0])
    pr[:, 48:64] = fm(inp["od_norm_mlp"][0])
    pr[:, 64:80] = fm(inp["final_norm"])
    pr[:, P_GB:P_GB + 4] = fm(inp["ev_gla_gate_b"][0])
    pr[:, P_GN:P_GN + 2] = fm(inp["ev_gla_out_norm"][0])
    pr[:, P_HN:P_HN + 1] = fm(inp["od_hgrn_out_norm"][0])
    pr[:, P_LB:P_LB + 8] = fm(inp["hgrn_lb_raw"][0])
    pr[:, P_LB + 8:P_LB + 16] = fm(inp["hgrn_lb_raw"][1])
    pr[0:8, P_FB] = np.asarray(inp["od_fox_fgate_b"][0], np.float32)
    return pr


_NC_CACHE = {}


def make_in_maps(inp, ncores=8):
    cst = _consts()
    prm = _params(inp)
    shared = {"w2": np.ascontiguousarray(np.asarray(inp["ev_gla_gate_w2"][0], np.float32)), "prm": prm, "cst": cst}
    for p in ("ev", "od"):
        for w in ("w_in", "w_out", "w_up", "w_down"):
            shared["%s_%s" % (p, w)] = np.ascontiguousarray(np.asarray(inp["%s_%s" % (p, w)][0], np.float32))
    x = np.asarray(inp["x"], np.float32)
    maps = []
    for b in range(ncores):
        m = dict(shared)
        m["xT"] = np.ascontiguousarray(x[b].T)
        maps.append(m)
    return maps


def kernel(**inputs):
    if "nc" not in _NC_CACHE:
        _NC_CACHE["nc"] = Builder().run()
    nc = _NC_CACHE["nc"]
    maps = make_in_maps(inputs)
    res = run_bass_kernel_spmd(nc, maps, core_ids=list(range(8)))
    out = np.stack([np.asarray(r["outT"]).T for r in res.results], axis=0)
    return np.ascontiguousarray(out.astype(np.float32))
```

```python
import numpy as np
from contextlib import ExitStack
import concourse.bass as bass
import concourse.mybir as mybir
from concourse.bass_utils import run_bass_kernel_spmd

F32 = mybir.dt.float32
BF16 = mybir.dt.bfloat16
AF = mybir.ActivationFunctionType
ALU = mybir.AluOpType
AX = mybir.AxisListType

S = 2048
D = 2048
DFF = 8192
NCH = 16
EPS = 1e-6
NIN = (6160, 7176)
NEG = -30000.0

PE, ACT, DVE, POOL, SP = "pe", "act", "dve", "pool", "sp"
ENGS = [PE, ACT, DVE, POOL, SP]
NDMA_SEM = 8

C_ID, C_UB, C_CM, C_ONE, C_ALC, C_PM, C_P01, C_O01 = 0, 128, 256, 384, 512, 640, 768, 896
C_EM, C_EF, C_AR, C_RM, NCST = 1024, 2048, 3072, 5120, 7168
P_NORM = 0
P_GB = 80
P_GN = 84
P_HN = 86
P_LB = 87
P_FB = 103
NPAR = 104


class Op:
    __slots__ = ("eng", "fn", "deps", "dma", "idx", "signal", "cnt", "dsem", "dval", "prev")

    def __init__(self, eng, fn, dma):
        self.eng, self.fn, self.dma = eng, fn, dma
        self.deps = set()
        self.signal = False
        self.cnt = 0
        self.dsem = None
        self.dval = 0
        self.prev = None


class Prog:
    def __init__(self, nc):
        self.nc = nc
        self.ops = []
        self.lastw = {}
        self.readers = {}
        self.last_eng = {}
        self.dmas_since = []

    def op(self, eng, fn, reads=(), writes=(), dma=False):
        o = Op(eng, fn, dma)
        o.idx = len(self.ops)
        pr = [r for r in reads if r.startswith("pb")]
        if pr:
            reads = [r for r in reads if not r.startswith("pb")]
            writes = list(writes) + pr
        for r in reads:
            w = self.lastw.get(r)
            if w is not None:
                o.deps.add(w)
        for r in writes:
            w = self.lastw.get(r)
            if w is not None:
                o.deps.add(w)
            for rd in self.readers.get(r, ()):
                o.deps.add(rd)
        for r in writes:
            self.lastw[r] = o.idx
            self.readers[r] = []
        for r in reads:
            if r not in writes:
                self.readers.setdefault(r, []).append(o.idx)
        o.deps.discard(o.idx)
        self.ops.append(o)
        if dma:
            self.dmas_since.append(o.idx)
        else:
            self.last_eng[eng] = o.idx
        return o

    def barrier(self):
        deps = set(self.last_eng.values()) | set(self.dmas_since)
        for e in ENGS:
            o = Op(e, None, False)
            o.idx = len(self.ops)
            o.deps = set(deps)
            self.ops.append(o)
        self.lastw.clear()
        self.readers.clear()
        self.dmas_since = []

    def emit(self, stack):
        nc = self.nc
        ops = self.ops
        for o in ops:
            best = {}
            nd = set()
            for d in o.deps:
                p = ops[d]
                if p.dma:
                    nd.add(d)
                    continue
                if p.eng == o.eng and o.eng == PE and not o.dma:
                    continue
                if p.eng not in best or best[p.eng] < d:
                    best[p.eng] = d
            nd |= set(best.values())
            o.deps = nd
            for d in nd:
                ops[d].signal = True
        sems = {e: stack.enter_context(nc.semaphore("s_" + e)) for e in ENGS}
        dsems = {e: [stack.enter_context(nc.semaphore("d_%s_%d" % (e, i))) for i in range(NDMA_SEM)]
                 for e in (ACT, POOL, SP)}
        cnt = {e: 0 for e in ENGS}
        per = {e: [] for e in ENGS}
        hist = {e: [] for e in ENGS}
        for o in ops:
            if o.dma:
                j = len(hist[o.eng])
                o.dsem = dsems[o.eng][j % NDMA_SEM]
                o.dval = 16 * (j // NDMA_SEM + 1)
                if j >= NDMA_SEM:
                    o.prev = hist[o.eng][j - NDMA_SEM]
                hist[o.eng].append(o)
            elif o.signal:
                cnt[o.eng] += 1
                o.cnt = cnt[o.eng]
            per[o.eng].append(o)
        self.stats = {e: len(per[e]) for e in ENGS}
        self.stats["sig"] = dict(cnt)
        block = stack.enter_context(nc.Block())

        def run(eng_name, e):
            waited = {}

            def wait(sem, val):
                k = id(sem)
                if waited.get(k, 0) >= val:
                    return
                waited[k] = val
                e.wait_ge(sem, val)

            for o in per[eng_name]:
                need = {}
                for d in o.deps:
                    p = ops[d]
                    s, v = (p.dsem, p.dval) if p.dma else (sems[p.eng], p.cnt)
                    if id(s) not in need or need[id(s)][1] < v:
                        need[id(s)] = (s, v)
                if o.dma and o.prev is not None:
                    s, v = o.prev.dsem, o.prev.dval
                    if id(s) not in need or need[id(s)][1] < v:
                        need[id(s)] = (s, v)
                for s, v in need.values():
                    wait(s, v)
                if o.fn is None:
                    continue
                ins = o.fn(e)
                if o.dma:
                    ins.then_inc(o.dsem, 16)
                elif o.signal:
                    ins.then_inc(sems[o.eng], 1)
            for o in hist[eng_name][-NDMA_SEM:]:
                wait(o.dsem, o.dval)

        block.tensor(lambda e: run(PE, e))
        block.scalar(lambda e: run(ACT, e))
        block.vector(lambda e: run(DVE, e))
        block.gpsimd(lambda e: run(POOL, e))
        block.sync(lambda e: run(SP, e))


class Arena:
    def __init__(self, ap, nwords):
        self.ap = ap
        self.n = nwords
        self.off = 0

    def alloc(self, shape, dtype=F32, parts=128):
        n = int(np.prod(shape))
        words = n if dtype == F32 else (n + 1) // 2
        words = (words + 7) // 8 * 8
        assert self.off + words <= self.n, ("arena overflow", self.off, words, self.n)
        v = self.ap[0:parts, self.off:self.off + words]
        self.off += words
        if dtype != F32:
            v = v.bitcast(dtype)
        v = v[:, 0:n]
        if len(shape) == 2:
            v = v.rearrange("p (a b) -> p a b", a=shape[0])
        elif len(shape) == 3:
            v = v.rearrange("p (a b c) -> p a b c", a=shape[0], b=shape[1])
        return v


class Builder:
    def __init__(self, dbg=None):
        self.dbg = dbg
        nc = self.nc = bass.Bass("TRN2", target_bir_lowering=False)
        self.P = Prog(nc)

        def di(name, shape, dt=F32):
            return nc.dram_tensor(name, shape, dt, kind="ExternalInput").ap()

        self.xT = di("xT", [D, S])
        self.W = []
        for l, p in ((0, "ev"), (1, "od")):
            self.W.append(dict(w_in=di(p + "_w_in", [D, NIN[l]]), w_out=di(p + "_w_out", [D, D]),
                               w_up=di(p + "_w_up", [D, DFF]), w_down=di(p + "_w_down", [DFF, D])))
        self.w2d = di("w2", [16, 512])
        self.prmd = di("prm", [128, NPAR])
        self.cstd = di("cst", [128, NCST])
        self.outT = nc.dram_tensor("outT", [D, S], F32, kind="ExternalOutput").ap()
        self.hT = nc.dram_tensor("hT", [D, S], F32).ap()
        self.mixd = nc.dram_tensor("mixd", [D, S], BF16, kind=("ExternalOutput" if dbg in ("mix", "s2", "s3", "s4", "s5", "s6") else "Internal")).ap()
        if dbg == "h":
            self.hdbg = nc.dram_tensor("hdbg", [D, S], F32, kind="ExternalOutput").ap()

    def bank(self, b, n=512, c0=0):
        return self.PS[:, 512 * b + c0:512 * b + c0 + n]

    def banks4(self, h):
        return self.PS[:, 2048 * h:2048 * h + 2048]

    def pk(self, h):
        return ["pb%d" % b for b in range(4 * h, 4 * h + 4)]

    def run(self):
        nc, P = self.nc, self.P
        with ExitStack() as st:
            NW = 53000
            arena_t = st.enter_context(nc.sbuf_tensor("arena", [128, NW], F32))
            self.PS = st.enter_context(nc.psum_tensor("ps", [128, 4096], F32))
            A = self.A = Arena(arena_t, NW)
            self.XN = A.alloc([NCH, S], BF16)
            self.WB = [A.alloc([NCH, 256], BF16) for _ in range(2)]
            self.HS = [A.alloc([S], F32) for _ in range(2)]
            self.RSTD = A.alloc([S], F32)
            self.SQB = A.alloc([2, S], BF16)
            self.cbf = A.alloc([512], BF16)
            self.EM = A.alloc([8, 128], BF16)
            self.EF = A.alloc([8, 128], BF16)
            self.ROWM = A.alloc([S], BF16)
            self.ROWF = A.alloc([S], BF16)
            self.RM = A.alloc([S], BF16)
            self.cf = A.alloc([1024], F32)
            self.prm = A.alloc([NPAR], F32)
            self.w2 = A.alloc([512], F32)
            self.lbt = A.alloc([32], F32)
            self.mark = A.off
            self.wslot = 0
            self.ident_bf = self.cbf[:, 0:128]
            self.ublk_bf = self.cbf[:, 128:256]
            self.cm_bf = self.cbf[:, 256:384]
            self.ones_bf = self.cbf[:, 384:512]
            self.ident_f = self.cf[:, C_ID:C_ID + 128]
            self.ublk_f = self.cf[:, C_UB:C_UB + 128]

            self.load_consts()
            self.copy_x()
            if self.dbg != "s1":
                self.layer(0)
            if self.dbg not in ("l0", "mix", "s1", "s2", "s3", "s4"):
                self.layer(1)
            self.final_norm()
            P.emit(st)
        return nc

    def load_consts(self):
        P, c = self.P, self.cstd
        P.op(POOL, lambda e: e.dma_start(out=self.cbf, in_=c[:, 0:512]), writes=["cbf"], dma=True)
        P.op(POOL, lambda e: e.dma_start(out=self.EM.rearrange("p a b -> p (a b)"), in_=c[:, C_EM:C_EM + 1024]),
             writes=["EM"], dma=True)
        P.op(POOL, lambda e: e.dma_start(out=self.EF.rearrange("p a b -> p (a b)"), in_=c[:, C_EF:C_EF + 1024]),
             writes=["EF"], dma=True)
        P.op(POOL, lambda e: e.dma_start(out=self.ROWM, in_=c[:, C_AR:C_AR + S]), writes=["ROWM"], dma=True)
        P.op(POOL, lambda e: e.dma_start(out=self.RM, in_=c[:, C_RM:C_RM + S]), writes=["RM"], dma=True)
        P.op(SP, lambda e: e.dma_start(out=self.cf, in_=c[:, 0:1024]), writes=["cf"], dma=True)
        P.op(SP, lambda e: e.dma_start(out=self.prm, in_=self.prmd), writes=["prm"], dma=True)
        P.op(SP, lambda e: e.dma_start(out=self.w2[0:16, :], in_=self.w2d), writes=["w2"], dma=True)
        P.op(DVE, lambda e: e.memset(self.ROWF, 0.0), writes=["ROWF"])

    def copy_x(self):
        for c in range(NCH):
            self.P.op(SP, lambda e, c=c: e.dma_start(out=self.hT[c * 128:(c + 1) * 128, :],
                                                      in_=self.xT[c * 128:(c + 1) * 128, :]),
                      writes=["hT%d" % c], dma=True)

    def norm(self, gcol, final=False):
        P, A = self.P, self.A
        m0 = A.off
        NBUF = 10
        bufs = [A.alloc([S], F32) for _ in range(NBUF - 2)] + [self.HS[0], self.HS[1]]
        bkey = ["NB%d" % i for i in range(NBUF - 2)] + ["HS0", "HS1"]
        for c in range(NCH):
            hs, hk = bufs[c % NBUF], bkey[c % NBUF]
            sq, sk = self.SQB[:, c % 2, :], "SQB%d" % (c % 2)
            P.op(SP, lambda e, c=c, hs=hs: e.dma_start(out=hs, in_=self.hT[c * 128:(c + 1) * 128, :]),
                 reads=["hT%d" % c], writes=[hk], dma=True)
            P.op(ACT, lambda e, hs=hs, sq=sq: e.activation(out=sq, in_=hs, func=AF.Square),
                 reads=[hk], writes=[sk])
            for n in range(4):
                P.op(PE, lambda e, c=c, n=n, sq=sq: e.matmul(self.bank(n), lhsT=self.ones_bf,
                                                             rhs=sq[:, n * 512:(n + 1) * 512],
                                                             start=(c == 0), stop=(c == NCH - 1)),
                     reads=[sk, "cbf"], writes=["pb%d" % n])
        P.op(ACT, lambda e: e.activation(out=self.RSTD, in_=self.banks4(0), func=AF.Ln, scale=1.0 / D, bias=EPS),
             reads=self.pk(0), writes=["RSTD"])
        P.op(ACT, lambda e: e.activation(out=self.RSTD, in_=self.RSTD, func=AF.Exp, scale=-0.5),
             reads=["RSTD"], writes=["RSTD"])
        first = NCH - NBUF
        order = list(range(first, NCH)) + list(range(0, first))
        for pos, c in enumerate(order):
            if c >= first:
                bi = c % NBUF
            else:
                bi = order[pos - NBUF] % NBUF
            hs, hk = bufs[bi], bkey[bi]
            if c < first:
                P.op(SP, lambda e, c=c, hs=hs: e.dma_start(out=hs, in_=self.hT[c * 128:(c + 1) * 128, :]),
                     reads=["hT%d" % c], writes=[hk], dma=True)
            g = self.prm[:, gcol + c:gcol + c + 1]
            if not final:
                P.op(DVE, lambda e, c=c, hs=hs, g=g: e.scalar_tensor_tensor(
                    out=self.XN[:, c, :], in0=hs, scalar=g, in1=self.RSTD, op0=ALU.mult, op1=ALU.mult),
                    reads=[hk, "RSTD", "prm"], writes=["xn%d" % c])
            else:
                P.op(DVE, lambda e, c=c, hs=hs, g=g: e.scalar_tensor_tensor(
                    out=hs, in0=hs, scalar=g, in1=self.RSTD, op0=ALU.mult, op1=ALU.mult),
                    reads=[hk, "RSTD", "prm"], writes=[hk])
                P.op(SP, lambda e, c=c, hs=hs: e.dma_start(out=self.outT[c * 128:(c + 1) * 128, :], in_=hs),
                     reads=[hk], writes=["out%d" % c], dma=True)
        A.off = m0

    def final_norm(self):
        if self.dbg == "h":
            for c in range(NCH):
                self.P.op(SP, lambda e, c=c: e.dma_start(out=self.hdbg[c * 128:(c + 1) * 128, :],
                                                          in_=self.hT[c * 128:(c + 1) * 128, :]),
                          reads=["hT%d" % c], writes=["hd%d" % c], dma=True)
        self.norm(P_NORM + 64, final=True)

    def load_w(self, wd, r0, nk, c0, ncols):
        s = self.wslot % 2
        self.wslot += 1
        wb, key = self.WB[s], "wb%d" % s
        self.P.op(POOL, lambda e: e.dma_start(
            out=wb[:, 0:nk, 0:ncols],
            in_=wd[r0:r0 + nk * 128, c0:c0 + ncols].rearrange("(c p) n -> p c n", p=128)),
            writes=[key], dma=True)
        return wb, key

    def gemm_fm(self, wd, r0, c0, widths, rhs, rkey, evac, nk=NCH, nbank=4):
        P = self.P
        mi = 0
        i = 0
        while i < len(widths):
            grp = [widths[i]]
            if i + 1 < len(widths) and widths[i] + widths[i + 1] <= 256:
                grp.append(widths[i + 1])
            tot = sum(grp)
            wb, wkey = self.load_w(wd, r0, nk, c0, tot)
            off = 0
            for mw in grp:
                h = self.gpar % 2
                self.gpar += 1
                for k in range(nk):
                    for n in range(nbank):
                        P.op(PE, lambda e, k=k, n=n, h=h, off=off, mw=mw, wb=wb: e.matmul(
                            self.bank(4 * h + n)[0:mw, :], lhsT=wb[:, k, off:off + mw], rhs=rhs(k, n),
                            start=(k == 0), stop=(k == nk - 1)),
                            reads=[wkey, rkey(k)], writes=["pb%d" % (4 * h + n)])
                evac(mi, h)
                mi += 1
                off += mw
            c0 += tot
            i += len(grp)

    def xn_rhs(self, k, n):
        return self.XN[:, k, n * 512:(n + 1) * 512]

    def gemm_tm(self, wd, c0, ncols, vtm, vkey):
        P = self.P
        vT = self.SQB[:, 1, :]

        def ev(mi, h):
            P.op(ACT, lambda e, h=h: e.activation(out=vT, in_=self.banks4(h), func=AF.Copy),
                 reads=self.pk(h), writes=["SQB1"])
            for hb in range(2):
                b = 4 * h + 2 * (self.tmb % 2) + hb
                pbv = self.bank(b).bitcast(BF16)
                for j in range(8):
                    t = hb * 8 + j
                    P.op(PE, lambda e, t=t, j=j, pbv=pbv: e.transpose(pbv[:, j * 128:(j + 1) * 128],
                                                                      vT[:, t * 128:(t + 1) * 128], self.ident_bf),
                         reads=["SQB1", "cbf"], writes=["pb%d" % b])
                P.op(DVE, lambda e, hb=hb, pbv=pbv, mi=mi: e.tensor_copy(
                    out=vtm[:, hb * 8:hb * 8 + 8, mi * 128:(mi + 1) * 128],
                    in_=pbv.rearrange("p (a c) -> p a c", a=8)),
                    reads=["pb%d" % b], writes=[vkey])
            self.tmb += 1
        self.gemm_fm(wd, 0, c0, [128] * (ncols // 128), self.xn_rhs, lambda k: "xn%d" % k, ev)

    def gla_core(self, qg, kg, kgtm, vtm, eb, dvh, oT, St, Sbf, PT):
        P = self.P
        dv = dvh * 128
        P.op(DVE, lambda e: e.memset(St, 0.0), writes=["St"])
        P.op(DVE, lambda e: e.memset(Sbf[0], 0.0), writes=["Sbf0"])
        sidx = 0

        def emit_sc(i):
            tl = slice(i * 128, (i + 1) * 128)
            sb = 4 if i % 2 == 0 else 7
            pt, ptk = PT[i % 2], "PT%d" % (i % 2)
            P.op(PE, lambda e: e.matmul(self.bank(sb, 128), lhsT=kg[:, tl], rhs=qg[:, tl], start=True, stop=True),
                 reads=["kg", "qg"], writes=["pb%d" % sb])
            P.op(DVE, lambda e: e.tensor_tensor(out=pt, in0=self.bank(sb, 128), in1=self.ublk_f, op=ALU.mult),
                 reads=["pb%d" % sb, "cf"], writes=[ptk])

        def ubank(i, cc):
            if dvh == 1 and i % 2 == 1:
                return 1 + 2 * cc
            return 5 + cc

        def emit_u(i):
            for cc in range(2):
                rs = slice(cc * 64, cc * 64 + 64)
                ub = ubank(i, cc)
                P.op(PE, lambda e, cc=cc, rs=rs, ub=ub: e.matmul(
                    self.bank(ub, dv), lhsT=kgtm[rs, i, :], rhs=vtm[rs, i, :], start=True, stop=True),
                    reads=["kgtm", "vtm"], writes=["pb%d" % ub])

        emit_u(0)
        emit_sc(0)
        for i in range(NCH):
            g = i // 4
            pt, ptk = PT[i % 2], "PT%d" % (i % 2)
            obanks = [(2 * (g % 2) + hh) for hh in range(dvh)]
            col = (i % 4) * 128
            for hh in range(dvh):
                ob = obanks[hh]
                P.op(PE, lambda e, i=i, hh=hh, ob=ob, col=col, pt=pt: e.matmul(
                    self.bank(ob, 128, col), lhsT=vtm[:, i, hh * 128:(hh + 1) * 128], rhs=pt, start=True, stop=False),
                    reads=["vtm", ptk], writes=["pb%d" % ob])
            if i + 1 < NCH:
                emit_sc(i + 1)
                if dvh == 1:
                    emit_u(i + 1)
            for cc in range(2):
                ub = ubank(i, cc)
                c = 2 * i + cc
                cur, nxt = Sbf[sidx % 2], Sbf[(sidx + 1) % 2]
                ck, nk_ = "Sbf%d" % (sidx % 2), "Sbf%d" % ((sidx + 1) % 2)
                qs = slice(i * 128 + cc * 64, i * 128 + cc * 64 + 64)
                for hh in range(dvh):
                    ob = obanks[hh]
                    P.op(PE, lambda e, hh=hh, ob=ob, col=col, cc=cc, cur=cur, qs=qs: e.matmul(
                        self.bank(ob, 64, col + cc * 64), lhsT=cur[:, hh * 128:(hh + 1) * 128], rhs=qg[:, qs],
                        start=False, stop=(cc == 1)),
                        reads=[ck, "qg"], writes=["pb%d" % ob])
                if c == 2 * NCH - 1:
                    break
                el = eb[:, c * 64 + 63:c * 64 + 64]
                P.op(DVE, lambda e, cc=cc, nxt=nxt, el=el, ub=ub: e.scalar_tensor_tensor(
                    out=nxt, in0=St, scalar=el, in1=self.bank(ub, dv), op0=ALU.mult, op1=ALU.add),
                    reads=["pb%d" % ub, "St", "eb"], writes=[nk_])
                P.op(DVE, lambda e, cc=cc, el=el, ub=ub: e.scalar_tensor_tensor(
                    out=St, in0=St, scalar=el, in1=self.bank(ub, dv), op0=ALU.mult, op1=ALU.add),
                    reads=["pb%d" % ub, "St", "eb"], writes=["St"])
                sidx += 1
            if dvh != 1 and i + 1 < NCH:
                emit_u(i + 1)
            if i % 4 == 3:
                for hh in range(dvh):
                    ob = obanks[hh]
                    P.op(ACT, lambda e, hh=hh, ob=ob, g=g: e.activation(
                        out=oT[:, hh, g * 512:(g + 1) * 512], in_=self.bank(ob), func=AF.Copy),
                        reads=["pb%d" % ob], writes=["oT"])

    def transpose_kg(self, kg, kgtm):
        P = self.P
        for hb in range(2):
            b = 6 + hb
            pbv = self.bank(b).bitcast(BF16)
            for j in range(8):
                t = hb * 8 + j
                P.op(PE, lambda e, t=t, j=j, pbv=pbv: e.transpose(pbv[:, j * 128:(j + 1) * 128],
                                                                  kg[:, t * 128:(t + 1) * 128], self.ident_bf),
                     reads=["SQB0", "cbf"], writes=["pb%d" % b])
            P.op(DVE, lambda e, hb=hb, pbv=pbv: e.tensor_copy(
                out=kgtm[:, hb * 8:hb * 8 + 8, :], in_=pbv.rearrange("p (a c) -> p a c", a=8)),
                reads=["pb%d" % b], writes=["kgtm"])

    def gated_out(self, oT, sg, dvh, gain_col, row0):
        P = self.P
        dv = dvh * 128
        for hh in range(dvh):
            P.op(ACT, lambda e, hh=hh: e.activation(out=self.SQB[:, hh, :], in_=oT[:, hh, :], func=AF.Square),
                 reads=["oT"], writes=["SQB%d" % hh])
        for n in range(4):
            for hh in range(dvh):
                P.op(PE, lambda e, n=n, hh=hh: e.matmul(self.bank(4 + n), lhsT=self.ones_bf,
                                                        rhs=self.SQB[:, hh, n * 512:(n + 1) * 512],
                                                        start=(hh == 0), stop=(hh == dvh - 1)),
                     reads=["SQB%d" % hh, "cbf"], writes=["pb%d" % (4 + n)])
        P.op(ACT, lambda e: e.activation(out=self.RSTD, in_=self.banks4(1), func=AF.Ln, scale=1.0 / dv, bias=EPS),
             reads=self.pk(1), writes=["RSTD"])
        P.op(ACT, lambda e: e.activation(out=self.RSTD, in_=self.RSTD, func=AF.Exp, scale=-0.5),
             reads=["RSTD"], writes=["RSTD"])
        for hh in range(dvh):
            g = self.prm[:, gain_col + hh:gain_col + hh + 1]
            P.op(DVE, lambda e, hh=hh, g=g: e.scalar_tensor_tensor(
                out=oT[:, hh, :], in0=oT[:, hh, :], scalar=g, in1=self.RSTD, op0=ALU.mult, op1=ALU.mult),
                reads=["oT", "RSTD", "prm"], writes=["oT"])
            ms = self.HS[1].bitcast(BF16)[:, hh * S:(hh + 1) * S]
            P.op(DVE, lambda e, hh=hh, ms=ms: e.tensor_tensor(out=ms, in0=oT[:, hh, :], in1=sg[:, hh, :], op=ALU.mult),
                 reads=["oT", "sg"], writes=["ms%d" % hh])
            r = row0 + hh * 128
            P.op(SP, lambda e, ms=ms, r=r: e.dma_start(out=self.mixd[r:r + 128, :], in_=ms),
                 reads=["ms%d" % hh], writes=["mix%d" % (r // 128)], dma=True)

    def attention(self, qT, kT, vtm, bias_col, bias_key, row_lhsT, row_rhs, nrow, row_keys, Pb, osb, row0):
        P = self.P
        steps = [(n, j) for n in range(4) for j in range(4 * n + 4)]

        def geo(s):
            n, j = steps[s]
            c0 = max(0, j - 4 * n) * 128
            return n, j, c0, 512 - c0, s % 4, Pb[s % 3], "Pb%d" % (s % 3)

        def emit_qk(s):
            n, j, c0, N, sb, pb, pbk = geo(s)
            diag = j >= 4 * n
            tq = slice(512 * n + c0, 512 * (n + 1))
            P.op(PE, lambda e: e.matmul(self.bank(sb, N, c0), lhsT=kT[:, j * 128:(j + 1) * 128], rhs=qT[:, tq],
                                        start=True, stop=False),
                 reads=["kT", "qT"], writes=["pb%d" % sb])
            P.op(PE, lambda e: e.matmul(self.bank(sb, N, c0), lhsT=row_lhsT(j), rhs=row_rhs[0:nrow, tq],
                                        start=False, stop=(not diag)),
                 reads=row_keys, writes=["pb%d" % sb])
            if diag:
                P.op(PE, lambda e: e.matmul(self.bank(sb, 128, c0), lhsT=self.ident_bf, rhs=self.cm_bf,
                                            start=False, stop=True),
                     reads=["cbf"], writes=["pb%d" % sb])
            P.op(ACT, lambda e: e.activation(out=pb[:, c0:512], in_=self.bank(sb, N, c0), func=AF.Exp, bias=bias_col(j)),
                 reads=["pb%d" % sb, bias_key], writes=[pbk])

        def emit_pv(s):
            n, j, c0, N, sb, pb, pbk = geo(s)
            ob, lb = (4, 5) if n % 2 == 0 else (6, 7)
            jmax = 4 * n + 3
            P.op(PE, lambda e: e.matmul(self.bank(ob, N, c0), lhsT=vtm[:, j, :], rhs=pb[:, c0:512],
                                        start=(j == 0), stop=(j == jmax)),
                 reads=["vtm", pbk], writes=["pb%d" % ob])
            P.op(PE, lambda e: e.matmul(self.bank(lb, N, c0), lhsT=self.ones_bf, rhs=pb[:, c0:512],
                                        start=(j == 0), stop=(j == jmax)),
                 reads=["cbf", pbk], writes=["pb%d" % lb])
            if j == jmax:
                rl, rk = osb[n % 2], "osb%d" % (n % 2)
                P.op(DVE, lambda e: e.reciprocal(out=rl, in_=self.bank(lb)), reads=["pb%d" % lb], writes=[rk])
                ms = self.HS[1].bitcast(BF16)[:, n * 512:(n + 1) * 512]
                P.op(DVE, lambda e: e.tensor_tensor(out=ms, in0=self.bank(ob), in1=rl, op=ALU.mult),
                     reads=["pb%d" % ob, rk], writes=["ms%d" % n])
                P.op(SP, lambda e: e.dma_start(out=self.mixd[row0:row0 + 128, n * 512:(n + 1) * 512], in_=ms),
                     reads=["ms%d" % n], writes=["mix%d_%d" % (row0 // 128, n)], dma=True)

        emit_qk(0)
        emit_qk(1)
        for s in range(len(steps)):
            emit_pv(s)
            if s + 2 < len(steps):
                emit_qk(s + 2)

    def layer(self, l):
        P = self.P
        W = self.W[l]
        self.gpar = 0
        self.tmb = 0
        self.norm(P_NORM + 32 * l)
        P.barrier()
        if l == 0:
            self.mix_even(W["w_in"])
        else:
            self.mix_odd(W["w_in"])
        P.barrier()
        self.A.off = self.mark
        if self.dbg in ("s2", "s3", "s5", "s6"):
            return
        P.op(SP, lambda e: e.dma_start(out=self.XN[:, 0:8, :],
                                       in_=self.mixd[0:1024, :].rearrange("(c p) t -> p c t", p=128)),
             writes=["xn%d" % c for c in range(8)], dma=True)
        P.op(SP, lambda e: e.dma_start(out=self.XN[:, 8:16, :],
                                       in_=self.mixd[1024:2048, :].rearrange("(c p) t -> p c t", p=128)),
             writes=["xn%d" % c for c in range(8, 16)], dma=True)
        self.gemm_fm(W["w_out"], 0, 0, [128] * 16, self.xn_rhs, lambda k: "xn%d" % k, self.evac_resid(0))
        P.barrier()
        self.norm(P_NORM + 32 * l + 16)
        P.barrier()
        aT = self.A.alloc([NCH, S], BF16)
        for fg in range(4):
            def evac_up(mi, h, aT=aT):
                hs, hk = self.HS[mi % 2], "HS%d" % (mi % 2)
                P.op(ACT, lambda e, h=h, hs=hs: e.activation(out=hs, in_=self.banks4(h), func=AF.Relu),
                     reads=self.pk(h), writes=[hk])
                P.op(DVE, lambda e, mi=mi, hs=hs: e.tensor_tensor(out=aT[:, mi, :], in0=hs, in1=hs, op=ALU.mult),
                     reads=[hk], writes=["aT%d" % mi])
            self.gemm_fm(W["w_up"], 0, fg * 2048, [128] * 16, self.xn_rhs, lambda k: "xn%d" % k, evac_up)
            self.gemm_fm(W["w_down"], fg * 2048, 0, [128] * 16,
                         lambda k, n, aT=aT: aT[:, k, n * 512:(n + 1) * 512], lambda k: "aT%d" % k,
                         self.evac_resid(0))
        P.barrier()
        self.A.off = self.mark

    def evac_resid(self, _):
        P = self.P

        def pre(mi):
            hs, hk = self.HS[mi % 2], "HS%d" % (mi % 2)
            P.op(SP, lambda e, mi=mi, hs=hs: e.dma_start(out=hs, in_=self.hT[mi * 128:(mi + 1) * 128, :]),
                 reads=["hT%d" % mi], writes=[hk], dma=True)

        def ev(mi, h):
            hs, hk = self.HS[mi % 2], "HS%d" % (mi % 2)
            if mi < 2:
                pre(mi)
            P.op(DVE, lambda e, h=h, hs=hs: e.tensor_tensor(out=hs, in0=self.banks4(h), in1=hs, op=ALU.add),
                 reads=self.pk(h) + [hk], writes=[hk])
            P.op(SP, lambda e, mi=mi, hs=hs: e.dma_start(out=self.hT[mi * 128:(mi + 1) * 128, :], in_=hs),
                 reads=[hk], writes=["hT%d" % mi], dma=True)
            if mi + 2 < NCH:
                pre(mi + 2)
        return ev

    def mix_even(self, w_in):
        P, A = self.P, self.A
        xk = lambda k: "xn%d" % k
        glrT = self.HS[0]
        def ev_glr(mi, h):
            P.op(ACT, lambda e, h=h: e.activation(out=glrT[0:16, :], in_=self.banks4(h)[0:16, :], func=AF.Copy),
                 reads=self.pk(h), writes=["glr"])
        self.gemm_fm(w_in, 0, 3072, [16], self.xn_rhs, xk, ev_glr)
        m0 = A.off
        eb, enb, ebl = A.alloc([S]), A.alloc([S]), A.alloc([S])
        qg, kg = A.alloc([S], BF16), A.alloc([S], BF16)
        kgl = self.SQB[:, 0, :]
        c3 = lambda a: a.rearrange("p (c j) -> p c j", j=64)
        kgtm = A.alloc([NCH, 128], BF16)
        vtm = A.alloc([NCH, 256], BF16)
        sg = A.alloc([2, S], BF16)
        oT = A.alloc([2, S])
        St = A.alloc([256])
        Sbf = [A.alloc([256], BF16) for _ in range(2)]
        PT = [A.alloc([128], BF16) for _ in range(2)]
        cs = self.RSTD
        for hd in ([] if self.dbg == "s3" else [0] if self.dbg == "s2" else range(4)):
            for n in range(4):
                P.op(PE, lambda e, hd=hd, n=n: e.matmul(self.bank(n), lhsT=self.w2[0:16, hd * 128:(hd + 1) * 128],
                                                       rhs=glrT[0:16, n * 512:(n + 1) * 512], start=True, stop=True),
                     reads=["w2", "glr"], writes=["pb%d" % n])
            nb = self.lbt[:, 16 + hd:17 + hd]
            P.op(DVE, lambda e, hd=hd, nb=nb: e.tensor_scalar(out=nb, in0=self.prm[:, P_GB + hd:P_GB + hd + 1],
                                                             scalar1=-1.0, scalar2=None, op0=ALU.mult),
                 reads=["prm"], writes=["nb"])
            P.op(ACT, lambda e, nb=nb: e.activation(out=eb, in_=self.banks4(0), func=AF.Exp, scale=-1.0, bias=nb),
                 reads=self.pk(0) + ["nb"], writes=["eb"])
            P.op(ACT, lambda e: e.activation(out=eb, in_=eb, func=AF.Ln, bias=1.0), reads=["eb"], writes=["eb"])
            P.op(DVE, lambda e: e.tensor_tensor_scan(out=cs, data0=self.RM, data1=eb, initial=0.0,
                                                     op0=ALU.mult, op1=ALU.add),
                 reads=["RM", "eb"], writes=["RSTD"])
            P.op(ACT, lambda e: e.activation(out=eb, in_=cs, func=AF.Exp, scale=-1.0 / 16), reads=["RSTD"], writes=["eb"])
            P.op(ACT, lambda e: e.activation(out=enb, in_=cs, func=AF.Exp, scale=1.0 / 16), reads=["RSTD"], writes=["enb"])
            P.op(DVE, lambda e: e.tensor_tensor(out=c3(ebl), in0=c3(enb), in1=c3(eb)[:, :, 63:64].to_broadcast([128, 32, 64]),
                                                op=ALU.mult), reads=["enb", "eb"], writes=["ebl"])

            def ev_q(mi, h):
                P.op(DVE, lambda e, h=h: e.scalar_tensor_tensor(out=qg, in0=self.banks4(h), scalar=128 ** -0.5, in1=eb,
                                                                op0=ALU.mult, op1=ALU.mult),
                     reads=self.pk(h) + ["eb"], writes=["qg"])

            def ev_k(mi, h):
                P.op(DVE, lambda e, h=h: e.tensor_tensor(out=kg, in0=self.banks4(h), in1=enb, op=ALU.mult),
                     reads=self.pk(h) + ["enb"], writes=["kg"])
                P.op(DVE, lambda e, h=h: e.tensor_tensor(out=kgl, in0=self.banks4(h), in1=ebl, op=ALU.mult),
                     reads=self.pk(h) + ["ebl"], writes=["SQB0"])

            def ev_g(mi, h):
                P.op(ACT, lambda e, mi=mi, h=h: e.activation(out=sg[:, mi, :], in_=self.banks4(h), func=AF.Silu),
                     reads=self.pk(h), writes=["sg"])
            self.gemm_fm(w_in, 0, hd * 128, [128], self.xn_rhs, xk, ev_q)
            self.gemm_fm(w_in, 0, 512 + hd * 128, [128], self.xn_rhs, xk, ev_k)
            self.gemm_fm(w_in, 0, 2048 + hd * 256, [128, 128], self.xn_rhs, xk, ev_g)
            self.gemm_tm(w_in, 1024 + hd * 256, 256, vtm, "vtm")
            self.transpose_kg(kgl, kgtm)
            self.gla_core(qg, kg, kgtm, vtm, eb, 2, oT, St, Sbf, PT)
            self.gated_out(oT, sg, 2, P_GN, hd * 256)
        P.barrier()
        A.off = m0
        qT, kT = A.alloc([S], BF16), A.alloc([S], BF16)
        vt = A.alloc([NCH, 128], BF16)
        Pb = [A.alloc([512], BF16) for _ in range(3)]
        osb = [A.alloc([512]) for _ in range(2)]
        Gm = A.alloc([NCH, 8])
        m8 = A.alloc([NCH, 8])
        sel = A.alloc([NCH, 8])
        ksum = A.alloc([8])
        kmT = A.alloc([8], BF16)
        Eh = A.alloc([8, 128], BF16)
        pm = self.cf[:, C_PM:C_PM + 128].rearrange("p (a b) -> p a b", a=NCH)
        p01 = self.cf[:, C_P01:C_P01 + 128].rearrange("p (a b) -> p a b", a=NCH)
        o01 = self.cf[:, C_O01:C_O01 + 128].rearrange("p (a b) -> p a b", a=NCH)
        for hd in ([] if self.dbg == "s2" else [0, 5] if self.dbg == "s3" else range(8)):
            slope = 2.0 ** (-(hd + 1))

            def ev_q(mi, h):
                P.op(DVE, lambda e, h=h: e.tensor_scalar(out=qT, in0=self.banks4(h), scalar1=128 ** -0.5, scalar2=None,
                                                         op0=ALU.mult),
                     reads=self.pk(h), writes=["qT"])

            def ev_k(mi, h):
                for n in range(8):
                    P.op(DVE, lambda e, h=h, n=n: e.tensor_scalar(
                        out=kT[:, n * 256:(n + 1) * 256], in0=self.banks4(h)[:, n * 256:(n + 1) * 256], scalar1=1.0,
                        scalar2=None, op0=ALU.mult, op1=ALU.add, accum_out=ksum[:, n:n + 1]),
                        reads=self.pk(h), writes=["kT", "ksum"])
                P.op(DVE, lambda e: e.tensor_scalar(out=kmT, in0=ksum, scalar1=1.0 / 256, scalar2=None, op0=ALU.mult),
                     reads=["ksum"], writes=["kmT"])
            self.gemm_fm(w_in, 0, 3088 + hd * 128, [128], self.xn_rhs, xk, ev_q)
            self.gemm_fm(w_in, 0, 4112 + hd * 128, [128], self.xn_rhs, xk, ev_k)
            self.gemm_tm(w_in, 5136 + hd * 128, 128, vt, "vtm")
            import os
            stop = os.environ.get("MOBA_STOP", "")
            if stop in ("a", "a0"):
                continue
            for i in range(NCH):
                P.op(PE, lambda e, i=i: e.matmul(self.bank(0, 8, i * 8), lhsT=qT[:, i * 128:(i + 1) * 128], rhs=kmT,
                                                start=True, stop=True),
                     reads=["qT", "kmT"], writes=["pb0"])
            P.op(DVE, lambda e: e.tensor_tensor(out=Gm, in0=self.bank(0, 128).rearrange("p (a b) -> p a b", a=NCH),
                                                in1=pm, op=ALU.add),
                 reads=["pb0", "cf"], writes=["Gm"])
            for i in range(NCH):
                P.op(DVE, lambda e, i=i: e.max(out=m8[:, i, :], in_=Gm[:, i, :]), reads=["Gm"], writes=["m8"])
            P.op(DVE, lambda e: e.tensor_tensor(out=sel, in0=Gm, in1=m8[:, :, 2:3].to_broadcast([128, NCH, 8]),
                                                op=ALU.is_ge),
                 reads=["Gm", "m8"], writes=["sel"])
            P.op(DVE, lambda e: e.tensor_tensor(out=sel, in0=sel, in1=p01, op=ALU.mult),
                 reads=["sel", "cf"], writes=["sel"])
            P.op(DVE, lambda e: e.tensor_tensor(out=sel, in0=sel, in1=o01, op=ALU.add),
                 reads=["sel", "cf"], writes=["sel"])
            P.op(DVE, lambda e: e.tensor_scalar(out=sel, in0=sel, scalar1=-1.0, scalar2=-NEG, op0=ALU.add, op1=ALU.mult),
                 reads=["sel"], writes=["sel"])
            if stop == "b":
                continue
            for i in range(NCH):
                P.op(PE, lambda e, i=i: e.transpose(self.bank(4 + i // 4, 128, (i % 4) * 128)[0:8, :],
                                                   sel[:, i, :], self.ident_f),
                     reads=["sel", "cf"], writes=["pb%d" % (4 + i // 4)])
            P.op(ACT, lambda e: e.activation(out=self.ROWM[0:8, :], in_=self.banks4(1)[0:8, :], func=AF.Copy),
                 reads=self.pk(1), writes=["ROWM"])
            P.op(DVE, lambda e: e.tensor_copy(out=Eh[0:64], in_=self.EM[0:64]), reads=["EM"], writes=["Eh"])
            P.op(DVE, lambda e, slope=slope: e.tensor_scalar(out=Eh[32:34], in0=self.EM[32:34], scalar1=slope,
                                                            scalar2=None, op0=ALU.mult),
                 reads=["EM", "Eh"], writes=["Eh"])
            if stop == "c":
                continue
            self.attention(qT, kT, vt,
                           lambda j, hd=hd: self.cf[:, C_ALC + hd * 16 + j:C_ALC + hd * 16 + j + 1], "cf",
                           lambda j: Eh[0:64, j // 2, :], self.ROWM, 64, ["Eh", "ROWM"], Pb, osb, 1024 + hd * 128)
        P.barrier()
        A.off = m0

    def mix_odd(self, w_in):
        P, A = self.P, self.A
        xk = lambda k: "xn%d" % k
        m0 = A.off
        lbt = self.lbt
        P.op(ACT, lambda e: e.activation(out=lbt[:, 0:16], in_=self.prm[:, P_LB:P_LB + 16], func=AF.Exp),
             reads=["prm"], writes=["lbt"])
        P.op(DVE, lambda e: e.tensor_tensor(out=lbt[:, 0:8], in0=lbt[:, 0:8], in1=lbt[:, 8:16], op=ALU.add),
             reads=["lbt"], writes=["lbt"])
        P.op(DVE, lambda e: e.reciprocal(out=lbt[:, 0:8], in_=lbt[:, 0:8]), reads=["lbt"], writes=["lbt"])
        P.op(DVE, lambda e: e.tensor_tensor(out=lbt[:, 8:16], in0=lbt[:, 8:16], in1=lbt[:, 0:8], op=ALU.mult),
             reads=["lbt"], writes=["lbt"])
        P.op(DVE, lambda e: e.tensor_scalar(out=lbt[:, 0:8], in0=lbt[:, 8:16], scalar1=-1.0, scalar2=1.0,
                                            op0=ALU.mult, op1=ALU.add),
             reads=["lbt"], writes=["lbt"])
        lf = self.HS[0]
        cs = self.RSTD
        cstm = A.alloc([NCH, 8])
        tmp = A.alloc([S])
        nfb = lbt[:, 24:25]
        P.op(DVE, lambda e: e.tensor_scalar(out=nfb[0:8], in0=self.prm[0:8, P_FB:P_FB + 1], scalar1=-1.0, scalar2=None,
                                            op0=ALU.mult), reads=["prm"], writes=["nfb"])

        def ev_f(mi, h):
            P.op(ACT, lambda e, h=h: e.activation(out=lf[0:8, :], in_=self.banks4(h)[0:8, :], func=AF.Exp, scale=-1.0,
                                                  bias=nfb[0:8]), reads=self.pk(h) + ["nfb"], writes=["lf"])
        self.gemm_fm(w_in, 0, 3072, [8], self.xn_rhs, xk, ev_f)
        P.op(ACT, lambda e: e.activation(out=lf[0:8, :], in_=lf[0:8, :], func=AF.Ln, bias=1.0), reads=["lf"], writes=["lf"])
        P.op(DVE, lambda e: e.tensor_tensor_scan(out=cs[0:8, :], data0=self.cbf[0:8, 384:385].to_broadcast([8, S]),
                                                 data1=lf[0:8, :], initial=0.0, op0=ALU.mult, op1=ALU.add),
             reads=["cbf", "lf"], writes=["RSTD"])
        P.op(DVE, lambda e: e.tensor_copy(out=self.ROWF[0:8, :], in_=cs[0:8, :]), reads=["RSTD"], writes=["ROWF"])
        P.op(DVE, lambda e: e.tensor_tensor(out=tmp[0:8, :], in0=cs[0:8, :], in1=self.ROWF[0:8, :], op=ALU.subtract),
             reads=["RSTD", "ROWF"], writes=["tmp"])
        P.op(SP, lambda e: e.dma_start(out=tmp[32:40, :], in_=tmp[0:8, :]), reads=["tmp"], writes=["tmp32"], dma=True)
        P.op(DVE, lambda e: e.tensor_copy(out=self.ROWF[32:40, :], in_=tmp[32:40, :]), reads=["tmp32"], writes=["ROWF"])
        P.op(DVE, lambda e: e.tensor_tensor(out=tmp[32:40, :], in0=tmp[32:40, :], in1=self.ROWF[32:40, :],
                                            op=ALU.subtract), reads=["tmp32", "ROWF"], writes=["tmp32"])
        P.op(SP, lambda e: e.dma_start(out=tmp[64:72, :], in_=tmp[32:40, :]), reads=["tmp32"], writes=["tmp64"], dma=True)
        P.op(DVE, lambda e: e.tensor_copy(out=self.ROWF[64:72, :], in_=tmp[64:72, :]), reads=["tmp64"], writes=["ROWF"])
        for i in range(NCH):
            P.op(PE, lambda e, i=i: e.transpose(self.bank(0, 8, i * 8), cs[0:8, i * 128:(i + 1) * 128],
                                               self.ident_f[0:8, 0:8]),
                 reads=["RSTD", "cf"], writes=["pb0"])
        P.op(DVE, lambda e: e.tensor_copy(out=cstm, in_=self.bank(0, 128).rearrange("p (a b) -> p a b", a=NCH)),
             reads=["pb0"], writes=["bcol"])
        qT, kT = A.alloc([S], BF16), A.alloc([S], BF16)
        vt = A.alloc([NCH, 128], BF16)
        Pb = [A.alloc([512], BF16) for _ in range(3)]
        osb = [A.alloc([512]) for _ in range(2)]
        for hd in range(8):
            def ev_q(mi, h):
                P.op(DVE, lambda e, h=h: e.tensor_scalar(out=qT, in0=self.banks4(h), scalar1=128 ** -0.5, scalar2=None,
                                                         op0=ALU.mult),
                     reads=self.pk(h), writes=["qT"])

            def ev_k(mi, h):
                P.op(ACT, lambda e, h=h: e.activation(out=kT, in_=self.banks4(h), func=AF.Copy),
                     reads=self.pk(h), writes=["kT"])
            self.gemm_fm(w_in, 0, hd * 128, [128], self.xn_rhs, xk, ev_q)
            self.gemm_fm(w_in, 0, 1024 + hd * 128, [128], self.xn_rhs, xk, ev_k)
            self.gemm_tm(w_in, 2048 + hd * 128, 128, vt, "vtm")
            self.attention(qT, kT, vt, lambda j, hd=hd: cstm[:, j, hd:hd + 1], "bcol",
                           lambda j, hd=hd: self.EF[:, hd, :], self.ROWF, 128, ["EF", "ROWF"], Pb, osb, hd * 128)
        P.barrier()
        A.off = m0
        F1, eb, enb, ebl = A.alloc([S]), A.alloc([S]), A.alloc([S]), A.alloc([S])
        qg, kg = A.alloc([S], BF16), A.alloc([S], BF16)
        kgl = self.SQB[:, 0, :]
        c3 = lambda a: a.rearrange("p (c j) -> p c j", j=64)
        kgtm = A.alloc([NCH, 128], BF16)
        vtm = A.alloc([NCH, 128], BF16)
        sg = A.alloc([1, S], BF16)
        oT = A.alloc([1, S])
        St = A.alloc([128])
        Sbf = [A.alloc([128], BF16) for _ in range(2)]
        PT = [A.alloc([128], BF16) for _ in range(2)]
        for hd in range(8):
            lb = lbt[:, 8 + hd:9 + hd]
            oml = lbt[:, hd:hd + 1]

            def ev_f(mi, h, oml=oml, lb=lb):
                P.op(ACT, lambda e, h=h: e.activation(out=F1, in_=self.banks4(h), func=AF.Sigmoid),
                     reads=self.pk(h), writes=["F1"])
                P.op(DVE, lambda e, oml=oml, lb=lb: e.tensor_scalar(out=F1, in0=F1, scalar1=oml, scalar2=lb,
                                                                   op0=ALU.mult, op1=ALU.add),
                     reads=["F1", "lbt"], writes=["F1"])
                P.op(ACT, lambda e: e.activation(out=eb, in_=F1, func=AF.Ln), reads=["F1"], writes=["eb"])
                P.op(DVE, lambda e: e.tensor_tensor_scan(out=cs, data0=self.RM, data1=eb, initial=0.0,
                                                         op0=ALU.mult, op1=ALU.add),
                     reads=["RM", "eb"], writes=["RSTD"])
                P.op(ACT, lambda e: e.activation(out=enb, in_=cs, func=AF.Exp, scale=-1.0), reads=["RSTD"], writes=["enb"])
                P.op(ACT, lambda e: e.activation(out=eb, in_=cs, func=AF.Exp), reads=["RSTD"], writes=["eb"])
                P.op(DVE, lambda e: e.tensor_scalar(out=F1, in0=F1, scalar1=-1.0, scalar2=1.0, op0=ALU.mult, op1=ALU.add),
                     reads=["F1"], writes=["F1"])
                P.op(DVE, lambda e: e.tensor_tensor(out=kg, in0=F1, in1=enb, op=ALU.mult),
                     reads=["F1", "enb"], writes=["kg"])
                P.op(DVE, lambda e: e.tensor_tensor(out=c3(ebl), in0=c3(enb),
                                                    in1=c3(eb)[:, :, 63:64].to_broadcast([128, 32, 64]), op=ALU.mult),
                     reads=["enb", "eb"], writes=["ebl"])
                P.op(DVE, lambda e: e.tensor_tensor(out=kgl, in0=F1, in1=ebl, op=ALU.mult),
                     reads=["F1", "ebl"], writes=["SQB0"])

            def ev_q(mi, h):
                P.op(ACT, lambda e, h=h: e.activation(out=F1, in_=self.banks4(h), func=AF.Silu),
                     reads=self.pk(h), writes=["F1"])
                P.op(DVE, lambda e: e.tensor_tensor(out=qg, in0=F1, in1=eb, op=ALU.mult),
                     reads=["F1", "eb"], writes=["qg"])

            def ev_g(mi, h):
                P.op(ACT, lambda e, h=h: e.activation(out=sg[:, 0, :], in_=self.banks4(h), func=AF.Silu),
                     reads=self.pk(h), writes=["sg"])
            self.gemm_fm(w_in, 0, 4104 + hd * 128, [128], self.xn_rhs, xk, ev_f)
            self.gemm_fm(w_in, 0, 3080 + hd * 128, [128], self.xn_rhs, xk, ev_q)
            self.gemm_fm(w_in, 0, 6152 + hd * 128, [128], self.xn_rhs, xk, ev_g)
            self.gemm_tm(w_in, 5128 + hd * 128, 128, vtm, "vtm")
            self.transpose_kg(kgl, kgtm)
            self.gla_core(qg, kg, kgtm, vtm, eb, 1, oT, St, Sbf, PT)
            self.gated_out(oT, sg, 1, P_HN, 1024 + hd * 128)
        P.barrier()
        A.off = m0


def _consts():
    c = np.zeros((128, NCST), np.float32)
    p = np.arange(128)
    c[:, C_ID:C_ID + 128] = np.eye(128, dtype=np.float32)
    s, t = p[:, None], p[None, :]
    c[:, C_UB:C_UB + 128] = ((s <= t) & (s // 64 == t // 64)).astype(np.float32)
    c[:, C_CM:C_CM + 128] = np.where(s <= t, 0.0, NEG).astype(np.float32)
    c[:, C_ONE:C_ONE + 128] = 1.0
    for h in range(8):
        for j in range(16):
            c[:, C_ALC + h * 16 + j] = (2.0 ** (-(h + 1))) * (128 * j + p)
    for i in range(16):
        for n in range(8):
            c[:, C_PM + i * 8 + n] = 0.0 if n < i // 2 else -1e30
            c[:, C_P01 + i * 8 + n] = 1.0 if n < i // 2 else 0.0
            c[:, C_O01 + i * 8 + n] = 1.0 if n == i // 2 else 0.0
    for n in range(8):
        c[n, C_EM + n * 128:C_EM + (n + 1) * 128] = 1.0
        c[32:34, C_EM + n * 128:C_EM + (n + 1) * 128] = 1.0
        for r in (n, 32 + n, 64 + n):
            c[r, C_EF + n * 128:C_EF + (n + 1) * 128] = -1.0
    tt = np.arange(S)
    c[32, C_AR:C_AR + S] = -128.0 * (tt // 128)
    c[33, C_AR:C_AR + S] = -(tt % 128).astype(np.float32)
    c[:, C_RM:C_RM + S] = (tt % 64 != 0).astype(np.float32)[None, :]
    return c


def _params(inp):
    pr = np.zeros((128, NPAR), np.float32)
    fm = lambda v: np.asarray(v, np.float32).reshape(-1, 128).T
    pr[:, 0:16] = fm(inp["ev_norm_mix"][0])
    pr[:, 16:32] = fm(inp["ev_norm_mlp"][0])
    pr[:, 32:48] = fm(inp["od_norm_mix"][0])
    pr[:, 48:64] = fm(inp["od_norm_mlp"][0])
    pr[:, 64:80] = fm(inp["final_norm"])
    pr[:, P_GB:P_GB + 4] = fm(inp["ev_gla_gate_b"][0])
    pr[:, P_GN:P_GN + 2] = fm(inp["ev_gla_out_norm"][0])
    pr[:, P_HN:P_HN + 1] = fm(inp["od_hgrn_out_norm"][0])
    pr[:, P_LB:P_LB + 8] = fm(inp["hgrn_lb_raw"][0])
    pr[:, P_LB + 8:P_LB + 16] = fm(inp["hgrn_lb_raw"][1])
    pr[0:8, P_FB] = np.asarray(inp["od_fox_fgate_b"][0], np.float32)
    return pr


_NC_CACHE = {}


def make_in_maps(inp, ncores=8):
    cst = _consts()
    prm = _params(inp)
    shared = {"w2": np.ascontiguousarray(np.asarray(inp["ev_gla_gate_w2"][0], np.float32)), "prm": prm, "cst": cst}
    for p in ("ev", "od"):
        for w in ("w_in", "w_out", "w_up", "w_down"):
            shared["%s_%s" % (p, w)] = np.ascontiguousarray(np.asarray(inp["%s_%s" % (p, w)][0], np.float32))
    x = np.asarray(inp["x"], np.float32)
    maps = []
    for b in range(ncores):
        m = dict(shared)
        m["xT"] = np.ascontiguousarray(x[b].T)
        maps.append(m)
    return maps


def kernel(**inputs):
    if "nc" not in _NC_CACHE:
        _NC_CACHE["nc"] = Builder().run()
    nc = _NC_CACHE["nc"]
    maps = make_in_maps(inputs)
    res = run_bass_kernel_spmd(nc, maps, core_ids=list(range(8)))
    out = np.stack([np.asarray(r["outT"]).T for r in res.results], axis=0)
    return np.ascontiguousarray(out.astype(np.float32))
```

```python
import numpy as np
from contextlib import ExitStack
import concourse.bass as bass
import concourse.mybir as mybir
from concourse.bass_utils import run_bass_kernel_spmd

F32 = mybir.dt.float32
BF16 = mybir.dt.bfloat16
AF = mybir.ActivationFunctionType
ALU = mybir.AluOpType
AX = mybir.AxisListType

S = 2048
D = 2048
DFF = 8192
NCH = 16
EPS = 1e-6
NIN = (6160, 7176)
NEG = -30000.0

PE, ACT, DVE, POOL, SP = "pe", "act", "dve", "pool", "sp"
ENGS = [PE, ACT, DVE, POOL, SP]
NDMA_SEM = 8

C_ID, C_UB, C_CM, C_ONE, C_ALC, C_PM, C_P01, C_O01 = 0, 128, 256, 384, 512, 640, 768, 896
C_EM, C_EF, C_AR, C_RM, NCST = 1024, 2048, 3072, 5120, 7168
P_NORM = 0
P_GB = 80
P_GN = 84
P_HN = 86
P_LB = 87
P_FB = 103
NPAR = 104


class Op:
    __slots__ = ("eng", "fn", "deps", "dma", "idx", "signal", "cnt", "dsem", "dval", "prev")

    def __init__(self, eng, fn, dma):
        self.eng, self.fn, self.dma = eng, fn, dma
        self.deps = set()
        self.signal = False
        self.cnt = 0
        self.dsem = None
        self.dval = 0
        self.prev = None


class Prog:
    def __init__(self, nc):
        self.nc = nc
        self.ops = []
        self.lastw = {}
        self.readers = {}
        self.last_eng = {}
        self.dmas_since = []

    def op(self, eng, fn, reads=(), writes=(), dma=False):
        o = Op(eng, fn, dma)
        o.idx = len(self.ops)
        pr = [r for r in reads if r.startswith("pb")]
        if pr:
            reads = [r for r in reads if not r.startswith("pb")]
            writes = list(writes) + pr
        for r in reads:
            w = self.lastw.get(r)
            if w is not None:
                o.deps.add(w)
        for r in writes:
            w = self.lastw.get(r)
            if w is not None:
                o.deps.add(w)
            for rd in self.readers.get(r, ()):
                o.deps.add(rd)
        for r in writes:
            self.lastw[r] = o.idx
            self.readers[r] = []
        for r in reads:
            if r not in writes:
                self.readers.setdefault(r, []).append(o.idx)
        o.deps.discard(o.idx)
        self.ops.append(o)
        if dma:
            self.dmas_since.append(o.idx)
        else:
            self.last_eng[eng] = o.idx
        return o

    def barrier(self):
        deps = set(self.last_eng.values()) | set(self.dmas_since)
        for e in ENGS:
            o = Op(e, None, False)
            o.idx = len(self.ops)
            o.deps = set(deps)
            self.ops.append(o)
        self.lastw.clear()
        self.readers.clear()
        self.dmas_since = []

    def emit(self, stack):
        nc = self.nc
        ops = self.ops
        for o in ops:
            best = {}
            nd = set()
            for d in o.deps:
                p = ops[d]
                if p.dma:
                    nd.add(d)
                    continue
                if p.eng == o.eng and o.eng == PE and not o.dma:
                    continue
                if p.eng not in best or best[p.eng] < d:
                    best[p.eng] = d
            nd |= set(best.values())
            o.deps = nd
            for d in nd:
                ops[d].signal = True
        sems = {e: stack.enter_context(nc.semaphore("s_" + e)) for e in ENGS}
        dsems = {e: [stack.enter_context(nc.semaphore("d_%s_%d" % (e, i))) for i in range(NDMA_SEM)]
                 for e in (ACT, POOL, SP)}
        cnt = {e: 0 for e in ENGS}
        per = {e: [] for e in ENGS}
        hist = {e: [] for e in ENGS}
        for o in ops:
            if o.dma:
                j = len(hist[o.eng])
                o.dsem = dsems[o.eng][j % NDMA_SEM]
                o.dval = 16 * (j // NDMA_SEM + 1)
                if j >= NDMA_SEM:
                    o.prev = hist[o.eng][j - NDMA_SEM]
                hist[o.eng].append(o)
            elif o.signal:
                cnt[o.eng] += 1
                o.cnt = cnt[o.eng]
            per[o.eng].append(o)
        self.stats = {e: len(per[e]) for e in ENGS}
        self.stats["sig"] = dict(cnt)
        block = stack.enter_context(nc.Block())

        def run(eng_name, e):
            waited = {}

            def wait(sem, val):
                k = id(sem)
                if waited.get(k, 0) >= val:
                    return
                waited[k] = val
                e.wait_ge(sem, val)

            for o in per[eng_name]:
                need = {}
                for d in o.deps:
                    p = ops[d]
                    s, v = (p.dsem, p.dval) if p.dma else (sems[p.eng], p.cnt)
                    if id(s) not in need or need[id(s)][1] < v:
                        need[id(s)] = (s, v)
                if o.dma and o.prev is not None:
                    s, v = o.prev.dsem, o.prev.dval
                    if id(s) not in need or need[id(s)][1] < v:
                        need[id(s)] = (s, v)
                for s, v in need.values():
                    wait(s, v)
                if o.fn is None:
                    continue
                ins = o.fn(e)
                if o.dma:
                    ins.then_inc(o.dsem, 16)
                elif o.signal:
                    ins.then_inc(sems[o.eng], 1)
            for o in hist[eng_name][-NDMA_SEM:]:
                wait(o.dsem, o.dval)

        block.tensor(lambda e: run(PE, e))
        block.scalar(lambda e: run(ACT, e))
        block.vector(lambda e: run(DVE, e))
        block.gpsimd(lambda e: run(POOL, e))
        block.sync(lambda e: run(SP, e))


class Arena:
    def __init__(self, ap, nwords):
        self.ap = ap
        self.n = nwords
        self.off = 0

    def alloc(self, shape, dtype=F32, parts=128):
        n = int(np.prod(shape))
        words = n if dtype == F32 else (n + 1) // 2
        words = (words + 7) // 8 * 8
        assert self.off + words <= self.n, ("arena overflow", self.off, words, self.n)
        v = self.ap[0:parts, self.off:self.off + words]
        self.off += words
        if dtype != F32:
            v = v.bitcast(dtype)
        v = v[:, 0:n]
        if len(shape) == 2:
            v = v.rearrange("p (a b) -> p a b", a=shape[0])
        elif len(shape) == 3:
            v = v.rearrange("p (a b c) -> p a b c", a=shape[0], b=shape[1])
        return v


class Builder:
    def __init__(self, dbg=None):
        self.dbg = dbg
        nc = self.nc = bass.Bass("TRN2", target_bir_lowering=False)
        self.P = Prog(nc)

        def di(name, shape, dt=F32):
            return nc.dram_tensor(name, shape, dt, kind="ExternalInput").ap()

        self.xT = di("xT", [D, S])
        self.W = []
        for l, p in ((0, "ev"), (1, "od")):
            self.W.append(dict(w_in=di(p + "_w_in", [D, NIN[l]]), w_out=di(p + "_w_out", [D, D]),
                               w_up=di(p + "_w_up", [D, DFF]), w_down=di(p + "_w_down", [DFF, D])))
        self.w2d = di("w2", [16, 512])
        self.prmd = di("prm", [128, NPAR])
        self.cstd = di("cst", [128, NCST])
        self.outT = nc.dram_tensor("outT", [D, S], F32, kind="ExternalOutput").ap()
        self.hT = nc.dram_tensor("hT", [D, S], F32).ap()
        self.mixd = nc.dram_tensor("mixd", [D, S], BF16, kind=("ExternalOutput" if dbg in ("mix", "s2", "s3", "s4", "s5", "s6") else "Internal")).ap()
        if dbg == "h":
            self.hdbg = nc.dram_tensor("hdbg", [D, S], F32, kind="ExternalOutput").ap()

    def bank(self, b, n=512, c0=0):
        return self.PS[:, 512 * b + c0:512 * b + c0 + n]

    def banks4(self, h):
        return self.PS[:, 2048 * h:2048 * h + 2048]

    def pk(self, h):
        return ["pb%d" % b for b in range(4 * h, 4 * h + 4)]

    def run(self):
        nc, P = self.nc, self.P
        with ExitStack() as st:
            NW = 53000
            arena_t = st.enter_context(nc.sbuf_tensor("arena", [128, NW], F32))
            self.PS = st.enter_context(nc.psum_tensor("ps", [128, 4096], F32))
            A = self.A = Arena(arena_t, NW)
            self.XN = A.alloc([NCH, S], BF16)
            self.WB = [A.alloc([NCH, 256], BF16) for _ in range(2)]
            self.HS = [A.alloc([S], F32) for _ in range(2)]
            self.RSTD = A.alloc([S], F32)
            self.SQB = A.alloc([2, S], BF16)
            self.cbf = A.alloc([512], BF16)
            self.EM = A.alloc([8, 128], BF16)
            self.EF = A.alloc([8, 128], BF16)
            self.ROWM = A.alloc([S], BF16)
            self.ROWF = A.alloc([S], BF16)
            self.RM = A.alloc([S], BF16)
            self.cf = A.alloc([1024], F32)
            self.prm = A.alloc([NPAR], F32)
            self.w2 = A.alloc([512], F32)
            self.lbt = A.alloc([32], F32)
            self.mark = A.off
            self.wslot = 0
            self.ident_bf = self.cbf[:, 0:128]
            self.ublk_bf = self.cbf[:, 128:256]
            self.cm_bf = self.cbf[:, 256:384]
            self.ones_bf = self.cbf[:, 384:512]
            self.ident_f = self.cf[:, C_ID:C_ID + 128]
            self.ublk_f = self.cf[:, C_UB:C_UB + 128]

            self.load_consts()
            self.copy_x()
            if self.dbg != "s1":
                self.layer(0)
            if self.dbg not in ("l0", "mix", "s1", "s2", "s3", "s4"):
                self.layer(1)
            self.final_norm()
            P.emit(st)
        return nc

    def load_consts(self):
        P, c = self.P, self.cstd
        P.op(POOL, lambda e: e.dma_start(out=self.cbf, in_=c[:, 0:512]), writes=["cbf"], dma=True)
        P.op(POOL, lambda e: e.dma_start(out=self.EM.rearrange("p a b -> p (a b)"), in_=c[:, C_EM:C_EM + 1024]),
             writes=["EM"], dma=True)
        P.op(POOL, lambda e: e.dma_start(out=self.EF.rearrange("p a b -> p (a b)"), in_=c[:, C_EF:C_EF + 1024]),
             writes=["EF"], dma=True)
        P.op(POOL, lambda e: e.dma_start(out=self.ROWM, in_=c[:, C_AR:C_AR + S]), writes=["ROWM"], dma=True)
        P.op(POOL, lambda e: e.dma_start(out=self.RM, in_=c[:, C_RM:C_RM + S]), writes=["RM"], dma=True)
        P.op(SP, lambda e: e.dma_start(out=self.cf, in_=c[:, 0:1024]), writes=["cf"], dma=True)
        P.op(SP, lambda e: e.dma_start(out=self.prm, in_=self.prmd), writes=["prm"], dma=True)
        P.op(SP, lambda e: e.dma_start(out=self.w2[0:16, :], in_=self.w2d), writes=["w2"], dma=True)
        P.op(DVE, lambda e: e.memset(self.ROWF, 0.0), writes=["ROWF"])

    def copy_x(self):
        for c in range(NCH):
            self.P.op(ACT, lambda e, c=c: e.dma_start(out=self.hT[c * 128:(c + 1) * 128, :],
                                                       in_=self.xT[c * 128:(c + 1) * 128, :]),
                      writes=["hT%d" % c], dma=True)

    def norm(self, gcol, final=False, src=None):
        P, A = self.P, self.A
        hsrc = self.hT if src is None else src
        hkey = (lambda c: ["hT%d" % c]) if src is None else (lambda c: [])
        m0 = A.off
        NBUF = 10
        bufs = [A.alloc([S], F32) for _ in range(NBUF - 2)] + [self.HS[0], self.HS[1]]
        bkey = ["NB%d" % i for i in range(NBUF - 2)] + ["HS0", "HS1"]
        for c in range(NCH):
            hs, hk = bufs[c % NBUF], bkey[c % NBUF]
            sq, sk = self.SQB[:, c % 2, :], "SQB%d" % (c % 2)
            P.op(SP, lambda e, c=c, hs=hs: e.dma_start(out=hs, in_=hsrc[c * 128:(c + 1) * 128, :]),
                 reads=hkey(c), writes=[hk], dma=True)
            P.op(ACT, lambda e, hs=hs, sq=sq: e.activation(out=sq, in_=hs, func=AF.Square),
                 reads=[hk], writes=[sk])
            for n in range(4):
                P.op(PE, lambda e, c=c, n=n, sq=sq: e.matmul(self.bank(n), lhsT=self.ones_bf,
                                                             rhs=sq[:, n * 512:(n + 1) * 512],
                                                             start=(c == 0), stop=(c == NCH - 1)),
                     reads=[sk, "cbf"], writes=["pb%d" % n])
        P.op(ACT, lambda e: e.activation(out=self.RSTD, in_=self.banks4(0), func=AF.Ln, scale=1.0 / D, bias=EPS),
             reads=self.pk(0), writes=["RSTD"])
        P.op(ACT, lambda e: e.activation(out=self.RSTD, in_=self.RSTD, func=AF.Exp, scale=-0.5),
             reads=["RSTD"], writes=["RSTD"])
        first = NCH - NBUF
        order = list(range(first, NCH)) + list(range(0, first))
        for pos, c in enumerate(order):
            if c >= first:
                bi = c % NBUF
            else:
                bi = order[pos - NBUF] % NBUF
            hs, hk = bufs[bi], bkey[bi]
            if c < first:
                P.op(SP, lambda e, c=c, hs=hs: e.dma_start(out=hs, in_=hsrc[c * 128:(c + 1) * 128, :]),
                     reads=hkey(c), writes=[hk], dma=True)
            g = self.prm[:, gcol + c:gcol + c + 1]
            if not final:
                P.op(DVE, lambda e, c=c, hs=hs, g=g: e.scalar_tensor_tensor(
                    out=self.XN[:, c, :], in0=hs, scalar=g, in1=self.RSTD, op0=ALU.mult, op1=ALU.mult),
                    reads=[hk, "RSTD", "prm"], writes=["xn%d" % c])
            else:
                P.op(DVE, lambda e, c=c, hs=hs, g=g: e.scalar_tensor_tensor(
                    out=hs, in0=hs, scalar=g, in1=self.RSTD, op0=ALU.mult, op1=ALU.mult),
                    reads=[hk, "RSTD", "prm"], writes=[hk])
                P.op(SP, lambda e, c=c, hs=hs: e.dma_start(out=self.outT[c * 128:(c + 1) * 128, :], in_=hs),
                     reads=[hk], writes=["out%d" % c], dma=True)
        A.off = m0

    def final_norm(self):
        if self.dbg == "h":
            for c in range(NCH):
                self.P.op(SP, lambda e, c=c: e.dma_start(out=self.hdbg[c * 128:(c + 1) * 128, :],
                                                          in_=self.hT[c * 128:(c + 1) * 128, :]),
                          reads=["hT%d" % c], writes=["hd%d" % c], dma=True)
        self.norm(P_NORM + 64, final=True)

    def load_w(self, wd, r0, nk, c0, ncols):
        s = self.wslot % 2
        self.wslot += 1
        wb, key = self.WB[s], "wb%d" % s
        self.P.op(POOL, lambda e: e.dma_start(
            out=wb[:, 0:nk, 0:ncols],
            in_=wd[r0:r0 + nk * 128, c0:c0 + ncols].rearrange("(c p) n -> p c n", p=128)),
            writes=[key], dma=True)
        return wb, key

    def gemm_fm(self, wd, r0, c0, widths, rhs, rkey, evac, nk=NCH, nbank=4):
        P = self.P
        mi = 0
        i = 0
        while i < len(widths):
            grp = [widths[i]]
            if i + 1 < len(widths) and widths[i] + widths[i + 1] <= 256:
                grp.append(widths[i + 1])
            tot = sum(grp)
            wb, wkey = self.load_w(wd, r0, nk, c0, tot)
            off = 0
            for mw in grp:
                h = self.gpar % 2
                self.gpar += 1
                for k in range(nk):
                    for n in range(nbank):
                        P.op(PE, lambda e, k=k, n=n, h=h, off=off, mw=mw, wb=wb: e.matmul(
                            self.bank(4 * h + n)[0:mw, :], lhsT=wb[:, k, off:off + mw], rhs=rhs(k, n),
                            start=(k == 0), stop=(k == nk - 1)),
                            reads=[wkey, rkey(k)], writes=["pb%d" % (4 * h + n)])
                evac(mi, h)
                mi += 1
                off += mw
            c0 += tot
            i += len(grp)

    def xn_rhs(self, k, n):
        return self.XN[:, k, n * 512:(n + 1) * 512]

    def gemm_tm(self, wd, c0, ncols, vtm, vkey):
        P = self.P
        vT = self.SQB[:, 1, :]

        def ev(mi, h):
            P.op(ACT, lambda e, h=h: e.activation(out=vT, in_=self.banks4(h), func=AF.Copy),
                 reads=self.pk(h), writes=["SQB1"])
            for hb in range(2):
                b = 4 * h + 2 * (self.tmb % 2) + hb
                pbv = self.bank(b).bitcast(BF16)
                for j in range(8):
                    t = hb * 8 + j
                    P.op(PE, lambda e, t=t, j=j, pbv=pbv: e.transpose(pbv[:, j * 128:(j + 1) * 128],
                                                                      vT[:, t * 128:(t + 1) * 128], self.ident_bf),
                         reads=["SQB1", "cbf"], writes=["pb%d" % b])
                P.op(DVE, lambda e, hb=hb, pbv=pbv, mi=mi: e.tensor_copy(
                    out=vtm[:, hb * 8:hb * 8 + 8, mi * 128:(mi + 1) * 128],
                    in_=pbv.rearrange("p (a c) -> p a c", a=8)),
                    reads=["pb%d" % b], writes=[vkey])
            self.tmb += 1
        self.gemm_fm(wd, 0, c0, [128] * (ncols // 128), self.xn_rhs, lambda k: "xn%d" % k, ev)

    def gla_core(self, qg, kg, kgtm, vtm, eb, dvh, oT, St, Sbf, PT):
        P = self.P
        dv = dvh * 128
        P.op(DVE, lambda e: e.memset(St, 0.0), writes=["St"])
        P.op(DVE, lambda e: e.memset(Sbf[0], 0.0), writes=["Sbf0"])
        sidx = 0

        def emit_sc(i):
            tl = slice(i * 128, (i + 1) * 128)
            sb = 4 if i % 2 == 0 else 7
            pt, ptk = PT[i % 2], "PT%d" % (i % 2)
            P.op(PE, lambda e: e.matmul(self.bank(sb, 128), lhsT=kg[:, tl], rhs=qg[:, tl], start=True, stop=True),
                 reads=["kg", "qg"], writes=["pb%d" % sb])
            P.op(DVE, lambda e: e.tensor_tensor(out=pt, in0=self.bank(sb, 128), in1=self.ublk_f, op=ALU.mult),
                 reads=["pb%d" % sb, "cf"], writes=[ptk])

        def ubank(i, cc):
            if dvh == 1 and i % 2 == 1:
                return 1 + 2 * cc
            return 5 + cc

        def emit_u(i):
            for cc in range(2):
                rs = slice(cc * 64, cc * 64 + 64)
                ub = ubank(i, cc)
                P.op(PE, lambda e, cc=cc, rs=rs, ub=ub: e.matmul(
                    self.bank(ub, dv), lhsT=kgtm[rs, i, :], rhs=vtm[rs, i, :], start=True, stop=True),
                    reads=["kgtm", "vtm"], writes=["pb%d" % ub])

        emit_u(0)
        emit_sc(0)
        for i in range(NCH):
            g = i // 4
            pt, ptk = PT[i % 2], "PT%d" % (i % 2)
            obanks = [(2 * (g % 2) + hh) for hh in range(dvh)]
            col = (i % 4) * 128
            for hh in range(dvh):
                ob = obanks[hh]
                P.op(PE, lambda e, i=i, hh=hh, ob=ob, col=col, pt=pt: e.matmul(
                    self.bank(ob, 128, col), lhsT=vtm[:, i, hh * 128:(hh + 1) * 128], rhs=pt, start=True, stop=False),
                    reads=["vtm", ptk], writes=["pb%d" % ob])
            if i + 1 < NCH:
                emit_sc(i + 1)
                if dvh == 1:
                    emit_u(i + 1)
            for cc in range(2):
                ub = ubank(i, cc)
                c = 2 * i + cc
                cur, nxt = Sbf[sidx % 2], Sbf[(sidx + 1) % 2]
                ck, nk_ = "Sbf%d" % (sidx % 2), "Sbf%d" % ((sidx + 1) % 2)
                qs = slice(i * 128 + cc * 64, i * 128 + cc * 64 + 64)
                for hh in range(dvh):
                    ob = obanks[hh]
                    P.op(PE, lambda e, hh=hh, ob=ob, col=col, cc=cc, cur=cur, qs=qs: e.matmul(
                        self.bank(ob, 64, col + cc * 64), lhsT=cur[:, hh * 128:(hh + 1) * 128], rhs=qg[:, qs],
                        start=False, stop=(cc == 1)),
                        reads=[ck, "qg"], writes=["pb%d" % ob])
                if c == 2 * NCH - 1:
                    break
                el = eb[:, c * 64 + 63:c * 64 + 64]
                P.op(DVE, lambda e, cc=cc, nxt=nxt, el=el, ub=ub: e.scalar_tensor_tensor(
                    out=nxt, in0=St, scalar=el, in1=self.bank(ub, dv), op0=ALU.mult, op1=ALU.add),
                    reads=["pb%d" % ub, "St", "eb"], writes=[nk_])
                P.op(DVE, lambda e, cc=cc, el=el, ub=ub: e.scalar_tensor_tensor(
                    out=St, in0=St, scalar=el, in1=self.bank(ub, dv), op0=ALU.mult, op1=ALU.add),
                    reads=["pb%d" % ub, "St", "eb"], writes=["St"])
                sidx += 1
            if dvh != 1 and i + 1 < NCH:
                emit_u(i + 1)
            if i % 4 == 3:
                for hh in range(dvh):
                    ob = obanks[hh]
                    P.op(ACT, lambda e, hh=hh, ob=ob, g=g: e.activation(
                        out=oT[:, hh, g * 512:(g + 1) * 512], in_=self.bank(ob), func=AF.Copy),
                        reads=["pb%d" % ob], writes=["oT"])

    def transpose_kg(self, kg, kgtm):
        P = self.P
        for hb in range(2):
            b = 6 + hb
            pbv = self.bank(b).bitcast(BF16)
            for j in range(8):
                t = hb * 8 + j
                P.op(PE, lambda e, t=t, j=j, pbv=pbv: e.transpose(pbv[:, j * 128:(j + 1) * 128],
                                                                  kg[:, t * 128:(t + 1) * 128], self.ident_bf),
                     reads=["SQB0", "cbf"], writes=["pb%d" % b])
            P.op(DVE, lambda e, hb=hb, pbv=pbv: e.tensor_copy(
                out=kgtm[:, hb * 8:hb * 8 + 8, :], in_=pbv.rearrange("p (a c) -> p a c", a=8)),
                reads=["pb%d" % b], writes=["kgtm"])

    def gated_out(self, oT, sg, dvh, gain_col, row0):
        P = self.P
        dv = dvh * 128
        for hh in range(dvh):
            P.op(ACT, lambda e, hh=hh: e.activation(out=self.SQB[:, hh, :], in_=oT[:, hh, :], func=AF.Square),
                 reads=["oT"], writes=["SQB%d" % hh])
        for n in range(4):
            for hh in range(dvh):
                P.op(PE, lambda e, n=n, hh=hh: e.matmul(self.bank(4 + n), lhsT=self.ones_bf,
                                                        rhs=self.SQB[:, hh, n * 512:(n + 1) * 512],
                                                        start=(hh == 0), stop=(hh == dvh - 1)),
                     reads=["SQB%d" % hh, "cbf"], writes=["pb%d" % (4 + n)])
        P.op(ACT, lambda e: e.activation(out=self.RSTD, in_=self.banks4(1), func=AF.Ln, scale=1.0 / dv, bias=EPS),
             reads=self.pk(1), writes=["RSTD"])
        P.op(ACT, lambda e: e.activation(out=self.RSTD, in_=self.RSTD, func=AF.Exp, scale=-0.5),
             reads=["RSTD"], writes=["RSTD"])
        for hh in range(dvh):
            g = self.prm[:, gain_col + hh:gain_col + hh + 1]
            P.op(DVE, lambda e, hh=hh, g=g: e.scalar_tensor_tensor(
                out=oT[:, hh, :], in0=oT[:, hh, :], scalar=g, in1=self.RSTD, op0=ALU.mult, op1=ALU.mult),
                reads=["oT", "RSTD", "prm"], writes=["oT"])
            ms = self.HS[1].bitcast(BF16)[:, hh * S:(hh + 1) * S]
            P.op(DVE, lambda e, hh=hh, ms=ms: e.tensor_tensor(out=ms, in0=oT[:, hh, :], in1=sg[:, hh, :], op=ALU.mult),
                 reads=["oT", "sg"], writes=["ms%d" % hh])
            r = row0 + hh * 128
            P.op(SP, lambda e, ms=ms, r=r: e.dma_start(out=self.mixd[r:r + 128, :], in_=ms),
                 reads=["ms%d" % hh], writes=["mix%d" % (r // 128)], dma=True)

    def attention(self, qT, kT, vtm, bias_col, bias_key, row_lhsT, row_rhs, nrow, row_keys, Pb, osb, row0):
        P = self.P
        steps = [(n, j) for n in range(4) for j in range(4 * n + 4)]

        def geo(s):
            n, j = steps[s]
            c0 = max(0, j - 4 * n) * 128
            return n, j, c0, 512 - c0, s % 4, Pb[s % 3], "Pb%d" % (s % 3)

        def emit_qk(s):
            n, j, c0, N, sb, pb, pbk = geo(s)
            diag = j >= 4 * n
            tq = slice(512 * n + c0, 512 * (n + 1))
            P.op(PE, lambda e: e.matmul(self.bank(sb, N, c0), lhsT=kT[:, j * 128:(j + 1) * 128], rhs=qT[:, tq],
                                        start=True, stop=False),
                 reads=["kT", "qT"], writes=["pb%d" % sb])
            P.op(PE, lambda e: e.matmul(self.bank(sb, N, c0), lhsT=row_lhsT(j), rhs=row_rhs[0:nrow, tq],
                                        start=False, stop=(not diag)),
                 reads=row_keys, writes=["pb%d" % sb])
            if diag:
                P.op(PE, lambda e: e.matmul(self.bank(sb, 128, c0), lhsT=self.ident_bf, rhs=self.cm_bf,
                                            start=False, stop=True),
                     reads=["cbf"], writes=["pb%d" % sb])
            P.op(ACT, lambda e: e.activation(out=pb[:, c0:512], in_=self.bank(sb, N, c0), func=AF.Exp, bias=bias_col(j)),
                 reads=["pb%d" % sb, bias_key], writes=[pbk])

        def emit_pv(s):
            n, j, c0, N, sb, pb, pbk = geo(s)
            ob, lb = (4, 5) if n % 2 == 0 else (6, 7)
            jmax = 4 * n + 3
            P.op(PE, lambda e: e.matmul(self.bank(ob, N, c0), lhsT=vtm[:, j, :], rhs=pb[:, c0:512],
                                        start=(j == 0), stop=(j == jmax)),
                 reads=["vtm", pbk], writes=["pb%d" % ob])
            P.op(PE, lambda e: e.matmul(self.bank(lb, N, c0), lhsT=self.ones_bf, rhs=pb[:, c0:512],
                                        start=(j == 0), stop=(j == jmax)),
                 reads=["cbf", pbk], writes=["pb%d" % lb])
            if j == jmax:
                rl, rk = osb[n % 2], "osb%d" % (n % 2)
                P.op(DVE, lambda e: e.reciprocal(out=rl, in_=self.bank(lb)), reads=["pb%d" % lb], writes=[rk])
                ms = self.HS[1].bitcast(BF16)[:, n * 512:(n + 1) * 512]
                P.op(DVE, lambda e: e.tensor_tensor(out=ms, in0=self.bank(ob), in1=rl, op=ALU.mult),
                     reads=["pb%d" % ob, rk], writes=["ms%d" % n])
                P.op(SP, lambda e: e.dma_start(out=self.mixd[row0:row0 + 128, n * 512:(n + 1) * 512], in_=ms),
                     reads=["ms%d" % n], writes=["mix%d_%d" % (row0 // 128, n)], dma=True)

        emit_qk(0)
        emit_qk(1)
        for s in range(len(steps)):
            emit_pv(s)
            if s + 2 < len(steps):
                emit_qk(s + 2)

    def layer(self, l):
        P = self.P
        W = self.W[l]
        self.gpar = 0
        self.tmb = 0
        self.norm(P_NORM + 32 * l, src=(self.xT if l == 0 else None))
        P.barrier()
        if l == 0:
            self.mix_even(W["w_in"])
        else:
            self.mix_odd(W["w_in"])
        P.barrier()
        self.A.off = self.mark
        if self.dbg in ("s2", "s3", "s5", "s6"):
            return
        P.op(SP, lambda e: e.dma_start(out=self.XN[:, 0:8, :],
                                       in_=self.mixd[0:1024, :].rearrange("(c p) t -> p c t", p=128)),
             writes=["xn%d" % c for c in range(8)], dma=True)
        P.op(SP, lambda e: e.dma_start(out=self.XN[:, 8:16, :],
                                       in_=self.mixd[1024:2048, :].rearrange("(c p) t -> p c t", p=128)),
             writes=["xn%d" % c for c in range(8, 16)], dma=True)
        self.gemm_fm(W["w_out"], 0, 0, [128] * 16, self.xn_rhs, lambda k: "xn%d" % k, self.evac_resid(0))
        P.barrier()
        self.norm(P_NORM + 32 * l + 16)
        P.barrier()
        aT = self.A.alloc([NCH, S], BF16)
        for fg in range(4):
            def evac_up(mi, h, aT=aT):
                hs, hk = self.HS[mi % 2], "HS%d" % (mi % 2)
                P.op(ACT, lambda e, h=h, hs=hs: e.activation(out=hs, in_=self.banks4(h), func=AF.Relu),
                     reads=self.pk(h), writes=[hk])
                P.op(DVE, lambda e, mi=mi, hs=hs: e.tensor_tensor(out=aT[:, mi, :], in0=hs, in1=hs, op=ALU.mult),
                     reads=[hk], writes=["aT%d" % mi])
            self.gemm_fm(W["w_up"], 0, fg * 2048, [128] * 16, self.xn_rhs, lambda k: "xn%d" % k, evac_up)
            self.gemm_fm(W["w_down"], fg * 2048, 0, [128] * 16,
                         lambda k, n, aT=aT: aT[:, k, n * 512:(n + 1) * 512], lambda k: "aT%d" % k,
                         self.evac_resid(0))
        P.barrier()
        self.A.off = self.mark

    def evac_resid(self, _):
        P = self.P

        def pre(mi):
            hs, hk = self.HS[mi % 2], "HS%d" % (mi % 2)
            P.op(SP, lambda e, mi=mi, hs=hs: e.dma_start(out=hs, in_=self.hT[mi * 128:(mi + 1) * 128, :]),
                 reads=["hT%d" % mi], writes=[hk], dma=True)

        def ev(mi, h):
            hs, hk = self.HS[mi % 2], "HS%d" % (mi % 2)
            if mi < 2:
                pre(mi)
            P.op(DVE, lambda e, h=h, hs=hs: e.tensor_tensor(out=hs, in0=self.banks4(h), in1=hs, op=ALU.add),
                 reads=self.pk(h) + [hk], writes=[hk])
            P.op(SP, lambda e, mi=mi, hs=hs: e.dma_start(out=self.hT[mi * 128:(mi + 1) * 128, :], in_=hs),
                 reads=[hk], writes=["hT%d" % mi], dma=True)
            if mi + 2 < NCH:
                pre(mi + 2)
        return ev

    def mix_even(self, w_in):
        P, A = self.P, self.A
        xk = lambda k: "xn%d" % k
        glrT = self.HS[0]
        def ev_glr(mi, h):
            P.op(ACT, lambda e, h=h: e.activation(out=glrT[0:16, :], in_=self.banks4(h)[0:16, :], func=AF.Copy),
                 reads=self.pk(h), writes=["glr"])
        self.gemm_fm(w_in, 0, 3072, [16], self.xn_rhs, xk, ev_glr)
        m0 = A.off
        eb, enb, ebl = A.alloc([S]), A.alloc([S]), A.alloc([S])
        qg, kg = A.alloc([S], BF16), A.alloc([S], BF16)
        kgl = self.SQB[:, 0, :]
        c3 = lambda a: a.rearrange("p (c j) -> p c j", j=64)
        kgtm = A.alloc([NCH, 128], BF16)
        vtm = A.alloc([NCH, 256], BF16)
        sg = A.alloc([2, S], BF16)
        oT = A.alloc([2, S])
        St = A.alloc([256])
        Sbf = [A.alloc([256], BF16) for _ in range(2)]
        PT = [A.alloc([128], BF16) for _ in range(2)]
        cs = self.RSTD
        for hd in ([] if self.dbg == "s3" else [0] if self.dbg == "s2" else range(4)):
            for n in range(4):
                P.op(PE, lambda e, hd=hd, n=n: e.matmul(self.bank(n), lhsT=self.w2[0:16, hd * 128:(hd + 1) * 128],
                                                       rhs=glrT[0:16, n * 512:(n + 1) * 512], start=True, stop=True),
                     reads=["w2", "glr"], writes=["pb%d" % n])
            nb = self.lbt[:, 16 + hd:17 + hd]
            P.op(DVE, lambda e, hd=hd, nb=nb: e.tensor_scalar(out=nb, in0=self.prm[:, P_GB + hd:P_GB + hd + 1],
                                                             scalar1=-1.0, scalar2=None, op0=ALU.mult),
                 reads=["prm"], writes=["nb"])
            P.op(ACT, lambda e, nb=nb: e.activation(out=eb, in_=self.banks4(0), func=AF.Exp, scale=-1.0, bias=nb),
                 reads=self.pk(0) + ["nb"], writes=["eb"])
            P.op(ACT, lambda e: e.activation(out=eb, in_=eb, func=AF.Ln, bias=1.0), reads=["eb"], writes=["eb"])
            P.op(DVE, lambda e: e.tensor_tensor_scan(out=cs, data0=self.RM, data1=eb, initial=0.0,
                                                     op0=ALU.mult, op1=ALU.add),
                 reads=["RM", "eb"], writes=["RSTD"])
            P.op(ACT, lambda e: e.activation(out=eb, in_=cs, func=AF.Exp, scale=-1.0 / 16), reads=["RSTD"], writes=["eb"])
            P.op(ACT, lambda e: e.activation(out=enb, in_=cs, func=AF.Exp, scale=1.0 / 16), reads=["RSTD"], writes=["enb"])
            P.op(DVE, lambda e: e.tensor_tensor(out=c3(ebl), in0=c3(enb), in1=c3(eb)[:, :, 63:64].to_broadcast([128, 32, 64]),
                                                op=ALU.mult), reads=["enb", "eb"], writes=["ebl"])

            def ev_q(mi, h):
                P.op(DVE, lambda e, h=h: e.scalar_tensor_tensor(out=qg, in0=self.banks4(h), scalar=128 ** -0.5, in1=eb,
                                                                op0=ALU.mult, op1=ALU.mult),
                     reads=self.pk(h) + ["eb"], writes=["qg"])

            def ev_k(mi, h):
                P.op(DVE, lambda e, h=h: e.tensor_tensor(out=kg, in0=self.banks4(h), in1=enb, op=ALU.mult),
                     reads=self.pk(h) + ["enb"], writes=["kg"])
                P.op(DVE, lambda e, h=h: e.tensor_tensor(out=kgl, in0=self.banks4(h), in1=ebl, op=ALU.mult),
                     reads=self.pk(h) + ["ebl"], writes=["SQB0"])

            def ev_g(mi, h):
                P.op(ACT, lambda e, mi=mi, h=h: e.activation(out=sg[:, mi, :], in_=self.banks4(h), func=AF.Silu),
                     reads=self.pk(h), writes=["sg"])
            self.gemm_fm(w_in, 0, hd * 128, [128], self.xn_rhs, xk, ev_q)
            self.gemm_fm(w_in, 0, 512 + hd * 128, [128], self.xn_rhs, xk, ev_k)
            self.gemm_fm(w_in, 0, 2048 + hd * 256, [128, 128], self.xn_rhs, xk, ev_g)
            self.gemm_tm(w_in, 1024 + hd * 256, 256, vtm, "vtm")
            self.transpose_kg(kgl, kgtm)
            self.gla_core(qg, kg, kgtm, vtm, eb, 2, oT, St, Sbf, PT)
            self.gated_out(oT, sg, 2, P_GN, hd * 256)
        P.barrier()
        A.off = m0
        qT, kT = A.alloc([S], BF16), A.alloc([S], BF16)
        vt = A.alloc([NCH, 128], BF16)
        Pb = [A.alloc([512], BF16) for _ in range(3)]
        osb = [A.alloc([512]) for _ in range(2)]
        Gm = A.alloc([NCH, 8])
        m8 = A.alloc([NCH, 8])
        sel = A.alloc([NCH, 8])
        ksum = A.alloc([8])
        kmT = A.alloc([8], BF16)
        Eh = A.alloc([8, 128], BF16)
        pm = self.cf[:, C_PM:C_PM + 128].rearrange("p (a b) -> p a b", a=NCH)
        p01 = self.cf[:, C_P01:C_P01 + 128].rearrange("p (a b) -> p a b", a=NCH)
        o01 = self.cf[:, C_O01:C_O01 + 128].rearrange("p (a b) -> p a b", a=NCH)
        for hd in ([] if self.dbg == "s2" else [0, 5] if self.dbg == "s3" else range(8)):
            slope = 2.0 ** (-(hd + 1))

            def ev_q(mi, h):
                P.op(DVE, lambda e, h=h: e.tensor_scalar(out=qT, in0=self.banks4(h), scalar1=128 ** -0.5, scalar2=None,
                                                         op0=ALU.mult),
                     reads=self.pk(h), writes=["qT"])

            def ev_k(mi, h):
                for n in range(8):
                    P.op(DVE, lambda e, h=h, n=n: e.tensor_scalar(
                        out=kT[:, n * 256:(n + 1) * 256], in0=self.banks4(h)[:, n * 256:(n + 1) * 256], scalar1=1.0,
                        scalar2=None, op0=ALU.mult, op1=ALU.add, accum_out=ksum[:, n:n + 1]),
                        reads=self.pk(h), writes=["kT", "ksum"])
                P.op(DVE, lambda e: e.tensor_scalar(out=kmT, in0=ksum, scalar1=1.0 / 256, scalar2=None, op0=ALU.mult),
                     reads=["ksum"], writes=["kmT"])
            self.gemm_fm(w_in, 0, 3088 + hd * 128, [128], self.xn_rhs, xk, ev_q)
            self.gemm_fm(w_in, 0, 4112 + hd * 128, [128], self.xn_rhs, xk, ev_k)
            self.gemm_tm(w_in, 5136 + hd * 128, 128, vt, "vtm")
            import os
            stop = os.environ.get("MOBA_STOP", "")
            if stop in ("a", "a0"):
                continue
            for i in range(NCH):
                P.op(PE, lambda e, i=i: e.matmul(self.bank(0, 8, i * 8), lhsT=qT[:, i * 128:(i + 1) * 128], rhs=kmT,
                                                start=True, stop=True),
                     reads=["qT", "kmT"], writes=["pb0"])
            P.op(DVE, lambda e: e.tensor_tensor(out=Gm, in0=self.bank(0, 128).rearrange("p (a b) -> p a b", a=NCH),
                                                in1=pm, op=ALU.add),
                 reads=["pb0", "cf"], writes=["Gm"])
            for i in range(NCH):
                P.op(DVE, lambda e, i=i: e.max(out=m8[:, i, :], in_=Gm[:, i, :]), reads=["Gm"], writes=["m8"])
            P.op(DVE, lambda e: e.tensor_tensor(out=sel, in0=Gm, in1=m8[:, :, 2:3].to_broadcast([128, NCH, 8]),
                                                op=ALU.is_ge),
                 reads=["Gm", "m8"], writes=["sel"])
            P.op(DVE, lambda e: e.tensor_tensor(out=sel, in0=sel, in1=p01, op=ALU.mult),
                 reads=["sel", "cf"], writes=["sel"])
            P.op(DVE, lambda e: e.tensor_tensor(out=sel, in0=sel, in1=o01, op=ALU.add),
                 reads=["sel", "cf"], writes=["sel"])
            P.op(DVE, lambda e: e.tensor_scalar(out=sel, in0=sel, scalar1=-1.0, scalar2=-NEG, op0=ALU.add, op1=ALU.mult),
                 reads=["sel"], writes=["sel"])
            if stop == "b":
                continue
            for i in range(NCH):
                P.op(PE, lambda e, i=i: e.transpose(self.bank(4 + i // 4, 128, (i % 4) * 128)[0:8, :],
                                                   sel[:, i, :], self.ident_f),
                     reads=["sel", "cf"], writes=["pb%d" % (4 + i // 4)])
            P.op(ACT, lambda e: e.activation(out=self.ROWM[0:8, :], in_=self.banks4(1)[0:8, :], func=AF.Copy),
                 reads=self.pk(1), writes=["ROWM"])
            P.op(DVE, lambda e: e.tensor_copy(out=Eh[0:64], in_=self.EM[0:64]), reads=["EM"], writes=["Eh"])
            P.op(DVE, lambda e, slope=slope: e.tensor_scalar(out=Eh[32:34], in0=self.EM[32:34], scalar1=slope,
                                                            scalar2=None, op0=ALU.mult),
                 reads=["EM", "Eh"], writes=["Eh"])
            if stop == "c":
                continue
            self.attention(qT, kT, vt,
                           lambda j, hd=hd: self.cf[:, C_ALC + hd * 16 + j:C_ALC + hd * 16 + j + 1], "cf",
                           lambda j: Eh[0:64, j // 2, :], self.ROWM, 64, ["Eh", "ROWM"], Pb, osb, 1024 + hd * 128)
        P.barrier()
        A.off = m0

    def mix_odd(self, w_in):
        P, A = self.P, self.A
        xk = lambda k: "xn%d" % k
        m0 = A.off
        lbt = self.lbt
        P.op(ACT, lambda e: e.activation(out=lbt[:, 0:16], in_=self.prm[:, P_LB:P_LB + 16], func=AF.Exp),
             reads=["prm"], writes=["lbt"])
        P.op(DVE, lambda e: e.tensor_tensor(out=lbt[:, 0:8], in0=lbt[:, 0:8], in1=lbt[:, 8:16], op=ALU.add),
             reads=["lbt"], writes=["lbt"])
        P.op(DVE, lambda e: e.reciprocal(out=lbt[:, 0:8], in_=lbt[:, 0:8]), reads=["lbt"], writes=["lbt"])
        P.op(DVE, lambda e: e.tensor_tensor(out=lbt[:, 8:16], in0=lbt[:, 8:16], in1=lbt[:, 0:8], op=ALU.mult),
             reads=["lbt"], writes=["lbt"])
        P.op(DVE, lambda e: e.tensor_scalar(out=lbt[:, 0:8], in0=lbt[:, 8:16], scalar1=-1.0, scalar2=1.0,
                                            op0=ALU.mult, op1=ALU.add),
             reads=["lbt"], writes=["lbt"])
        lf = self.HS[0]
        cs = self.RSTD
        cstm = A.alloc([NCH, 8])
        tmp = A.alloc([S])
        nfb = lbt[:, 24:25]
        P.op(DVE, lambda e: e.tensor_scalar(out=nfb[0:8], in0=self.prm[0:8, P_FB:P_FB + 1], scalar1=-1.0, scalar2=None,
                                            op0=ALU.mult), reads=["prm"], writes=["nfb"])

        def ev_f(mi, h):
            P.op(ACT, lambda e, h=h: e.activation(out=lf[0:8, :], in_=self.banks4(h)[0:8, :], func=AF.Exp, scale=-1.0,
                                                  bias=nfb[0:8]), reads=self.pk(h) + ["nfb"], writes=["lf"])
        self.gemm_fm(w_in, 0, 3072, [8], self.xn_rhs, xk, ev_f)
        P.op(ACT, lambda e: e.activation(out=lf[0:8, :], in_=lf[0:8, :], func=AF.Ln, bias=1.0), reads=["lf"], writes=["lf"])
        P.op(DVE, lambda e: e.tensor_tensor_scan(out=cs[0:8, :], data0=self.cbf[0:8, 384:385].to_broadcast([8, S]),
                                                 data1=lf[0:8, :], initial=0.0, op0=ALU.mult, op1=ALU.add),
             reads=["cbf", "lf"], writes=["RSTD"])
        P.op(DVE, lambda e: e.tensor_copy(out=self.ROWF[0:8, :], in_=cs[0:8, :]), reads=["RSTD"], writes=["ROWF"])
        P.op(DVE, lambda e: e.tensor_tensor(out=tmp[0:8, :], in0=cs[0:8, :], in1=self.ROWF[0:8, :], op=ALU.subtract),
             reads=["RSTD", "ROWF"], writes=["tmp"])
        P.op(SP, lambda e: e.dma_start(out=tmp[32:40, :], in_=tmp[0:8, :]), reads=["tmp"], writes=["tmp32"], dma=True)
        P.op(DVE, lambda e: e.tensor_copy(out=self.ROWF[32:40, :], in_=tmp[32:40, :]), reads=["tmp32"], writes=["ROWF"])
        P.op(DVE, lambda e: e.tensor_tensor(out=tmp[32:40, :], in0=tmp[32:40, :], in1=self.ROWF[32:40, :],
                                            op=ALU.subtract), reads=["tmp32", "ROWF"], writes=["tmp32"])
        P.op(SP, lambda e: e.dma_start(out=tmp[64:72, :], in_=tmp[32:40, :]), reads=["tmp32"], writes=["tmp64"], dma=True)
        P.op(DVE, lambda e: e.tensor_copy(out=self.ROWF[64:72, :], in_=tmp[64:72, :]), reads=["tmp64"], writes=["ROWF"])
        for i in range(NCH):
            P.op(PE, lambda e, i=i: e.transpose(self.bank(0, 8, i * 8), cs[0:8, i * 128:(i + 1) * 128],
                                               self.ident_f[0:8, 0:8]),
                 reads=["RSTD", "cf"], writes=["pb0"])
        P.op(DVE, lambda e: e.tensor_copy(out=cstm, in_=self.bank(0, 128).rearrange("p (a b) -> p a b", a=NCH)),
             reads=["pb0"], writes=["bcol"])
        qT, kT = A.alloc([S], BF16), A.alloc([S], BF16)
        vt = A.alloc([NCH, 128], BF16)
        Pb = [A.alloc([512], BF16) for _ in range(3)]
        osb = [A.alloc([512]) for _ in range(2)]
        for hd in range(8):
            def ev_q(mi, h):
                P.op(DVE, lambda e, h=h: e.tensor_scalar(out=qT, in0=self.banks4(h), scalar1=128 ** -0.5, scalar2=None,
                                                         op0=ALU.mult),
                     reads=self.pk(h), writes=["qT"])

            def ev_k(mi, h):
                P.op(ACT, lambda e, h=h: e.activation(out=kT, in_=self.banks4(h), func=AF.Copy),
                     reads=self.pk(h), writes=["kT"])
            self.gemm_fm(w_in, 0, hd * 128, [128], self.xn_rhs, xk, ev_q)
            self.gemm_fm(w_in, 0, 1024 + hd * 128, [128], self.xn_rhs, xk, ev_k)
            self.gemm_tm(w_in, 2048 + hd * 128, 128, vt, "vtm")
            self.attention(qT, kT, vt, lambda j, hd=hd: cstm[:, j, hd:hd + 1], "bcol",
                           lambda j, hd=hd: self.EF[:, hd, :], self.ROWF, 128, ["EF", "ROWF"], Pb, osb, hd * 128)
        P.barrier()
        A.off = m0
        F1, eb, enb, ebl = A.alloc([S]), A.alloc([S]), A.alloc([S]), A.alloc([S])
        qg, kg = A.alloc([S], BF16), A.alloc([S], BF16)
        kgl = self.SQB[:, 0, :]
        c3 = lambda a: a.rearrange("p (c j) -> p c j", j=64)
        kgtm = A.alloc([NCH, 128], BF16)
        vtm = A.alloc([NCH, 128], BF16)
        sg = A.alloc([1, S], BF16)
        oT = A.alloc([1, S])
        St = A.alloc([128])
        Sbf = [A.alloc([128], BF16) for _ in range(2)]
        PT = [A.alloc([128], BF16) for _ in range(2)]
        for hd in range(8):
            lb = lbt[:, 8 + hd:9 + hd]
            oml = lbt[:, hd:hd + 1]

            def ev_f(mi, h, oml=oml, lb=lb):
                P.op(ACT, lambda e, h=h: e.activation(out=F1, in_=self.banks4(h), func=AF.Sigmoid),
                     reads=self.pk(h), writes=["F1"])
                P.op(DVE, lambda e, oml=oml, lb=lb: e.tensor_scalar(out=F1, in0=F1, scalar1=oml, scalar2=lb,
                                                                   op0=ALU.mult, op1=ALU.add),
                     reads=["F1", "lbt"], writes=["F1"])
                P.op(ACT, lambda e: e.activation(out=eb, in_=F1, func=AF.Ln), reads=["F1"], writes=["eb"])
                P.op(DVE, lambda e: e.tensor_tensor_scan(out=cs, data0=self.RM, data1=eb, initial=0.0,
                                                         op0=ALU.mult, op1=ALU.add),
                     reads=["RM", "eb"], writes=["RSTD"])
                P.op(ACT, lambda e: e.activation(out=enb, in_=cs, func=AF.Exp, scale=-1.0), reads=["RSTD"], writes=["enb"])
                P.op(ACT, lambda e: e.activation(out=eb, in_=cs, func=AF.Exp), reads=["RSTD"], writes=["eb"])
                P.op(DVE, lambda e: e.tensor_scalar(out=F1, in0=F1, scalar1=-1.0, scalar2=1.0, op0=ALU.mult, op1=ALU.add),
                     reads=["F1"], writes=["F1"])
                P.op(DVE, lambda e: e.tensor_tensor(out=kg, in0=F1, in1=enb, op=ALU.mult),
                     reads=["F1", "enb"], writes=["kg"])
                P.op(DVE, lambda e: e.tensor_tensor(out=c3(ebl), in0=c3(enb),
                                                    in1=c3(eb)[:, :, 63:64].to_broadcast([128, 32, 64]), op=ALU.mult),
                     reads=["enb", "eb"], writes=["ebl"])
                P.op(DVE, lambda e: e.tensor_tensor(out=kgl, in0=F1, in1=ebl, op=ALU.mult),
                     reads=["F1", "ebl"], writes=["SQB0"])

            def ev_q(mi, h):
                P.op(ACT, lambda e, h=h: e.activation(out=F1, in_=self.banks4(h), func=AF.Silu),
                     reads=self.pk(h), writes=["F1"])
                P.op(DVE, lambda e: e.tensor_tensor(out=qg, in0=F1, in1=eb, op=ALU.mult),
                     reads=["F1", "eb"], writes=["qg"])

            def ev_g(mi, h):
                P.op(ACT, lambda e, h=h: e.activation(out=sg[:, 0, :], in_=self.banks4(h), func=AF.Silu),
                     reads=self.pk(h), writes=["sg"])
            self.gemm_fm(w_in, 0, 4104 + hd * 128, [128], self.xn_rhs, xk, ev_f)
            self.gemm_fm(w_in, 0, 3080 + hd * 128, [128], self.xn_rhs, xk, ev_q)
            self.gemm_fm(w_in, 0, 6152 + hd * 128, [128], self.xn_rhs, xk, ev_g)
            self.gemm_tm(w_in, 5128 + hd * 128, 128, vtm, "vtm")
            self.transpose_kg(kgl, kgtm)
            self.gla_core(qg, kg, kgtm, vtm, eb, 1, oT, St, Sbf, PT)
            self.gated_out(oT, sg, 1, P_HN, 1024 + hd * 128)
        P.barrier()
        A.off = m0


def _consts():
    c = np.zeros((128, NCST), np.float32)
    p = np.arange(128)
    c[:, C_ID:C_ID + 128] = np.eye(128, dtype=np.float32)
    s, t = p[:, None], p[None, :]
    c[:, C_UB:C_UB + 128] = ((s <= t) & (s // 64 == t // 64)).astype(np.float32)
    c[:, C_CM:C_CM + 128] = np.where(s <= t, 0.0, NEG).astype(np.float32)
    c[:, C_ONE:C_ONE + 128] = 1.0
    for h in range(8):
        for j in range(16):
            c[:, C_ALC + h * 16 + j] = (2.0 ** (-(h + 1))) * (128 * j + p)
    for i in range(16):
        for n in range(8):
            c[:, C_PM + i * 8 + n] = 0.0 if n < i // 2 else -1e30
            c[:, C_P01 + i * 8 + n] = 1.0 if n < i // 2 else 0.0
            c[:, C_O01 + i * 8 + n] = 1.0 if n == i // 2 else 0.0
    for n in range(8):
        c[n, C_EM + n * 128:C_EM + (n + 1) * 128] = 1.0
        c[32:34, C_EM + n * 128:C_EM + (n + 1) * 128] = 1.0
        for r in (n, 32 + n, 64 + n):
            c[r, C_EF + n * 128:C_EF + (n + 1) * 128] = -1.0
    tt = np.arange(S)
    c[32, C_AR:C_AR + S] = -128.0 * (tt // 128)
    c[33, C_AR:C_AR + S] = -(tt % 128).astype(np.float32)
    c[:, C_RM:C_RM + S] = (tt % 64 != 0).astype(np.float32)[None, :]
    return c


def _params(inp):
    pr = np.zeros((128, NPAR), np.float32)
    fm = lambda v: np.asarray(v, np.float32).reshape(-1, 128).T
    pr[:, 0:16] = fm(inp["ev_norm_mix"][0])
    pr[:, 16:32] = fm(inp["ev_norm_mlp"][0])
    pr[:, 32:48] = fm(inp["od_norm_mix"][0])
    pr[:, 48:64] = fm(inp["od_norm_mlp"][0])
    pr[:, 64:80] = fm(inp["final_norm"])
    pr[:, P_GB:P_GB + 4] = fm(inp["ev_gla_gate_b"][0])
    pr[:, P_GN:P_GN + 2] = fm(inp["ev_gla_out_norm"][0])
    pr[:, P_HN:P_HN + 1] = fm(inp["od_hgrn_out_norm"][0])
    pr[:, P_LB:P_LB + 8] = fm(inp["hgrn_lb_raw"][0])
    pr[:, P_LB + 8:P_LB + 16] = fm(inp["hgrn_lb_raw"][1])
    pr[0:8, P_FB] = np.asarray(inp["od_fox_fgate_b"][0], np.float32)
    return pr


_NC_CACHE = {}


def make_in_maps(inp, ncores=8):
    cst = _consts()
    prm = _params(inp)
    shared = {"w2": np.ascontiguousarray(np.asarray(inp["ev_gla_gate_w2"][0], np.float32)), "prm": prm, "cst": cst}
    for p in ("ev", "od"):
        for w in ("w_in", "w_out", "w_up", "w_down"):
            shared["%s_%s" % (p, w)] = np.ascontiguousarray(np.asarray(inp["%s_%s" % (p, w)][0], np.float32))
    x = np.asarray(inp["x"], np.float32)
    maps = []
    for b in range(ncores):
        m = dict(shared)
        m["xT"] = np.ascontiguousarray(x[b].T)
        maps.append(m)
    return maps


def kernel(**inputs):
    if "nc" not in _NC_CACHE:
        _NC_CACHE["nc"] = Builder().run()
    nc = _NC_CACHE["nc"]
    maps = make_in_maps(inputs)
    res = run_bass_kernel_spmd(nc, maps, core_ids=list(range(8)))
    out = np.stack([np.asarray(r["outT"]).T for r in res.results], axis=0)
    return np.ascontiguousarray(out.astype(np.float32))
```
